# Optimizing a Trainium2 kernel written in Bass

```python
import jax
import jax.numpy as jnp
from jax import lax
import numpy as np

D_MODEL = 1024
BATCH = 4
SEQ = 4096
DEPTH = 2

CTX_LEN = 256
GRID_W = 64
N_GROUPS = 4
GROUP_W = D_MODEL // N_GROUPS
HEAD_DIM = 64
N_GROUP_HEADS = GROUP_W // HEAD_DIM
NORM_EPS = 1e-6
ROPE_BASE = 10000.0
HG_CHUNK = 64
HG_F_FLOOR = 1e-30
RW_DECAY_LORA = 64
RW_A_LORA = 64
RW_GATE_LORA = 128
RW_LN_EPS = 64e-5
SWA_WINDOW = 128
SWA_KV_HEADS = 2
MLA_Q_RANK = 192
MLA_KV_RANK = 128
MLA_NOPE = 64
MLA_ROPE = 32
MLA_V = 64
MLA_Q_BLOCK = 128
D_FF = 2816
N_EXPERTS = 8
TOP_K = 2
D_FF_EXPERT = 1408

HG_COLS = 5 * GROUP_W
RW_COLS = 3 * GROUP_W + 2 * RW_DECAY_LORA + RW_A_LORA + RW_GATE_LORA
SWA_COLS = GROUP_W + 2 * SWA_KV_HEADS * HEAD_DIM
MLA_COLS = MLA_Q_RANK + MLA_KV_RANK + MLA_ROPE
GROUP_COLS = (HG_COLS, RW_COLS, SWA_COLS, MLA_COLS)
IN_COLS = HG_COLS + RW_COLS + SWA_COLS + MLA_COLS

F32 = jnp.float32

kernel_name = 'hybrid_diffusion_prefix_block'


def split_cols(p, sizes):
    idx = np.cumsum(sizes)[:-1].tolist()
    return jnp.split(p, idx, axis=-1)


def rms_norm(x, g):
    xf = x.astype(F32)
    y = xf * lax.rsqrt(jnp.mean(xf * xf, axis=-1, keepdims=True) + NORM_EPS)
    return (y * g.astype(F32)).astype(x.dtype)


def to_heads(x, n):
    B, T, _ = x.shape
    return x.reshape(B, T, n, -1).transpose(0, 2, 1, 3)


def from_heads(x):
    B, n, T, d = x.shape
    return x.transpose(0, 2, 1, 3).reshape(B, T, n * d)


def axial_rope_tables(n_tok, rot_dim):
    rows = n_tok // GRID_W
    row = jnp.repeat(jnp.arange(rows, dtype=F32), GRID_W)
    col = jnp.tile(jnp.arange(GRID_W, dtype=F32), rows)
    n_freq = rot_dim // 4
    inv = ROPE_BASE ** (-jnp.arange(n_freq, dtype=F32) / n_freq)
    ang = jnp.stack([row[:, None] * inv, col[:, None] * inv], axis=1)
    return jnp.cos(ang), jnp.sin(ang)


def apply_axial_rope(x, cos, sin):
    shp = x.shape
    xf = x.astype(F32).reshape(shp[:-1] + (2, 2, shp[-1] // 4))
    x1, x2 = xf[..., 0, :], xf[..., 1, :]
    out = jnp.stack([x1 * cos - x2 * sin, x1 * sin + x2 * cos], axis=-2)
    return out.reshape(shp)


def gla_chunked(q, k, v, logf, s0):
    B, H, T, dk = q.shape
    dv = v.shape[-1]
    C = HG_CHUNK
    n = T // C

    def blocks(a):
        return a.astype(F32).reshape(B, H, n, C, a.shape[-1]).transpose(2, 0, 1, 3, 4)

    qc, kc, vc = blocks(q), blocks(k), blocks(v)
    bc = jnp.cumsum(blocks(logf), axis=-2)
    causal = jnp.tril(jnp.ones((C, C), dtype=bool))[:, :, None]

    def step(S, inp):
        qb, kb, vb, bb = inp
        inter = jnp.einsum('bhtk,bhkv->bhtv', qb * jnp.exp(bb), S)
        rel = jnp.where(causal, bb[:, :, :, None, :] - bb[:, :, None, :, :], -jnp.inf)
        att = jnp.einsum('bhtk,bhsk,bhtsk->bhts', qb, kb, jnp.exp(rel))
        o = inter + jnp.einsum('bhts,bhsv->bhtv', att, vb)
        b_end = bb[:, :, -1:, :]
        S = jnp.exp(b_end[:, :, 0, :, None]) * S + jnp.einsum('bhsk,bhsv->bhkv', kb * jnp.exp(b_end - bb), vb)
        return S, o

    S, o = lax.scan(step, s0, (qc, kc, vc, bc))
    return o.transpose(1, 2, 0, 3, 4).reshape(B, H, T, dv), S


def hgrn2_mixer(p_lat, p_ctx, lb, norm_g, ctx_out):
    H = N_GROUP_HEADS

    def prep(p):
        q, zf, zb, i, g = split_cols(p.astype(F32), (GROUP_W,) * 5)
        dirs = []
        for d, z in enumerate((zf, zb)):
            f = lb[d] + (1.0 - lb[d]) * jax.nn.sigmoid(z)
            logf = jnp.log(jnp.maximum(f, HG_F_FLOOR))
            key_in = (1.0 - lb[d]) * jax.nn.sigmoid(-z)
            dirs.append((to_heads(logf, H), to_heads(key_in, H)))
        return to_heads(jax.nn.silu(q), H), to_heads(i, H), dirs, g

    def readout(o, g):
        o = o * lax.rsqrt(jnp.mean(o * o, axis=-1, keepdims=True) + NORM_EPS)
        return from_heads(o) * norm_g * jax.nn.silu(g)

    def flip(a):
        return a[:, :, ::-1]

    qc, ic, dc, gc = prep(p_ctx)
    ql, il, dl, gl = prep(p_lat)
    s0 = jnp.zeros(qc.shape[:2] + (HEAD_DIM, HEAD_DIM), F32)
    (lfc, kc), (lfl, kl) = dc[0], dl[0]
    oc_f, sc_f = gla_chunked(qc, kc, ic, lfc, s0)
    ol_f, _ = gla_chunked(ql, kl, il, lfl, sc_f)
    (lfc, kc), (lfl, kl) = dc[1], dl[1]
    oc_b, sc_b = gla_chunked(flip(qc), flip(kc), flip(ic), flip(lfc), s0)
    ol_b, _ = gla_chunked(flip(ql), flip(kl), flip(il), flip(lfl), sc_b)
    out_lat = readout(ol_f + flip(ol_b), gl)
    out_ctx = readout(oc_f + flip(oc_b), gc) if ctx_out else None
    return out_lat, out_ctx


def token_shift(p, mu):
    prev = jnp.pad(p, ((0, 0), (1, 0), (0, 0)))[:, :-1]
    nxt = jnp.pad(p, ((0, 0), (0, 1), (0, 0)))[:, 1:]
    return p + mu[0] * (prev - p) + mu[1] * (nxt - p)


def rwkv7_scan(r, w, k, v, kk, b, s0, reverse):
    def step(S, inp):
        r_t, w_t, k_t, v_t, kk_t, b_t = inp
        sa = jnp.einsum('bhvk,bhk->bhv', S, kk_t)
        S = S * w_t[:, :, None, :] - sa[..., None] * b_t[:, :, None, :] + v_t[..., None] * k_t[:, :, None, :]
        return S, jnp.einsum('bhvk,bhk->bhv', S, r_t)

    xs = tuple(a.transpose(1, 0, 2, 3) for a in (r, w, k, v, kk, b))
    S, o = lax.scan(step, s0, xs, reverse=reverse)
    return o.transpose(1, 0, 2, 3), S


def rwkv7_mixer(p_lat, p_ctx, mu, w0, w2, a0, a2, g2, k_k, k_a, r_k, ln_g, ln_b, ctx_out):
    H, N = N_GROUP_HEADS, HEAD_DIM
    sizes = (GROUP_W, GROUP_W, GROUP_W, RW_DECAY_LORA, RW_DECAY_LORA, RW_A_LORA, RW_GATE_LORA)

    def prep(p):
        B, T, _ = p.shape

        def hd(a):
            return a.reshape(B, T, H, N)

        r, k, v, yw_f, yw_b, ya, yg = split_cols(token_shift(p.astype(F32), mu), sizes)
        decays = []
        for d, yw in enumerate((yw_f, yw_b)):
            w_log = -jax.nn.softplus(-(w0[d] + jnp.tanh(yw) @ w2[d])) - 0.5
            decays.append(hd(jnp.exp(-jnp.exp(w_log))))
        a = hd(jax.nn.sigmoid(a0 + ya @ a2))
        kk = hd(k * k_k)
        kk = kk / jnp.maximum(jnp.sqrt(jnp.sum(kk * kk, axis=-1, keepdims=True)), 1e-12)
        k = hd(k) * (1.0 + (a - 1.0) * k_a.reshape(H, N))
        g = jax.nn.sigmoid(yg) @ g2
        return hd(r), k, hd(v), kk, kk * a, decays, g

    def readout(o, r, k, v, g):
        B, T = o.shape[:2]
        mean = jnp.mean(o, axis=-1, keepdims=True)
        var = jnp.mean(jnp.square(o - mean), axis=-1, keepdims=True)
        o = ((o - mean) * lax.rsqrt(var + RW_LN_EPS)).reshape(B, T, GROUP_W) * ln_g + ln_b
        bonus = jnp.sum(r * k * r_k, axis=-1, keepdims=True) * v
        return (o + bonus.reshape(B, T, GROUP_W)) * g

    rc, kc, vc, kkc, bc, wc, gc = prep(p_ctx)
    rl, kl, vl, kkl, bl, wl, gl = prep(p_lat)
    s0 = jnp.zeros((rc.shape[0], H, N, N), F32)
    oc_f, sc_f = rwkv7_scan(rc, wc[0], kc, vc, kkc, bc, s0, False)
    ol_f, _ = rwkv7_scan(rl, wl[0], kl, vl, kkl, bl, sc_f, False)
    oc_b, sc_b = rwkv7_scan(rc, wc[1], kc, vc, kkc, bc, s0, True)
    ol_b, _ = rwkv7_scan(rl, wl[1], kl, vl, kkl, bl, sc_b, True)
    out_lat = readout(ol_f + ol_b, rl, kl, vl, gl)
    out_ctx = readout(oc_f + oc_b, rc, kc, vc, gc) if ctx_out else None
    return out_lat, out_ctx


def swa_mixer(p_lat, p_ctx, sink, cos, sin, ctx_out):
    H, KV, d, W = N_GROUP_HEADS, SWA_KV_HEADS, HEAD_DIM, SWA_WINDOW
    G = H // KV
    scale = HEAD_DIM ** -0.5

    def prep(p):
        q, k, v = split_cols(p, (GROUP_W, KV * d, KV * d))
        return to_heads(q, H), to_heads(k, KV), to_heads(v, KV)

    ql, kl, vl = prep(p_lat)
    qc, kc, vc = prep(p_ctx)
    ql = apply_axial_rope(ql, cos, sin)
    kl = apply_axial_rope(kl, cos, sin)
    B, _, T, _ = ql.shape
    Lc = kc.shape[2]
    nb = T // W
    qb = ql.reshape(B, KV, G, nb, W, d)

    def band(a):
        ap = jnp.pad(a, ((0, 0), (0, 0), (W, W), (0, 0))).reshape(B, KV, nb + 2, W, d)
        return jnp.concatenate([ap[:, :, :nb], ap[:, :, 1:nb + 1], ap[:, :, 2:]], axis=3)

    kw, vw = band(kl), band(vl)
    i = jnp.arange(W)[:, None]
    j = jnp.arange(3 * W)[None, :]
    s_abs = (jnp.arange(nb)[:, None, None] - 1) * W + j[None]
    mask = (jnp.abs(W + i - j) <= W)[None] & (s_abs >= 0) & (s_abs < T)
    s_loc = jnp.einsum('bkgnqd,bknsd->bkgnqs', qb, kw).astype(F32) * scale
    s_ctx = jnp.einsum('bkgnqd,bkcd->bkgnqc', qb, kc).astype(F32) * scale
    sink_l = jnp.broadcast_to(sink.astype(F32).reshape(1, KV, G, 1, 1, 1), (B, KV, G, nb, W, 1))
    logits = jnp.concatenate([jnp.where(mask, s_loc, -jnp.inf), s_ctx, sink_l], axis=-1)
    prob = jax.nn.softmax(logits, axis=-1)
    o = (jnp.einsum('bkgnqs,bknsd->bkgnqd', prob[..., :3 * W], vw)
         + jnp.einsum('bkgnqc,bkcd->bkgnqd', prob[..., 3 * W:3 * W + Lc], vc))
    out_lat = o.transpose(0, 3, 4, 1, 2, 5).reshape(B, T, GROUP_W)
    out_ctx = None
    if ctx_out:
        qg = qc.reshape(B, KV, G, Lc, d)
        s = jnp.einsum('bkgqd,bkcd->bkgqc', qg, kc).astype(F32) * scale
        sink_c = jnp.broadcast_to(sink.astype(F32).reshape(1, KV, G, 1, 1), (B, KV, G, Lc, 1))
        prob_c = jax.nn.softmax(jnp.concatenate([s, sink_c], axis=-1), axis=-1)[..., :Lc]
        oc = jnp.einsum('bkgqc,bkcd->bkgqd', prob_c, vc)
        out_ctx = oc.transpose(0, 3, 1, 2, 4).reshape(B, Lc, GROUP_W)
    return out_lat, out_ctx


def mla_mixer(p_lat, p_ctx, qnorm_g, wuq, kvnorm_g, wukv, cos, sin, ctx_out):
    H = N_GROUP_HEADS
    scale = (MLA_NOPE + MLA_ROPE) ** -0.5

    def prep(p):
        cq, ckv, kr = split_cols(p, (MLA_Q_RANK, MLA_KV_RANK, MLA_ROPE))
        q = to_heads(rms_norm(cq, qnorm_g) @ wuq, H)
        kv = to_heads(rms_norm(ckv, kvnorm_g) @ wukv, H)
        return q[..., :MLA_NOPE], q[..., MLA_NOPE:], kv[..., :MLA_NOPE], kv[..., MLA_NOPE:], kr

    qn_l, qr_l, kn_l, v_l, kr_l = prep(p_lat)
    qn_c, qr_c, kn_c, v_c, kr_c = prep(p_ctx)
    qr_l = apply_axial_rope(qr_l, cos, sin)
    kr_l = apply_axial_rope(kr_l, cos, sin)
    kn = jnp.concatenate([kn_l, kn_c], axis=2)
    vv = jnp.concatenate([v_l, v_c], axis=2)
    kr = jnp.concatenate([kr_l, kr_c.astype(kr_l.dtype)], axis=1)
    B, _, T, _ = qn_l.shape
    nb = T // MLA_Q_BLOCK

    def blocks(a):
        return a.reshape(B, H, nb, MLA_Q_BLOCK, a.shape[-1]).transpose(2, 0, 1, 3, 4)

    def attend(qs):
        qn, qr = qs
        s = jnp.einsum('bhqd,bhkd->bhqk', qn, kn) + jnp.einsum('bhqd,bkd->bhqk', qr, kr)
        prob = jax.nn.softmax(s.astype(F32) * scale, axis=-1)
        return jnp.einsum('bhqk,bhkd->bhqd', prob, vv)

    o = lax.map(attend, (blocks(qn_l), blocks(qr_l)))
    out_lat = o.transpose(1, 0, 3, 2, 4).reshape(B, T, H * MLA_V)
    out_ctx = None
    if ctx_out:
        s = jnp.einsum('bhqd,bhkd->bhqk', qn_c, kn_c) + jnp.einsum('bhqd,bkd->bhqk', qr_c, kr_c)
        prob = jax.nn.softmax(s.astype(F32) * scale, axis=-1)
        out_ctx = from_heads(jnp.einsum('bhqk,bhkd->bhqd', prob, v_c))
    return out_lat, out_ctx


def swiglu(h, w1, w3, w2):
    return (jax.nn.silu(h @ w1) * (h @ w3)) @ w2


def moe_swiglu(h, router, w1, w3, w2):
    logits = (h @ router).astype(F32)
    top_val, top_idx = lax.top_k(logits, TOP_K)
    gates = jax.nn.softmax(top_val, axis=-1)
    combine = jnp.sum(jax.nn.one_hot(top_idx, N_EXPERTS, dtype=F32) * gates[..., None], axis=-2)
    out = jnp.zeros(h.shape, F32)
    for e in range(N_EXPERTS):
        out = out + combine[..., e:e + 1] * swiglu(h, w1[e], w3[e], w2[e])
    return out


def setup_inputs(seed: int = 0) -> dict:
    key = jax.random.key(seed)
    ks = iter(jax.random.split(key, 40))

    def nrm(shape, std):
        return std * jax.random.normal(next(ks), shape, F32)

    L = DEPTH
    ND = (DEPTH + 1) // 2
    NM = DEPTH // 2
    H = N_GROUP_HEADS
    D = D_MODEL
    return {
        'x': nrm((BATCH, SEQ, D), 1.0),
        'c': nrm((BATCH, D), 1.0),
        'ctx': nrm((BATCH, CTX_LEN, D), 1.0),
        'c_ctx': nrm((D,), 1.0),
        'w_ada': nrm((L, D, 6 * D), 0.5 * D ** -0.5),
        'b_ada': nrm((L, 6 * D), 0.02),
        'norm1_g': 1.0 + nrm((L, D), 0.1),
        'norm2_g': 1.0 + nrm((L, D), 0.1),
        'w_in': nrm((L, D, IN_COLS), D ** -0.5),
        'hg_lb': nrm((L, 2, GROUP_W), 1.0),
        'hg_norm_g': 1.0 + nrm((L, GROUP_W), 0.1),
        'rw_mu': 0.3 + nrm((L, 2, RW_COLS), 0.1),
        'rw_w0': -1.0 + nrm((L, 2, GROUP_W), 0.5),
        'rw_w2': nrm((L, 2, RW_DECAY_LORA, GROUP_W), 0.5 * RW_DECAY_LORA ** -0.5),
        'rw_a0': nrm((L, GROUP_W), 0.3),
        'rw_a2': nrm((L, RW_A_LORA, GROUP_W), RW_A_LORA ** -0.5),
        'rw_g2': nrm((L, RW_GATE_LORA, GROUP_W), RW_GATE_LORA ** -0.5),
        'rw_k_k': 0.85 + nrm((L, GROUP_W), 0.1),
        'rw_k_a': 1.0 + nrm((L, GROUP_W), 0.1),
        'rw_r_k': nrm((L, H, HEAD_DIM), 0.1),
        'rw_ln_g': 1.0 + nrm((L, GROUP_W), 0.1),
        'rw_ln_b': nrm((L, GROUP_W), 0.02),
        'swa_sink': nrm((L, H), 0.5),
        'mla_qnorm_g': 1.0 + nrm((L, MLA_Q_RANK), 0.1),
        'mla_wuq': nrm((L, MLA_Q_RANK, H * (MLA_NOPE + MLA_ROPE)), MLA_Q_RANK ** -0.5),
        'mla_kvnorm_g': 1.0 + nrm((L, MLA_KV_RANK), 0.1),
        'mla_wukv': nrm((L, MLA_KV_RANK, H * (MLA_NOPE + MLA_V)), MLA_KV_RANK ** -0.5),
        'w_out': nrm((L, N_GROUPS * GROUP_W, D), (N_GROUPS * GROUP_W) ** -0.5),
        'ffn_w1': nrm((ND, D, D_FF), D ** -0.5),
        'ffn_w3': nrm((ND, D, D_FF), D ** -0.5),
        'ffn_w2': nrm((ND, D_FF, D), D_FF ** -0.5),
        'moe_router': nrm((NM, D, N_EXPERTS), D ** -0.5),
        'moe_w1': nrm((NM, N_EXPERTS, D, D_FF_EXPERT), D ** -0.5),
        'moe_w3': nrm((NM, N_EXPERTS, D, D_FF_EXPERT), D ** -0.5),
        'moe_w2': nrm((NM, N_EXPERTS, D_FF_EXPERT, D), D_FF_EXPERT ** -0.5),
        'final_norm_g': 1.0 + nrm((D,), 0.1),
    }


def reference(x, c, ctx, c_ctx, w_ada, b_ada, norm1_g, norm2_g, w_in, hg_lb, hg_norm_g,
              rw_mu, rw_w0, rw_w2, rw_a0, rw_a2, rw_g2, rw_k_k, rw_k_a, rw_r_k, rw_ln_g, rw_ln_b,
              swa_sink, mla_qnorm_g, mla_wuq, mla_kvnorm_g, mla_wukv, w_out,
              ffn_w1, ffn_w3, ffn_w2, moe_router, moe_w1, moe_w3, moe_w2, final_norm_g):
    T = x.shape[1]
    cos_h, sin_h = axial_rope_tables(T, HEAD_DIM)
    cos_m, sin_m = axial_rope_tables(T, MLA_ROPE)
    lb_w = jax.nn.softmax(hg_lb.astype(F32), axis=0)
    lb_all = jnp.cumsum(lb_w, axis=0) - lb_w[:1]

    def channel_mix(h, l):
        if l % 2 == 0:
            e = l // 2
            return swiglu(h, ffn_w1[e], ffn_w3[e], ffn_w2[e])
        e = l // 2
        return moe_swiglu(h, moe_router[e], moe_w1[e], moe_w3[e], moe_w2[e])

    x_lat, x_ctx = x, ctx
    for l in range(DEPTH):
        ctx_out = l < DEPTH - 1
        mod = jax.nn.silu(c) @ w_ada[l] + b_ada[l]
        mod_c = jax.nn.silu(c_ctx) @ w_ada[l] + b_ada[l]
        sh1, sc1, g1, sh2, sc2, g2 = jnp.split(mod[:, None, :], 6, axis=-1)
        csh1, csc1, cg1, csh2, csc2, cg2 = jnp.split(mod_c, 6, axis=-1)

        h_l = rms_norm(x_lat, norm1_g[l]) * (1.0 + sc1) + sh1
        h_c = rms_norm(x_ctx, norm1_g[l]) * (1.0 + csc1) + csh1
        pl = split_cols(h_l @ w_in[l], GROUP_COLS)
        pc = split_cols(h_c @ w_in[l], GROUP_COLS)

        hg_l, hg_c = hgrn2_mixer(pl[0], pc[0], lb_all[l], hg_norm_g[l], ctx_out)
        rw_l, rw_c = rwkv7_mixer(pl[1], pc[1], rw_mu[l], rw_w0[l], rw_w2[l], rw_a0[l], rw_a2[l], rw_g2[l],
                                 rw_k_k[l], rw_k_a[l], rw_r_k[l], rw_ln_g[l], rw_ln_b[l], ctx_out)
        sw_l, sw_c = swa_mixer(pl[2], pc[2], swa_sink[l], cos_h, sin_h, ctx_out)
        ml_l, ml_c = mla_mixer(pl[3], pc[3], mla_qnorm_g[l], mla_wuq[l], mla_kvnorm_g[l], mla_wukv[l],
                               cos_m, sin_m, ctx_out)

        x_lat = x_lat + g1 * (jnp.concatenate([hg_l, rw_l, sw_l, ml_l], axis=-1) @ w_out[l])
        h2 = rms_norm(x_lat, norm2_g[l]) * (1.0 + sc2) + sh2
        x_lat = x_lat + g2 * channel_mix(h2, l)
        if ctx_out:
            x_ctx = x_ctx + cg1 * (jnp.concatenate([hg_c, rw_c, sw_c, ml_c], axis=-1) @ w_out[l])
            h2c = rms_norm(x_ctx, norm2_g[l]) * (1.0 + csc2) + csh2
            x_ctx = x_ctx + cg2 * channel_mix(h2c, l)

    return rms_norm(x_lat, final_norm_g).astype(x.dtype)
```

```python
import numpy as np
import concourse.bass as bass
import concourse.mybir as mybir
from concourse.bass_utils import run_bass_kernel_spmd

F32 = mybir.dt.float32
BF16 = mybir.dt.bfloat16
ALU = mybir.AluOpType
AF = mybir.ActivationFunctionType
AX = mybir.AxisListType


class Sched:
    NDMA = 40
    ROT = 20000

    def __init__(self, nc):
        self.nc = nc
        self.e = dict(pe=nc.tensor, act=nc.scalar, dve=nc.vector, pool=nc.gpsimd, sp=nc.sync)
        self.sem = {}
        self.cnt = {}
        self.nsem = {}
        for k in ('pe', 'act', 'dve', 'pool'):
            self.nsem[k] = 0
            self.sem[k] = nc.alloc_semaphore(f"s_{k}_0")
            self.cnt[k] = 0
        self.seen = {k: {} for k in self.e}
        self.dsem = [nc.alloc_semaphore(f"sd{i}") for i in range(self.NDMA)]
        self.dcnt = [0] * self.NDMA
        self.drr = 0
        self.res = {}
        self.ninst = 0
        self.out_evs = {}

    @staticmethod
    def _merge(dst, src):
        for n, (h, v) in src.items():
            if n not in dst or dst[n][1] < v:
                dst[n] = (h, v)

    def _st(self, key):
        s = self.res.get(key)
        if s is None:
            s = dict(w={}, r={}, old={}, phase='w')
            self.res[key] = s
        return s

    def _key(self, x):
        if isinstance(x, str):
            return x, False
        if isinstance(x, tuple):
            return self._key(x[1])[0], True
        return x.name, False

    def _need(self, reads, writes):
        need = {}
        for r in reads:
            k, _ = self._key(r)
            self._merge(need, self._st(k)['w'])
            if k.startswith('ps'):
                self._merge(need, self._st(k)['r'])
        for w in writes:
            k, par = self._key(w)
            s = self._st(k)
            if par:
                if s['phase'] == 'r':
                    old = {}
                    self._merge(old, s['w'])
                    self._merge(old, s['r'])
                    s['old'] = old
                    s['w'] = {}
                    s['r'] = {}
                    s['phase'] = 'w'
                self._merge(need, s['old'])
            else:
                self._merge(need, s['w'])
                self._merge(need, s['r'])
                self._merge(need, s['old'])
        return need

    def _commit(self, reads, writes, ev):
        for r in reads:
            k, _ = self._key(r)
            s = self._st(k)
            self._merge(s['r'], ev)
            s['phase'] = 'r'
        for w in writes:
            k, par = self._key(w)
            s = self._st(k)
            if par:
                self._merge(s['w'], ev)
            else:
                s['w'] = dict(ev)
                s['r'] = {}
                s['old'] = {}
                s['phase'] = 'w'

    def _wait(self, eng, need):
        seen = self.seen[eng]
        for n, (h, v) in need.items():
            if eng == 'pe' and n.startswith('s_pe_'):
                continue
            if seen.get(n, 0) >= v:
                continue
            for k in ('pe', 'act', 'dve', 'pool'):
                if n == f"s_{k}_{self.nsem[k]}":
                    assert v <= self.cnt[k], (eng, n, v, self.cnt[k])
            self.e[eng].wait_ge(h, v)
            self.ninst += 1
            seen[n] = v

    def op(self, eng, fn, reads=(), writes=(), inc=True):
        need = self._need(reads, writes)
        self._wait(eng, need)
        ins = fn(self.e[eng])
        self.ninst += 1
        if inc:
            if self.cnt[eng] >= self.ROT:
                self.nsem[eng] += 1
                self.sem[eng] = self.nc.alloc_semaphore(f"s_{eng}_{self.nsem[eng]}")
                self.cnt[eng] = 0
            self.cnt[eng] += 1
            ins.then_inc(self.sem[eng], 1)
            ev = {self.sem[eng].name: (self.sem[eng], self.cnt[eng])}
        else:
            assert self.cnt[eng] < self.ROT - 64
            ev = {self.sem[eng].name: (self.sem[eng], self.cnt[eng] + 1)}
        self._commit(reads, writes, ev)
        return ev

    def dma(self, out, in_, reads=None, writes=None, q='sp', **kw):
        reads = [in_] if reads is None else reads
        writes = [out] if writes is None else writes
        i = self.drr
        self.drr = (self.drr + 1) % self.NDMA
        h = self.dsem[i]
        need = self._need(reads, writes)
        if self.dcnt[i] > 0:
            self._merge(need, {h.name: (h, 16 * self.dcnt[i])})
        self._wait(q, need)
        self.e[q].dma_start(out=out, in_=in_, **kw).then_inc(h, 16)
        self.ninst += 1
        self.dcnt[i] += 1
        ev = {h.name: (h, 16 * self.dcnt[i])}
        self._commit(reads, writes, ev)
        return ev

    def finish(self, keys, eng='sp'):
        need = {}
        for k in keys:
            kk, _ = self._key(k)
            self._merge(need, self._st(kk)['w'])
        self._wait(eng, need)

    def mm(self, out, lhsT, rhs, start=True, stop=True, inc=None, extra_r=()):
        inc = stop if inc is None else inc
        return self.op('pe', lambda e: e.matmul(out, lhsT, rhs, start=start, stop=stop),
                       reads=[lhsT, rhs, *extra_r], writes=[out], inc=inc)

    def tr(self, out, in_, ident, inc=True):
        return self.op('pe', lambda e: e.transpose(out, in_, ident), reads=[in_, ident], writes=[out], inc=inc)

    def act(self, out, in_, func, bias=None, scale=None, accum_out=None, eng='act'):
        kw = {}
        reads = [in_]
        writes = [out]
        if bias is not None:
            kw['bias'] = bias
            if not isinstance(bias, (int, float)):
                reads.append(bias)
        if scale is not None:
            kw['scale'] = scale
            if not isinstance(scale, (int, float)):
                reads.append(scale)
        if accum_out is not None:
            kw['accum_out'] = accum_out
            writes.append(accum_out)
        return self.op('act', lambda e: e.activation(out, in_, func, **kw), reads=reads, writes=writes)

    def tt(self, out, in0, in1, op, eng='dve'):
        return self.op(eng, lambda e: e.tensor_tensor(out, in0, in1, op), reads=[in0, in1], writes=[out])

    def ts(self, out, in0, s1, s2, op0, op1=None, eng='dve', accum_out=None):
        reads = [in0] + [s for s in (s1, s2) if s is not None and not isinstance(s, (int, float))]
        writes = [out] + ([accum_out] if accum_out is not None else [])
        kw = {}
        if accum_out is not None:
            kw['accum_out'] = accum_out
        if op1 is None:
            return self.op(eng, lambda e: e.tensor_scalar(out, in0, s1, None, op0, **kw), reads=reads, writes=writes)
        return self.op(eng, lambda e: e.tensor_scalar(out, in0, s1, s2, op0, op1, **kw), reads=reads, writes=writes)

    def stt(self, out, in0, scalar, in1, op0, op1, eng='dve'):
        reads = [in0, in1] + ([scalar] if not isinstance(scalar, (int, float)) else [])
        return self.op(eng, lambda e: e.scalar_tensor_tensor(out, in0, scalar, in1, op0, op1), reads=reads, writes=[out])

    def copy(self, out, in_, eng='dve'):
        if eng == 'act':
            return self.op('act', lambda e: e.copy(out, in_), reads=[in_], writes=[out])
        return self.op(eng, lambda e: e.tensor_copy(out, in_), reads=[in_], writes=[out])

    def memset(self, ap, val, eng='dve'):
        return self.op(eng, lambda e: e.memset(ap, val), reads=[], writes=[ap])

    def reduce(self, out, in_, op, eng='dve'):
        return self.op(eng, lambda e: e.tensor_reduce(out, in_, AX.X, op), reads=[in_], writes=[out])


from contextlib import ExitStack

D = 1024
NT = 34
TOK = 4352
INC = 3232
EPS = 1e-6
PROWS = 4356


def prow(t):
    return 1 + t if t < 256 else 3 + t


class Ctx:
    pass


def barrier(C):
    s = C.s
    need = {}
    for k in ('pe', 'act', 'dve', 'pool'):
        if s.cnt[k] > 0:
            need[s.sem[k].name] = (s.sem[k], s.cnt[k])
    for i, h in enumerate(s.dsem):
        if s.dcnt[i] > 0:
            need[h.name] = (h, 16 * s.dcnt[i])
    for eng in ('pe', 'act', 'dve', 'pool', 'sp'):
        seen = s.seen[eng]
        for n, (h, v) in need.items():
            if seen.get(n, 0) >= v:
                continue
            s.e[eng].wait_ge(h, v)
            s.ninst += 1
            seen[n] = v
    s.res = {}


class Stage:
    def __init__(self, C, name):
        self.C = C
        self.name = name
        self.es = ExitStack()
        self.n = 0

    def __enter__(self):
        self.es.__enter__()
        return self

    def sb(self, shape, dtype=F32, name=None):
        self.n += 1
        nm = f"{self.name}_{name or 't'}{self.n}"
        t = self.es.enter_context(self.C.nc.sbuf_tensor(nm, list(shape), dtype))
        return t.ap()

    def __exit__(self, *a):
        barrier(self.C)
        return self.es.__exit__(*a)


def bcast_load(C, st, vec_ap, n, name, q='sp'):
    t = st.sb([128, n], F32, name)
    C.s.dma(t, vec_ap.partition_broadcast(128), q=q)
    return t


def stage_consts(C):
    nc, s = C.nc, C.s
    C.ident = nc.alloc_sbuf_tensor("ident_sb", [128, 128], F32).ap()
    C.identb = nc.alloc_sbuf_tensor("identb", [128, 128], BF16).ap()
    C.zero = nc.alloc_sbuf_tensor("zero_sb", [128, 808], F32).ap()
    C.ones = nc.alloc_sbuf_tensor("ones_sb", [128, 128], F32).ap()
    s.dma(C.ident, C.d['ident'])
    s.copy(C.identb, C.ident)
    s.memset(C.zero, 0.0)
    s.memset(C.ones, 1.0)
    for r in (0, 257, 258, 4355):
        for q4 in range(4):
            s.dma(C.P[r:r + 1, q4 * 808:(q4 + 1) * 808], C.zero[0:1, :], writes=[('par', C.P)], q='pool')
    C.ps = [nc.alloc_psum_tensor(f"ps{i}", [128, 512], F32).ap() for i in range(6)]
    C.psb = [nc.alloc_psum_tensor(f"psb{i}", [128, 1024], BF16).ap() for i in range(2)]
    barrier(C)


def stage_mod(C, l):
    nc, s, d = C.nc, C.s, C.d
    with Stage(C, f"mod{l}") as t:
        MOD = t.sb([128, 2, 6144], F32, "MOD")
        cT = t.sb([128, 16], F32)
        cs = t.sb([128, 16], F32)
        cb = t.sb([128, 16, 128], F32)
        brow = t.sb([1, 6144], F32)
        s.dma(cT, d['cT'])
        s.dma(brow, d['b_ada'][l:l + 1, :])
        s.act(cs, cT, AF.Silu)
        for j in range(16):
            s.copy(cb[:, j, :], cs[:, j:j + 1].to_broadcast([128, 128]), eng='pool' if j % 2 else 'dve')
        wts = [t.sb([128, 8, 512], F32, f"wa{i}") for i in range(2)]
        wv = d['w_ada'][l].rearrange("(c p) n -> p c n", p=128)
        for n in range(12):
            w = wts[n % 2]
            s.dma(w, wv[:, :, n * 512:(n + 1) * 512])
            for r in range(2):
                ps = C.ps[(2 * n + r) % 4]
                for k in range(8):
                    s.mm(ps, cb[:, r * 8 + k, :], w[:, k, :], start=(k == 0), stop=False)
                s.mm(ps, C.ones[0:1, :], brow[0:1, n * 512:(n + 1) * 512], start=False, stop=True)
                s.copy(MOD[:, r, n * 512:(n + 1) * 512], ps, eng='act' if r else 'dve')
        g1 = bcast_load(C, t, d['norm1_g'][l], 1024, "g1")
        g2 = bcast_load(C, t, d['norm2_g'][l], 1024, "g2")
        for r in range(2):
            for (seg, g) in ((1, g1), (4, g2)):
                sl = MOD[:, r, seg * 1024:(seg + 1) * 1024]
                s.stt(sl, sl, 1.0, g, ALU.add, ALU.mult)
        for r in range(2):
            s.dma(C.MODD[l][r:r + 1, :], MOD[0:1, r, :], writes=[('par', C.MODD[l])], q='pool')


def rmsnorm_mod_T(C, st, xt, G, sh, hT, tmp):
    s = C.s
    s.act(tmp['junk'], xt, AF.Square, accum_out=tmp['ss'])
    s.act(tmp['rs'], tmp['ss'], AF.Sqrt, bias=EPS, scale=1.0 / D)
    s.op('dve', lambda e: e.reciprocal(tmp['rs'], tmp['rs']), reads=[tmp['rs']], writes=[tmp['rs']])
    s.stt(tmp['t1'], xt, tmp['rs'], G, ALU.mult, ALU.mult)
    s.tt(tmp['hb'], tmp['t1'], sh, ALU.add, eng='pool')
    pb = C.psb[tmp['i'] % 2]
    tmp['i'] += 1
    for k in range(8):
        s.tr(pb[:, k * 128:(k + 1) * 128], tmp['hb'][:, k * 128:(k + 1) * 128], C.identb, inc=(k == 7))
    s.copy(hT, pb.rearrange("p (c n) -> p c n", c=8), eng='act')


def norm_tmp(st):
    return dict(junk=st.sb([128, 1024], BF16), ss=st.sb([128, 1], F32), rs=st.sb([128, 1], F32),
                t1=st.sb([128, 1024], F32), hb=st.sb([128, 1024], BF16), i=0)


def load_cast_w(C, st, dst_bf, src_rows, stg, q='sp', eng='pool'):
    C.s.dma(stg, src_rows, q=q)
    C.s.copy(dst_bf, stg, eng=eng)


def stage_win(C, l, MODD, xsrc):
    nc, s, d = C.nc, C.s, C.d
    with Stage(C, f"win{l}") as t:
        wb = t.sb([128, 8, INC], BF16, "wb")
        stg = [t.sb([128, INC], F32, f"stg{i}") for i in range(2)]
        for k in range(8):
            load_cast_w(C, t, wb[:, k, :], d['w_in'][l, k * 128:(k + 1) * 128, :], stg[k % 2], eng='pool')
        tmp = norm_tmp(t)
        seg = {(r, sg): bcast_load(C, t, MODD[r, sg * 1024:(sg + 1) * 1024], 1024, f"seg{r}{sg}") for r in (0, 1) for sg in (0, 1)}
        xts = [t.sb([128, 1024], F32, f"x{i}") for i in range(2)]
        hTs = [t.sb([128, 8, 128], BF16, f"hT{i}") for i in range(2)]
        pts = [t.sb([128, INC], F32, f"p{i}") for i in range(2)]
        for i in range(NT):
            r = 0 if i < 2 else 1
            xt, hT, pt = xts[i % 2], hTs[i % 2], pts[i % 2]
            s.dma(xt, xsrc[i * 128:(i + 1) * 128, :])
            rmsnorm_mod_T(C, t, xt, seg[(r, 1)], seg[(r, 0)], hT, tmp)
            for n in range(7):
                c0, c1 = n * 512, min(INC, (n + 1) * 512)
                ps = C.ps[n % 4]
                for k in range(8):
                    s.mm(ps[:, 0:c1 - c0], hT[:, k, :], wb[:, k, c0:c1], start=(k == 0), stop=(k == 7))
                s.copy(pt[:, c0:c1], ps[:, 0:c1 - c0], eng='act' if n % 2 else 'dve')
            r0 = prow(i * 128)
            s.dma(C.P[r0:r0 + 128, :], pt, writes=[('par', C.P)], q='pool')


WSHAPES = dict(
    w_ada=(2, 1024, 6144), b_ada=(2, 6144), norm1_g=(2, 1024), norm2_g=(2, 1024), w_in=(2, 1024, 3232),
    hg_lb=(2, 2, 256), hg_norm_g=(2, 256), rw_mu=(2, 2, 1088), rw_w0=(2, 2, 256), rw_w2=(2, 2, 64, 256),
    rw_a0=(2, 256), rw_a2=(2, 64, 256), rw_g2=(2, 128, 256), rw_k_k=(2, 256), rw_k_a=(2, 256),
    rw_r_k=(2, 4, 64), rw_ln_g=(2, 256), rw_ln_b=(2, 256), swa_sink=(2, 4), mla_qnorm_g=(2, 192),
    mla_wuq=(2, 192, 384), mla_kvnorm_g=(2, 128), mla_wukv=(2, 128, 512), w_out=(2, 1024, 1024),
    ffn_w1=(1, 1024, 2816), ffn_w3=(1, 1024, 2816), ffn_w2=(1, 2816, 1024), moe_router=(1, 1024, 8),
    moe_w1=(1, 8, 1024, 1408), moe_w3=(1, 8, 1024, 1408), moe_w2=(1, 8, 1408, 1024), final_norm_g=(1024,),
)
CSHAPES = dict(ident=(128, 128), cT=(128, 16), cosH=(4096, 32), sinH=(4096, 32), cosM=(4096, 16), sinM=(4096, 16), maskP=(128, 128), maskN=(128, 128), scn=(2, 64, 450), bdm=(128, 128))


def build(upto=None, dbg=(), din=()):
    nc = bass.Bass("TRN2", target_bir_lowering=False)
    C = Ctx()
    C.nc = nc
    C.s = Sched(nc)
    C.d = {}
    C.d['xin'] = nc.dram_tensor("xin", [TOK, D], F32, kind="ExternalInput").ap()
    for k, shp in {**WSHAPES, **CSHAPES}.items():
        C.d[k] = nc.dram_tensor(k, list(shp), F32, kind="ExternalInput").ap()
    C.out = nc.dram_tensor("out", [4096, D], F32, kind="ExternalOutput").ap()

    def scratch(name, shape):
        kind = "ExternalOutput" if name in dbg else "Internal"
        return nc.dram_tensor(name, list(shape), F32, kind=kind).ap()
    C.P = scratch("P", [PROWS, INC])
    C.scratch = scratch
    C.MODD = [scratch(f"MODD{l}", [2, 6144]) for l in range(2)]
    C.CAT = scratch("CAT", [TOK, D]) if "CAT" not in din else nc.dram_tensor("CAT", [TOK, D], F32, kind="ExternalInput").ap()
    C.XMID = scratch("XMID", [TOK, D])
    C.XS = scratch("XS", [TOK, D])

    def bscratch(name, shape):
        return nc.dram_tensor(name, list(shape), BF16).ap()
    C.WB = dict(f1=bscratch("WBf1", [1024, 2816]), f3=bscratch("WBf3", [1024, 2816]), f2=bscratch("WBf2", [2816, 1024]),
                m1=[bscratch(f"WBm1_{e}", [1024, 1408]) for e in range(8)],
                m3=[bscratch(f"WBm3_{e}", [1024, 1408]) for e in range(8)],
                m2=[bscratch(f"WBm2_{e}", [1408, 1024]) for e in range(8)])
    C.SCH = scratch("SCH", [TOK, HGC['n']])
    C.SCR = scratch("SCR", [TOK, RWC['n']])
    C.OHG = [scratch(f"OHG{i}", [TOK, 256]) for i in range(2)]
    C.ORW = [scratch(f"ORW{i}", [TOK, 256]) for i in range(2)]
    stage_consts(C)
    program(C, upto)
    C.s.finish([C.out] + [n for n in dbg], 'sp')
    print("ninst", C.s.ninst, "sems", {k: v for k, v in C.s.nsem.items()})
    return nc


def program(C, upto):
    if upto in ('ffn', 'moe'):
        with Stage(C, "precast") as t_:
            for _ in precast_gen(C, t_):
                pass
    for l in range(2):
        xsrc = C.d['xin'] if l == 0 else C.XS
        ctx_out = l < 1
        stage_mod(C, l)
        stage_win(C, l, C.MODD[l], xsrc)
        if upto == 'win':
            return
        if upto in (None, 'scans'):
            stage_hg_prep(C, l)
            stage_rw_prep(C, l)
            stage_scans(C, l)
            stage_hg_read(C, l)
            stage_rw_read(C, l)
            if upto == 'scans':
                return
        if upto in (None, 'attn'):
            stage_swa(C, l, ctx_out)
            stage_mla(C, l, ctx_out)
            if upto == 'attn':
                return
        stage_out_ffn(C, l, C.MODD[l], xsrc, moe=(l % 2 == 1))
        if upto == 'ffn':
            return
        if upto == 'L0':
            return
    stage_final(C)


def host_inputs(inputs, b):
    m = {}
    m['xin'] = np.ascontiguousarray(np.concatenate([inputs['ctx'][b], inputs['x'][b]], axis=0), dtype=np.float32)
    cc = np.stack([np.asarray(inputs['c_ctx'], np.float32), np.asarray(inputs['c'][b], np.float32)], 0)
    m['cT'] = np.ascontiguousarray(cc.reshape(2, 8, 128).transpose(2, 0, 1).reshape(128, 16))
    m['ident'] = np.eye(128, dtype=np.float32)
    m.update(rope_consts())
    m.update(scan_consts())
    for k in WSHAPES:
        m[k] = np.ascontiguousarray(inputs[k], dtype=np.float32)
    return m


def precast_gen(C, t):
    nc, s, d = C.nc, C.s, C.d
    jobs = []
    for nm, src in (('f1', d['ffn_w1'][0]), ('f3', d['ffn_w3'][0]), ('f2', d['ffn_w2'][0])):
        jobs.append((C.WB[nm], src))
    for e in range(8):
        jobs.append((C.WB['m1'][e], d['moe_w1'][0, e]))
        jobs.append((C.WB['m3'][e], d['moe_w3'][0, e]))
        jobs.append((C.WB['m2'][e], d['moe_w2'][0, e]))
    NB = 2
    stg = [t.sb([128, 2816], F32, f"pcs{i}") for i in range(NB)]
    wbf = [t.sb([128, 2816], BF16, f"pcw{i}") for i in range(NB)]
    it = 0
    for dst, src in jobs:
        rows, cols = src.shape
        per = max(1, 2816 // cols)
        nchunk = rows // 128
        c0 = 0
        while c0 < nchunk:
            n = min(per, nchunk - c0)
            sg = stg[it % NB]
            wb = wbf[it % NB]
            sv = sg[:, 0:n * cols].rearrange("p (a n) -> p a n", a=n)
            wv = wb[:, 0:n * cols].rearrange("p (a n) -> p a n", a=n)
            s.dma(sv, src[c0 * 128:(c0 + n) * 128, :].rearrange("(a p) n -> p a n", p=128), reads=[src], writes=[sg])
            s.copy(wv, sv, eng='act')
            s.dma(dst[c0 * 128:(c0 + n) * 128, :].rearrange("(a p) n -> p a n", p=128), wv, reads=[wb], writes=[('par', dst)], q='pool')
            c0 += n
            it += 1
            yield


def stage_out_ffn(C, l, MODD, xsrc, moe):
    nc, s, d = C.nc, C.s, C.d
    G = 8
    groups = ([[0, 1]] if l == 0 else []) + [list(range(2 + g * G, 2 + (g + 1) * G)) for g in range(32 // G)]
    WB = C.WB
    if moe:
        experts = [(WB['m1'][e], WB['m3'][e], WB['m2'][e]) for e in range(8)]
    else:
        experts = [(WB['f1'][:, h * 1408:(h + 1) * 1408], WB['f3'][:, h * 1408:(h + 1) * 1408],
                    WB['f2'][h * 1408:(h + 1) * 1408, :]) for h in range(2)]
    with Stage(C, f"ffn{l}") as t:
        wob = t.sb([128, 8, 1024], BF16, "wob")
        stg = [t.sb([128, 1024], F32, f"stg{i}") for i in range(2)]
        for k in range(8):
            load_cast_w(C, t, wob[:, k, :], d['w_out'][l, k * 128:(k + 1) * 128, :], stg[k % 2])
        segt = {sg: t.sb([128, 1024], F32, f"seg{sg}") for sg in (2, 3, 4, 5)}
        seg = {(r, sg): segt[sg] for r in (0, 1) for sg in (2, 3, 4, 5)}
        cur_r = [None]
        if moe:
            rt = t.sb([128, 8, 8], F32, "router")
            s.dma(rt, d['moe_router'][0].rearrange("(c p) e -> p c e", p=128))
        tmp = norm_tmp(t)
        cts = [t.sb([128, 1024], F32, f"cat{i}") for i in range(1)]
        xts = [t.sb([128, 1024], F32, f"x{i}") for i in range(1)]
        catb = t.sb([128, 1024], BF16, "catb")
        catT = t.sb([128, 8, 128], BF16, "catT")
        xm = t.sb([128, 1024], F32, "xm")
        h2T = t.sb([128, 8, G * 128], BF16, "h2T")
        Y = t.sb([128, G, 1024], F32, "Y")
        actT = t.sb([128, 11, 512], BF16, "actT")
        sa = [t.sb([128, 512], BF16, f"sa{i}") for i in range(2)]
        w1b = t.sb([128, 8, 1408], BF16, "w1b")
        w3b = t.sb([128, 8, 1408], BF16, "w3b")
        w2b = t.sb([128, 11, 1024], BF16, "w2b")
        comb = t.sb([128, G, 8], F32, "comb")
        if moe:
            h2f = t.sb([128, 1024], F32, "h2f")
            h2Tf = t.sb([128, 8, 128], F32, "h2Tf")
            lg = t.sb([128, 8], F32, "lg")
            mx = t.sb([128, 8], F32, "mx")
            nv1 = t.sb([128, 1], F32, "nv1")
            msk = t.sb([128, 8], F32, "msk")
            ex = t.sb([128, 8], F32, "ex")
            sm = t.sb([128, 1], F32, "sm")
        ti = 0
        for grp in groups:
            r = 0 if grp[0] < 2 else 1
            ng = len(grp)
            if cur_r[0] != r:
                cur_r[0] = r
                for sg in (2, 3, 4, 5):
                    s.dma(segt[sg], MODD[r, sg * 1024:(sg + 1) * 1024].partition_broadcast(128))
            for j, i in enumerate(grp):
                ct, xt = cts[0], xts[0]
                ti += 1
                s.dma(ct, C.CAT[i * 128:(i + 1) * 128, :])
                s.dma(xt, xsrc[i * 128:(i + 1) * 128, :])
                s.copy(catb, ct, eng='pool')
                pb = C.psb[tmp['i'] % 2]
                tmp['i'] += 1
                for k in range(8):
                    s.tr(pb[:, k * 128:(k + 1) * 128], catb[:, k * 128:(k + 1) * 128], C.identb, inc=(k == 7))
                s.copy(catT, pb.rearrange("p (c n) -> p c n", c=8), eng='act')
                for hf in range(2):
                    ps = C.ps[hf]
                    for k in range(8):
                        s.mm(ps, catT[:, k, :], wob[:, k, hf * 512:(hf + 1) * 512], start=(k == 0), stop=(k == 7))
                    s.tt(xm[:, hf * 512:(hf + 1) * 512], ps, seg[(r, 2)][:, hf * 512:(hf + 1) * 512], ALU.mult)
                s.tt(xm, xm, xt, ALU.add, eng='pool')
                s.dma(C.XMID[i * 128:(i + 1) * 128, :], xm, writes=[('par', C.XMID)], q='pool')
                rmsnorm_mod_T(C, t, xm, seg[(r, 4)], seg[(r, 3)], h2T[:, :, j * 128:(j + 1) * 128], tmp)
                if moe:
                    s.tt(h2f, tmp['t1'], seg[(r, 3)], ALU.add)
                    for hf in range(2):
                        ps = C.ps[2 + hf]
                        for k in range(4):
                            kk = hf * 4 + k
                            s.tr(ps[:, k * 128:(k + 1) * 128], h2f[:, kk * 128:(kk + 1) * 128], C.ident, inc=(k == 3))
                        s.copy(h2Tf[:, hf * 4:(hf + 1) * 4, :], ps.rearrange("p (c n) -> p c n", c=4), eng='act')
                    ps = C.ps[4]
                    for k in range(8):
                        s.mm(ps[:, 0:8], h2Tf[:, k, :], rt[:, k, :], start=(k == 0), stop=(k == 7))
                    s.copy(lg, ps[:, 0:8])
                    s.op('dve', lambda e: e.max(out=mx, in_=lg), reads=[lg], writes=[mx])
                    s.ts(nv1, mx[:, 0:1], -1.0, None, ALU.mult)
                    s.ts(msk, lg, mx[:, 1:2], None, ALU.is_ge)
                    s.act(ex, lg, AF.Exp, bias=nv1)
                    s.tt(ex, ex, msk, ALU.mult)
                    s.reduce(sm, ex, ALU.add)
                    s.op('dve', lambda e: e.reciprocal(sm, sm), reads=[sm], writes=[sm])
                    s.ts(comb[:, j, :], ex, sm, None, ALU.mult)
            nh = (ng * 128 + 511) // 512
            for ei, (w1, w3, w2) in enumerate(experts):
                s.dma(w1b, w1.rearrange("(k p) n -> p k n", p=128))
                s.dma(w3b, w3.rearrange("(k p) n -> p k n", p=128))
                s.dma(w2b, w2.rearrange("(k p) n -> p k n", p=128))
                for hf in range(nh):
                    t0, t1 = hf * 512, min(ng * 128, (hf + 1) * 512)
                    for c in range(11):
                        pa, pbk = C.ps[(2 * c) % 4], C.ps[(2 * c + 1) % 4]
                        for k in range(8):
                            s.mm(pa[:, 0:t1 - t0], w1b[:, k, c * 128:(c + 1) * 128], h2T[:, k, t0:t1], start=(k == 0), stop=(k == 7))
                        for k in range(8):
                            s.mm(pbk[:, 0:t1 - t0], w3b[:, k, c * 128:(c + 1) * 128], h2T[:, k, t0:t1], start=(k == 0), stop=(k == 7))
                        sx = sa[c % 2]
                        s.act(sx[:, 0:t1 - t0], pa[:, 0:t1 - t0], AF.Silu)
                        s.tt(actT[:, c, 0:t1 - t0], sx[:, 0:t1 - t0], pbk[:, 0:t1 - t0], ALU.mult)
                    for j in range(hf * 4, min(ng, hf * 4 + 4)):
                        jl = j - hf * 4
                        for h2_ in range(2):
                            ps = C.ps[4 + (2 * j + h2_) % 2]
                            for c in range(11):
                                s.mm(ps, actT[:, c, jl * 128:(jl + 1) * 128], w2b[:, c, h2_ * 512:(h2_ + 1) * 512], start=(c == 0), stop=(c == 10))
                            ys = Y[:, j, h2_ * 512:(h2_ + 1) * 512]
                            if moe:
                                if ei == 0:
                                    s.ts(ys, ps, comb[:, j, ei:ei + 1], None, ALU.mult)
                                else:
                                    s.stt(ys, ps, comb[:, j, ei:ei + 1], ys, ALU.mult, ALU.add)
                            else:
                                if ei == 0:
                                    s.copy(ys, ps, eng='act')
                                else:
                                    s.tt(ys, ys, ps, ALU.add)
            for j, i in enumerate(grp):
                xt = xts[0]
                ti += 1
                s.dma(xt, C.XMID[i * 128:(i + 1) * 128, :])
                s.tt(Y[:, j, :], Y[:, j, :], seg[(r, 5)], ALU.mult, eng='pool')
                s.tt(Y[:, j, :], Y[:, j, :], xt, ALU.add)
                s.dma(C.XS[i * 128:(i + 1) * 128, :], Y[:, j, :], writes=[('par', C.XS)], q='pool')


def stage_final(C):
    s, d = C.s, C.d
    with Stage(C, "final") as t:
        g = bcast_load(C, t, d['final_norm_g'], 1024, "fg")
        xts = [t.sb([128, 1024], F32, f"x{i}") for i in range(2)]
        junk = t.sb([128, 1024], BF16)
        ss = t.sb([128, 1], F32)
        rs = t.sb([128, 1], F32)
        for i in range(32):
            xt = xts[i % 2]
            s.dma(xt, C.XS[(i + 2) * 128:(i + 3) * 128, :])
            s.act(junk, xt, AF.Square, accum_out=ss)
            s.act(rs, ss, AF.Sqrt, bias=EPS, scale=1.0 / D)
            s.op('dve', lambda e: e.reciprocal(rs, rs), reads=[rs], writes=[rs])
            s.stt(xt, xt, rs, g, ALU.mult, ALU.mult)
            s.dma(C.out[i * 128:(i + 1) * 128, :], xt, writes=[('par', C.out)], q='pool')


def rope_tm(C, out, x, cos, sin, H, n, ta, tb):
    s = C.s
    xv = x.rearrange("p (h a f n) -> p h a f n", h=H, a=2, f=2)
    ov = out.rearrange("p (h a f n) -> p h a f n", h=H, a=2, f=2)
    x1, x2 = xv[:, :, :, 0, :], xv[:, :, :, 1, :]
    cb = cos.rearrange("p (a n) -> p a n", a=2).unsqueeze(1).to_broadcast([128, H, 2, n])
    sb_ = sin.rearrange("p (a n) -> p a n", a=2).unsqueeze(1).to_broadcast([128, H, 2, n])
    tav = ta[:, 0:H * 2 * n].rearrange("p (h a n) -> p h a n", h=H, a=2)
    tbv = tb[:, 0:H * 2 * n].rearrange("p (h a n) -> p h a n", h=H, a=2)
    s.tt(tav, x1, cb, ALU.mult)
    s.tt(tbv, x2, sb_, ALU.mult, eng='pool')
    s.tt(ov[:, :, :, 0, :], tav, tbv, ALU.subtract)
    s.tt(tav, x1, sb_, ALU.mult)
    s.tt(tbv, x2, cb, ALU.mult, eng='pool')
    s.tt(ov[:, :, :, 1, :], tav, tbv, ALU.add)


def kmax_bcast(C, t, KS, nkm):
    s = C.s
    m1 = t.sb([128, 1], F32)
    row = t.sb([1, 128], F32)
    v = t.sb([1, 1], F32)
    s.reduce(m1, KS, ALU.max)
    ps = C.ps[5]
    s.tr(ps[0:1, 0:128], m1, C.ident)
    s.copy(row, ps[0:1, 0:128])
    s.reduce(v, row, ALU.max)
    s.act(v, v, AF.Sqrt)
    s.ts(v, v, -1.0, None, ALU.mult)
    s.mm(ps[:, 0:1], C.ones[0:1, :], v[0:1, 0:1])
    s.copy(nkm, ps[:, 0:1])


def attend(C, OT, ncols, q_rhs, blocks, scale, PTs, cnt, tail=None):
    s = C.s
    nb = len(blocks)
    LOOK = 2
    pss = {}

    def emit_scores(bi):
        ps = C.ps[(cnt[0] + bi) % 3]
        s.mm(ps[:, 0:ncols], blocks[bi][0], q_rhs)
        pss[bi] = ps
    for bi in range(min(LOOK, nb)):
        emit_scores(bi)
    for bi, (kT, vA, mask) in enumerate(blocks):
        if bi + LOOK < nb:
            emit_scores(bi + LOOK)
        ps = pss.pop(bi)
        PT = PTs[(cnt[0] + bi) % len(PTs)]
        s.act(PT[:, 0:ncols], ps[:, 0:ncols], AF.Exp, scale=scale)
        if mask is not None:
            nrep = ncols // 128
            pv = PT[:, 0:ncols].rearrange("p (r n) -> p r n", r=nrep)
            s.tt(pv, pv, mask.unsqueeze(1).to_broadcast([128, nrep, 128]), ALU.mult, eng='pool')
        s.mm(OT[0:65, 0:ncols], vA, PT[:, 0:ncols], start=(bi == 0), stop=(bi == nb - 1 and tail is None))
    cnt[0] += nb
    if tail is not None:
        tail()


def stage_swa(C, l, ctx_out):
    nc, s, d = C.nc, C.s, C.d
    scale = 64 ** -0.5
    with Stage(C, f"swa{l}") as t:
        KT = t.sb([65, 2, TOK], BF16, "KT")
        VA = t.sb([128, NT, 2, 65], BF16, "VA")
        QT = t.sb([65, 4, TOK], BF16, "QT")
        QR = t.sb([128, NT, 4, 65], BF16, "QR")
        SSQ = t.sb([128, NT, 4], F32, "SSQ")
        KS = t.sb([128, NT], F32, "KS")
        nkm = t.sb([128, 1], F32, "nkm")
        mP = t.sb([128, 128], BF16, "mP")
        mN = t.sb([128, 128], BF16, "mN")
        mf = t.sb([128, 128], F32, "mf")
        s.dma(mf, d['maskP'])
        s.copy(mP, mf)
        s.dma(mf, d['maskN'])
        s.copy(mN, mf)
        SINK = t.sb([65, 4], F32, "SINK")
        s.dma(SINK[64:65, :], d['swa_sink'][l:l + 1, :])
        E64 = t.sb([65, 65], BF16, "E64")
        s.memset(E64, 0.0)
        s.memset(E64[64:65, 64:65], 1.0)
        es = t.sb([65, 256], BF16, "es")
        s.memset(VA[:, :, :, 64:65], 1.0)
        kaug = t.sb([128, 2, 65], BF16, "kaug")
        s.memset(kaug[:, :, 64:65], 1.0)
        pqs = [t.sb([128, 512], F32, f"pq{i}") for i in range(2)]
        cs = [t.sb([128, 32], F32, f"cos{i}") for i in range(2)]
        sn = [t.sb([128, 32], F32, f"sin{i}") for i in range(2)]
        rot = t.sb([128, 384], F32, "rot")
        ta = t.sb([128, 384], F32, "ta")
        tb = t.sb([128, 384], F32, "tb")
        ssk = t.sb([128, 6], F32, "ssk")
        for i in range(NT):
            pq = pqs[i % 2]
            r0 = prow(i * 128)
            s.dma(pq, C.P[r0:r0 + 128, 2368:2880])
            if i >= 2:
                s.dma(cs[i % 2], d['cosH'][(i - 2) * 128:(i - 1) * 128, :])
                s.dma(sn[i % 2], d['sinH'][(i - 2) * 128:(i - 1) * 128, :])
                rope_tm(C, rot, pq[:, 0:384], cs[i % 2], sn[i % 2], 6, 16, ta, tb)
                src = rot
            else:
                src = pq[:, 0:384]
            s.tt(ta, pq[:, 0:384], pq[:, 0:384], ALU.mult)
            s.reduce(ssk, ta.rearrange("p (h n) -> p h n", h=6), ALU.add)
            s.copy(SSQ[:, i, :], ssk[:, 0:4], eng='pool')
            s.reduce(KS[:, i:i + 1], ssk[:, 4:6], ALU.max)
            s.copy(QR[:, i, :, 0:64], src[:, 0:256].rearrange("p (h n) -> p h n", h=4), eng='pool')
            s.copy(kaug[:, :, 0:64], src[:, 256:384].rearrange("p (h n) -> p h n", h=2))
            s.copy(VA[:, i, :, 0:64], pq[:, 384:512].rearrange("p (h n) -> p h n", h=2), eng='pool')
            pb = C.psb[i % 2]
            for j in range(2):
                s.tr(pb[0:65, j * 128:(j + 1) * 128], kaug[:, j, :], C.identb, inc=(j == 1))
            s.copy(KT[:, :, i * 128:(i + 1) * 128], pb[0:65, 0:256].rearrange("p (j n) -> p j n", j=2), eng='act')
        kmax_bcast(C, t, KS, nkm)
        nq = t.sb([128, 4], F32, "nq")
        for i in range(NT):
            if i < 2 and not ctx_out:
                continue
            s.act(nq, SSQ[:, i, :], AF.Sqrt)
            s.ts(QR[:, i, :, 64], nq, nkm, None, ALU.mult)
            pb = C.psb[i % 2]
            for h in range(4):
                s.tr(pb[0:65, h * 128:(h + 1) * 128], QR[:, i, h, :], C.identb, inc=(h == 3))
            s.copy(QT[:, :, i * 128:(i + 1) * 128], pb[0:65, 0:512].rearrange("p (j n) -> p j n", j=4), eng='act')
        PTs = [t.sb([128, 256], BF16, f"PT{i}") for i in range(3)]
        osb = t.sb([65, 256], F32, "osb")
        rden = t.sb([128, 2], F32, "rden")
        otile = [t.sb([128, 256], F32, f"ot{i}") for i in range(2)]
        cnt = [0]
        qblocks = ([0, 1] if ctx_out else []) + list(range(2, NT))
        for qi, n in enumerate(qblocks):
            ot = otile[qi % 2]
            for j in range(2):
                if n < 2:
                    kbs = [(0, None), (1, None)]
                else:
                    kbs = [(0, None), (1, None)]
                    if n - 1 >= 2:
                        kbs.append((n - 1, mP))
                    kbs.append((n, None))
                    if n + 1 < NT:
                        kbs.append((n + 1, mN))
                blocks = [(KT[0:65, j, kb * 128:(kb + 1) * 128], VA[:, kb, j, :], m) for kb, m in kbs]
                OT = C.ps[3 + (qi * 2 + j) % 2]
                q_rhs = QT[0:65, 2 * j:2 * j + 2, n * 128:(n + 1) * 128]

                def tail(OT=OT, j=j, n=n):
                    for hh in range(2):
                        s.act(es[64:65, hh * 128:(hh + 1) * 128], QT[64:65, 2 * j + hh, n * 128:(n + 1) * 128], AF.Exp,
                              bias=SINK[64:65, 2 * j + hh:2 * j + hh + 1], scale=scale)
                    s.mm(OT[0:65, 0:256], E64[64:65, :], es[64:65, :], start=False, stop=True)
                attend(C, OT, 256, q_rhs, blocks, scale, PTs, cnt, tail)
                s.copy(osb, OT[0:65, 0:256])
                ps = C.ps[5]
                for hh in range(2):
                    s.tr(ps[:, hh * 65:(hh + 1) * 65], osb[:, hh * 128:(hh + 1) * 128], C.ident[0:65, 0:65], inc=(hh == 1))
                pv = ps[:, 0:130].rearrange("p (h n) -> p h n", h=2)
                s.op('dve', lambda e, pv=pv: e.reciprocal(rden, pv[:, :, 64]), reads=[ps], writes=[rden])
                s.tt(ot[:, j * 128:(j + 1) * 128].rearrange("p (h n) -> p h n", h=2), pv[:, :, 0:64],
                     rden.unsqueeze(2).to_broadcast([128, 2, 64]), ALU.mult)
            s.dma(C.CAT[n * 128:(n + 1) * 128, 512:768], ot, writes=[('par', C.CAT)], q='pool')


def stage_mla(C, l, ctx_out):
    nc, s, d = C.nc, C.s, C.d
    scale = 96 ** -0.5
    with Stage(C, f"mla{l}") as t:
        KT = t.sb([97, 4, TOK], BF16, "KT")
        VA = t.sb([128, NT, 4, 65], BF16, "VA")
        QT = t.sb([97, 4, TOK], BF16, "QT")
        QR = t.sb([128, NT, 4, 97], BF16, "QR")
        SSQ = t.sb([128, NT, 4], F32, "SSQ")
        KS = t.sb([128, NT], F32, "KS")
        nkm = t.sb([128, 1], F32, "nkm")
        s.memset(VA[:, :, :, 64:65], 1.0)
        kaug = t.sb([128, 4, 97], BF16, "kaug")
        s.memset(kaug[:, :, 96:97], 1.0)
        stg = t.sb([128, 512], F32, "stg")
        wq0 = t.sb([128, 384], BF16, "wq0")
        wq1 = t.sb([64, 384], BF16, "wq1")
        wkv = t.sb([128, 512], BF16, "wkv")
        s.dma(stg[:, 0:384], d['mla_wuq'][l, 0:128, :])
        s.copy(wq0, stg[:, 0:384])
        s.dma(stg[0:64, 0:384], d['mla_wuq'][l, 128:192, :])
        s.copy(wq1, stg[0:64, 0:384])
        s.dma(stg, d['mla_wukv'][l])
        s.copy(wkv, stg)
        gq = bcast_load(C, t, d['mla_qnorm_g'][l], 192, "gq")
        gkv = bcast_load(C, t, d['mla_kvnorm_g'][l], 128, "gkv")
        pms = [t.sb([128, 352], F32, f"pm{i}") for i in range(2)]
        cs = [t.sb([128, 16], F32, f"cos{i}") for i in range(2)]
        sn = [t.sb([128, 16], F32, f"sin{i}") for i in range(2)]
        junk = t.sb([128, 192], F32, "junk")
        ss2 = t.sb([128, 2], F32, "ss2")
        cn = t.sb([128, 320], BF16, "cn")
        cT = t.sb([128, 3, 128], BF16, "cT")
        qf = t.sb([128, 384], F32, "qf")
        kvf = t.sb([128, 512], F32, "kvf")
        qr_in = t.sb([128, 128], F32, "qr_in")
        qr_out = t.sb([128, 128], F32, "qr_out")
        kr_out = t.sb([128, 32], F32, "kr_out")
        ta = t.sb([128, 64], F32, "ta")
        tb = t.sb([128, 64], F32, "tb")
        sq = t.sb([128, 512], F32, "sq")
        s4 = t.sb([128, 4], F32, "s4")
        s4b = t.sb([128, 4], F32, "s4b")
        s1 = t.sb([128, 1], F32, "s1")
        for i in range(NT):
            pm = pms[i % 2]
            r0 = prow(i * 128)
            s.dma(pm, C.P[r0:r0 + 128, 2880:3232])
            s.act(junk[:, 0:192], pm[:, 0:192], AF.Square, accum_out=ss2[:, 0:1])
            s.act(junk[:, 0:128], pm[:, 192:320], AF.Square, accum_out=ss2[:, 1:2])
            s.act(ss2[:, 0:1], ss2[:, 0:1], AF.Sqrt, bias=EPS, scale=1.0 / 192)
            s.act(ss2[:, 1:2], ss2[:, 1:2], AF.Sqrt, bias=EPS, scale=1.0 / 128)
            s.op('dve', lambda e: e.reciprocal(ss2, ss2), reads=[ss2], writes=[ss2])
            s.stt(cn[:, 0:192], pm[:, 0:192], ss2[:, 0:1], gq, ALU.mult, ALU.mult)
            s.stt(cn[:, 192:320], pm[:, 192:320], ss2[:, 1:2], gkv, ALU.mult, ALU.mult)
            pb = C.psb[i % 2]
            s.tr(pb[:, 0:128], cn[:, 0:128], C.identb, inc=False)
            s.tr(pb[0:64, 128:256], cn[:, 128:192], C.identb, inc=False)
            s.tr(pb[:, 256:384], cn[:, 192:320], C.identb)
            s.copy(cT, pb[:, 0:384].rearrange("p (c n) -> p c n", c=3), eng='act')
            pq, pk = C.ps[0], C.ps[1]
            s.mm(pq[:, 0:384], cT[:, 0, :], wq0, start=True, stop=False)
            s.mm(pq[:, 0:384], cT[0:64, 1, :], wq1, start=False, stop=True)
            s.mm(pk, cT[:, 2, :], wkv)
            s.copy(qf, pq[:, 0:384], eng='act')
            s.copy(kvf, pk)
            qv = qf.rearrange("p (h n) -> p h n", h=4)
            kvv = kvf.rearrange("p (h n) -> p h n", h=4)
            s.tt(sq[:, 0:384], qf, qf, ALU.mult, eng='pool')
            s.reduce(SSQ[:, i, :], sq[:, 0:384].rearrange("p (h n) -> p h n", h=4), ALU.add)
            s.tt(sq, kvf, kvf, ALU.mult, eng='pool')
            s.reduce(s4, sq.rearrange("p (h n) -> p h n", h=4)[:, :, 0:64], ALU.add)
            s.tt(ta[:, 0:32], pm[:, 320:352], pm[:, 320:352], ALU.mult)
            s.reduce(s1, ta[:, 0:32], ALU.add)
            s.ts(s4b, s4, s1, None, ALU.add)
            s.reduce(KS[:, i:i + 1], s4b, ALU.max)
            if i >= 2:
                s.dma(cs[i % 2], d['cosM'][(i - 2) * 128:(i - 1) * 128, :])
                s.dma(sn[i % 2], d['sinM'][(i - 2) * 128:(i - 1) * 128, :])
                s.copy(qr_in.rearrange("p (h n) -> p h n", h=4), qv[:, :, 64:96], eng='pool')
                rope_tm(C, qr_out, qr_in, cs[i % 2], sn[i % 2], 4, 8, ta, tb)
                rope_tm(C, kr_out, pm[:, 320:352], cs[i % 2], sn[i % 2], 1, 8, ta, tb)
                qr_src = qr_out.rearrange("p (h n) -> p h n", h=4)
                kr_src = kr_out
            else:
                qr_src = qv[:, :, 64:96]
                kr_src = pm[:, 320:352]
            s.copy(QR[:, i, :, 0:64], qv[:, :, 0:64], eng='pool')
            s.copy(QR[:, i, :, 64:96], qr_src, eng='pool')
            s.copy(kaug[:, :, 0:64], kvv[:, :, 0:64])
            s.copy(kaug[:, :, 64:96], kr_src.unsqueeze(1).to_broadcast([128, 4, 32]))
            s.copy(VA[:, i, :, 0:64], kvv[:, :, 64:128], eng='pool')
            pb = C.psb[(i + 1) % 2]
            for h in range(4):
                s.tr(pb[0:97, h * 128:(h + 1) * 128], kaug[:, h, :], C.identb, inc=(h == 3))
            s.copy(KT[:, :, i * 128:(i + 1) * 128], pb[0:97, 0:512].rearrange("p (j n) -> p j n", j=4), eng='act')
        kmax_bcast(C, t, KS, nkm)
        nq = t.sb([128, 4], F32, "nq")
        for i in range(NT):
            if i < 2 and not ctx_out:
                continue
            s.act(nq, SSQ[:, i, :], AF.Sqrt)
            s.ts(QR[:, i, :, 96], nq, nkm, None, ALU.mult)
            pb = C.psb[i % 2]
            for h in range(4):
                s.tr(pb[0:97, h * 128:(h + 1) * 128], QR[:, i, h, :], C.identb, inc=(h == 3))
            s.copy(QT[:, :, i * 128:(i + 1) * 128], pb[0:97, 0:512].rearrange("p (j n) -> p j n", j=4), eng='act')
        PTs = [t.sb([128, 512], BF16, f"PT{i}") for i in range(3)]
        osb = t.sb([65, 512], F32, "osb")
        rden = t.sb([128, 4], F32, "rden")
        otile = [t.sb([128, 4, 256], F32, f"ot{i}") for i in range(2)]
        cnt = [0]
        qtiles = ([(0, 256)] if ctx_out else []) + [(256 + q * 512, 512) for q in range(8)]
        for qi, (q0, qn) in enumerate(qtiles):
            ot = otile[qi % 2]
            kbs = [0, 1] if q0 < 256 else list(range(NT))
            for h in range(4):
                blocks = [(KT[0:97, h, kb * 128:(kb + 1) * 128], VA[:, kb, h, :], None) for kb in kbs]
                OT = C.ps[3 + (qi * 4 + h) % 2]
                attend(C, OT, qn, QT[0:97, h, q0:q0 + qn], blocks, scale, PTs, cnt)
                s.copy(osb[:, 0:qn], OT[0:65, 0:qn])
                ps = C.ps[5]
                nsub = qn // 128
                for sb_ in range(nsub):
                    s.tr(ps[:, sb_ * 65:(sb_ + 1) * 65], osb[:, sb_ * 128:(sb_ + 1) * 128], C.ident[0:65, 0:65], inc=(sb_ == nsub - 1))
                pv = ps[:, 0:nsub * 65].rearrange("p (h n) -> p h n", h=nsub)
                s.op('dve', lambda e, pv=pv, nsub=nsub: e.reciprocal(rden[:, 0:nsub], pv[:, :, 64]), reads=[ps], writes=[rden])
                s.tt(ot[:, 0:nsub, h * 64:(h + 1) * 64], pv[:, :, 0:64],
                     rden[:, 0:nsub].unsqueeze(2).to_broadcast([128, nsub, 64]), ALU.mult)
            for sb_ in range(qn // 128):
                s.dma(C.CAT[q0 + sb_ * 128:q0 + (sb_ + 1) * 128, 768:1024], ot[:, sb_, :], writes=[('par', C.CAT)], q='pool')


def rope_consts():
    out = {}
    row = np.repeat(np.arange(64, dtype=np.float32), 64)
    col = np.tile(np.arange(64, dtype=np.float32), 64)
    for nm, rot in (('H', 64), ('M', 32)):
        nf = rot // 4
        inv = (np.float32(10000.0) ** (-np.arange(nf, dtype=np.float32) / np.float32(nf))).astype(np.float32)
        ang = np.stack([row[:, None] * inv, col[:, None] * inv], axis=1).astype(np.float32)
        out['cos' + nm] = np.cos(ang).reshape(4096, 2 * nf).astype(np.float32)
        out['sin' + nm] = np.sin(ang).reshape(4096, 2 * nf).astype(np.float32)
    j = np.arange(128)[:, None]
    i = np.arange(128)[None, :]
    out['maskP'] = (j >= i).astype(np.float32)
    out['maskN'] = (j <= i).astype(np.float32)
    return out


HGC = dict(r=0, kf=256, kb=512, v=768, lwf=1024, lwb=1280, n=1536)
RWC = dict(r=0, kf=256, kb=256, v=512, lwf=768, lwb=1024, kk=1280, b=1536, g=1792, bonus=2048, n=2304)
NEG_EXP_HALF = -0.6065306597126334


def stage_hg_prep(C, l):
    s, d = C.s, C.d
    with Stage(C, f"hgp{l}") as t:
        LB = t.sb([128, 512], F32, "LB")
        OML = t.sb([128, 512], F32, "OML")
        if l == 0:
            s.memset(LB, 0.0)
        else:
            a0 = bcast_load(C, t, d['hg_lb'][0].rearrange("a n -> (a n)"), 512, "a0")
            a1 = bcast_load(C, t, d['hg_lb'][1].rearrange("a n -> (a n)"), 512, "a1")
            s.tt(a1, a1, a0, ALU.subtract)
            s.act(LB, a1, AF.Sigmoid)
        s.ts(OML, LB, -1.0, 1.0, ALU.mult, ALU.add)
        pzs = [t.sb([128, 1024], F32, f"pz{i}") for i in range(2)]
        scs = [t.sb([128, HGC['n']], F32, f"sc{i}") for i in range(2)]
        sg = t.sb([128, 512], F32, "sg")
        for i in range(NT):
            pz, sc = pzs[i % 2], scs[i % 2]
            r0 = prow(i * 128)
            s.dma(pz, C.P[r0:r0 + 128, 0:1024])
            s.act(sc[:, 0:256], pz[:, 0:256], AF.Silu)
            s.copy(sc[:, 768:1024], pz[:, 768:1024], eng='pool')
            s.act(sg, pz[:, 256:768], AF.Sigmoid)
            s.tt(sg, sg, OML, ALU.mult)
            s.tt(sg, sg, LB, ALU.add, eng='pool')
            s.ts(sg, sg, 1e-30, None, ALU.max)
            s.ts(sc[:, 256:768], sg, -1.0, 1.0, ALU.mult, ALU.add, eng='pool')
            s.act(sc[:, 1024:1536], sg, AF.Ln)
            s.dma(C.SCH[i * 128:(i + 1) * 128, :], sc, writes=[('par', C.SCH)], q='pool')


def stage_rw_prep(C, l):
    s, d = C.s, C.d
    with Stage(C, f"rwp{l}") as t:
        MU0 = bcast_load(C, t, d['rw_mu'][l, 0], 1088, "mu0")
        MU1 = bcast_load(C, t, d['rw_mu'][l, 1], 1088, "mu1")
        W0 = bcast_load(C, t, d['rw_w0'][l].rearrange("a n -> (a n)"), 512, "w0")
        A0 = bcast_load(C, t, d['rw_a0'][l], 256, "a0")
        KKb = bcast_load(C, t, d['rw_k_k'][l], 256, "kkb")
        KA = bcast_load(C, t, d['rw_k_a'][l], 256, "ka")
        RK = bcast_load(C, t, d['rw_r_k'][l].rearrange("a n -> (a n)"), 256, "rk")
        OMKA = t.sb([128, 256], F32, "omka")
        s.ts(OMKA, KA, -1.0, 1.0, ALU.mult, ALU.add)
        W2 = t.sb([128, 256], F32, "W2")
        s.dma(W2, d['rw_w2'][l].rearrange("a k n -> (a k) n"))
        A2 = t.sb([64, 256], F32, "A2")
        s.dma(A2, d['rw_a2'][l])
        G2 = t.sb([128, 256], F32, "G2")
        s.dma(G2, d['rw_g2'][l])
        p0s = [t.sb([128, 1088], F32, f"p0{i}") for i in range(2)]
        pms = [t.sb([128, 1088], F32, f"pm{i}") for i in range(2)]
        pns = [t.sb([128, 1088], F32, f"pn{i}") for i in range(2)]
        scs = [t.sb([128, RWC['n']], F32, f"sc{i}") for i in range(2)]
        xs = t.sb([128, 1088], F32, "xs")
        thT = t.sb([128, 128], F32, "thT")
        yaT = t.sb([64, 128], F32, "yaT")
        sgT = t.sb([128, 128], F32, "sgT")
        a = t.sb([128, 256], F32, "a")
        tq = t.sb([128, 256], F32, "tq")
        tq2 = t.sb([128, 256], F32, "tq2")
        r4 = t.sb([128, 4], F32, "r4")
        for i in range(NT):
            p0, pm, pn, sc = p0s[i % 2], pms[i % 2], pns[i % 2], scs[i % 2]
            r0 = prow(i * 128)
            s.dma(p0, C.P[r0:r0 + 128, 1280:2368])
            s.dma(pm, C.P[r0 - 1:r0 + 127, 1280:2368])
            s.dma(pn, C.P[r0 + 1:r0 + 129, 1280:2368])
            s.tt(pm, pm, p0, ALU.subtract)
            s.tt(pn, pn, p0, ALU.subtract, eng='pool')
            s.tt(pm, pm, MU0, ALU.mult)
            s.tt(pn, pn, MU1, ALU.mult, eng='pool')
            s.tt(xs, p0, pm, ALU.add)
            s.tt(xs, xs, pn, ALU.add)
            ps = C.ps[0]
            s.tr(ps[:, 0:128], xs[:, 768:896], C.ident)
            s.act(thT, ps[:, 0:128], AF.Tanh)
            ps = C.ps[1]
            s.tr(ps[0:64, 0:128], xs[:, 896:960], C.ident)
            s.copy(yaT, ps[0:64, 0:128])
            ps = C.ps[2]
            s.tr(ps[:, 0:128], xs[:, 960:1088], C.ident)
            s.act(sgT, ps[:, 0:128], AF.Sigmoid)
            pw = C.ps[3]
            pw2 = C.ps[5]
            s.mm(pw[:, 0:256], thT[0:64, :], W2[0:64, :])
            s.mm(pw2[:, 0:256], thT[64:128, :], W2[64:128, :])
            pa = C.ps[4]
            s.mm(pa[:, 0:256], yaT, A2)
            s.mm(pa[:, 256:512], sgT, G2)
            lw = sc[:, RWC['lwf']:RWC['lwf'] + 512]
            s.tt(lw[:, 0:256], pw[:, 0:256], W0[:, 0:256], ALU.add)
            s.tt(lw[:, 256:512], pw2[:, 0:256], W0[:, 256:512], ALU.add)
            s.act(lw, lw, AF.Sigmoid)
            s.ts(lw, lw, NEG_EXP_HALF, None, ALU.mult, eng='pool')
            s.tt(a, pa[:, 0:256], A0, ALU.add)
            s.act(a, a, AF.Sigmoid)
            s.copy(sc[:, RWC['g']:RWC['g'] + 256], pa[:, 256:512], eng='act')
            s.copy(sc[:, 0:256], xs[:, 0:256], eng='pool')
            s.copy(sc[:, 512:768], xs[:, 512:768], eng='pool')
            kk = sc[:, RWC['kk']:RWC['kk'] + 256]
            s.tt(kk, xs[:, 256:512], KKb, ALU.mult)
            s.tt(tq, kk, kk, ALU.mult)
            s.reduce(r4, tq.rearrange("p (h n) -> p h n", h=4), ALU.add)
            s.act(r4, r4, AF.Sqrt)
            s.ts(r4, r4, 1e-12, None, ALU.max)
            s.op('dve', lambda e: e.reciprocal(r4, r4), reads=[r4], writes=[r4])
            kkv = kk.rearrange("p (h n) -> p h n", h=4)
            s.tt(kkv, kkv, r4.unsqueeze(2).to_broadcast([128, 4, 64]), ALU.mult)
            s.tt(tq, a, KA, ALU.mult, eng='pool')
            s.tt(tq, tq, OMKA, ALU.add, eng='pool')
            s.tt(sc[:, 256:512], xs[:, 256:512], tq, ALU.mult)
            s.tt(sc[:, RWC['b']:RWC['b'] + 256], kk, a, ALU.mult, eng='pool')
            s.tt(tq2, xs[:, 0:256], sc[:, 256:512], ALU.mult)
            s.tt(tq2, tq2, RK, ALU.mult)
            s.reduce(r4, tq2.rearrange("p (h n) -> p h n", h=4), ALU.add)
            s.tt(sc[:, RWC['bonus']:RWC['bonus'] + 256].rearrange("p (h n) -> p h n", h=4),
                 xs[:, 512:768].rearrange("p (h n) -> p h n", h=4), r4.unsqueeze(2).to_broadcast([128, 4, 64]), ALU.mult)
            s.dma(C.SCR[i * 128:(i + 1) * 128, :], sc, writes=[('par', C.SCR)], q='pool')


def scan_consts():
    out = np.zeros((2, 64, 450), np.float32)
    sidx = np.arange(64)[:, None]
    tidx = np.arange(64)[None, :]
    for dd in range(2):
        if dd == 0:
            tri = (sidx <= tidx)
            tris = (sidx < tidx)
            mid = 31
        else:
            tri = (sidx >= tidx)
            tris = (sidx > tidx)
            mid = 32
        tri = tri.astype(np.float32)
        tris = tris.astype(np.float32)
        cm = tri[:, mid:mid + 1]
        out[dd, :, 0:64] = tri - cm
        out[dd, :, 64:128] = tris - cm
        out[dd, :, 128] = cm[:, 0]
        out[dd, :, 129] = 1.0
        out[dd, :, 130:194] = tris.T
        out[dd, :, 194:258] = tris
        out[dd, :, 258:322] = tri
        out[dd, :, 322:386] = tris
        out[dd, :, 386:450] = tris.T
    bd = np.zeros((128, 128), np.float32)
    bd[:64, :64] = 1.0
    bd[64:, 64:] = 1.0
    return dict(scn=out, bdm=bd)


def hq(h):
    return 2 * (h % 2) + h // 2


def scan_gen(C, t, tag, banks, SC, cols, OUTD, delta):
    s, d = C.s, C.d
    nsteps = TOK // 64
    if True:
        K_ = t.sb([64, 2, 450], F32, "scn" + tag)
        s.dma(K_, d['scn'].rearrange("a s n -> s a n"))
        BD = t.sb([128, 128], F32, "BD" + tag)
        s.dma(BD, d['bdm'])
        I4 = t.sb([64, 4, 64], BF16, "I4" + tag)
        for h in range(4):
            s.copy(I4[:, h, :], C.ident[0:64, 0:64])
        H2 = [[t.sb([128, 128], F32, f"H{tag}{dd}{hp}") for hp in range(2)] for dd in range(2)]
        for dd in range(2):
            for hp in range(2):
                s.memset(H2[dd][hp], 0.0)
        bank = [0]

        def nb():
            bank[0] = (bank[0] + 1) % len(banks)
            return banks[bank[0]]

        def mk(name, shape, n=2, dt=F32):
            return [[t.sb(shape, dt, f"{name}{tag}{dd}{k}") for k in range(n)] for dd in range(2)]
        names_tok = ['LW', 'R', 'K', 'V'] + (['KK', 'B'] if delta else [])
        T = {nm: mk(nm, [64, 256]) for nm in names_tok}
        eP = mk('eP', [128, 2, 128])
        eM = mk('eM', [128, 2, 64])
        eS = mk('eS', [128, 2, 2])
        eT = mk('eT', [64, 512])
        fm = {nm: mk(nm, [128, 2, 64], dt=(F32 if nm == 'rTin' else BF16))
              for nm in (['rTm', 'kTm', 'rTin'] + (['kkTm', 'bTm'] if delta else []))}
        ePin = mk('ePin', [128, 2, 64])
        Vb = mk('Vb', [64, 256], dt=BF16)
        kout = mk('kout', [64, 256])
        ArkT = mk('ArkT', [64, 4, 64], dt=BF16)
        for dd in range(2):
            for k_ in range(2):
                s.memset(ArkT[dd][k_], 0.0)
        Osb = mk('Osb', [64, 256])
        tmpH = mk('tmpH', [128, 128], 1)
        if delta:
            bout = mk('bout', [64, 256], dt=BF16)
            kkin = mk('kkin', [64, 256], dt=BF16)
            ArbT = mk('ArbT', [64, 4, 64], dt=BF16)
            AkkT = mk('AkkT', [64, 4, 64], dt=BF16)
            Xq = mk('Xq', [64, 4, 64], 2, dt=BF16)
            Xt = mk('Xt', [64, 4, 64], 2, dt=BF16)
            Pm = mk('Pm', [64, 4, 64], 2, dt=BF16)
            X0 = mk('X0', [64, 4, 64], 1, dt=BF16)
            U0 = mk('U0', [64, 256])
            KtT = mk('KtT', [128, 2, 64])
            U = mk('U', [64, 256], 1, dt=BF16)

        def chunk_of(dd, i):
            if dd == 0:
                return i
            return 3 - i if i < 4 else 71 - i

        for i in range(nsteps):
            b = i % 2
            cks = [chunk_of(0, i), chunk_of(1, i)]
            DD = (0, 1)
            for dd in DD:
                r0 = cks[dd] * 64
                lwc = cols['lwf'] if dd == 0 else cols['lwb']
                kc = cols['kf'] if dd == 0 else cols['kb']
                s.dma(T['LW'][dd][b], SC[r0:r0 + 64, lwc:lwc + 256])
                s.dma(T['R'][dd][b], SC[r0:r0 + 64, cols['r']:cols['r'] + 256])
                s.dma(T['K'][dd][b], SC[r0:r0 + 64, kc:kc + 256])
                s.dma(T['V'][dd][b], SC[r0:r0 + 64, cols['v']:cols['v'] + 256])
                if delta:
                    s.dma(T['KK'][dd][b], SC[r0:r0 + 64, cols['kk']:cols['kk'] + 256])
                    s.dma(T['B'][dd][b], SC[r0:r0 + 64, cols['b']:cols['b'] + 256])
            yield
            for dd in DD:
                LW = T['LW'][dd][b]
                pE = nb()
                for hp in range(2):
                    s.mm(pE[:, hp * 130:(hp + 1) * 130], LW[:, hp * 128:(hp + 1) * 128], K_[:, dd, 0:130], inc=(hp == 1))
                pEv = pE[:, 0:260].rearrange("p (h n) -> p h n", h=2)
                s.act(eP[dd][b], pEv[:, :, 0:128], AF.Exp)
                s.act(eM[dd][b], pEv[:, :, 0:64], AF.Exp, scale=-1.0)
                s.act(eS[dd][b], pEv[:, :, 128:130], AF.Exp)
                pT = nb()
                s.mm(pT[0:64, 0:256], K_[:, dd, 130:194], LW, inc=False)
                s.mm(pT[0:64, 256:512], K_[:, dd, 194:258], LW)
                s.act(eT[dd][b], pT[0:64, :], AF.Exp)
            yield
            for dd in DD:
                pR = nb()
                srcs = ['R', 'K'] + (['KK', 'B'] if delta else [])
                for si, nm in enumerate(srcs):
                    for hp in range(2):
                        last = (si == len(srcs) - 1 and hp == 1)
                        s.tr(pR[:, (si * 2 + hp) * 64:(si * 2 + hp + 1) * 64], T[nm][dd][b][:, hp * 128:(hp + 1) * 128],
                             C.ident[0:64, 0:64], inc=last)
                pv = pR.rearrange("p (a h n) -> p a h n", a=4, h=2)
                s.tt(fm['rTm'][dd][b], pv[:, 0], eP[dd][b][:, :, 0:64], ALU.mult)
                s.tt(fm['kTm'][dd][b], pv[:, 1], eM[dd][b], ALU.mult)
                if delta:
                    s.tt(fm['kkTm'][dd][b], pv[:, 2], eP[dd][b][:, :, 64:128], ALU.mult)
                    s.tt(fm['bTm'][dd][b], pv[:, 3], eM[dd][b], ALU.mult)
                s.tt(ePin[dd][b], eP[dd][b][:, :, 0:64], eS[dd][b][:, :, 0:1].to_broadcast([128, 2, 64]), ALU.mult)
                s.tt(fm['rTin'][dd][b], pv[:, 0], ePin[dd][b], ALU.mult)
                s.copy(Vb[dd][b], T['V'][dd][b], eng='pool')
                s.tt(kout[dd][b], T['K'][dd][b], eT[dd][b][:, 0:256], ALU.mult, eng='pool')
                if delta:
                    s.tt(bout[dd][b], T['B'][dd][b], eT[dd][b][:, 0:256], ALU.mult, eng='pool')
                    s.tt(kkin[dd][b], T['KK'][dd][b], eT[dd][b][:, 256:512], ALU.mult, eng='pool')
            yield
            for dd in DD:
                mInc = K_[:, dd, 258:322].unsqueeze(1).to_broadcast([64, 4, 64])
                mStr = K_[:, dd, 322:386].unsqueeze(1).to_broadcast([64, 4, 64])
                mStrT = K_[:, dd, 386:450].unsqueeze(1).to_broadcast([64, 4, 64])

                def amat(dst, lname, rname, mask, eng='dve', quad=False):
                    pX, pY = nb(), nb()
                    for hp in range(2):
                        for par, pA in ((0, pX), (1, pY)):
                            pr = 64 * par
                            L = fm[lname][dd][b][pr:pr + 64, hp, :]
                            R_ = fm[rname][dd][b][pr:pr + 64, hp, :]
                            o_ = pA[0:64, hp * 64:(hp + 1) * 64]
                            if not quad:
                                s.mm(o_, L, R_, inc=(hp == 1))
                            elif dd == 0:
                                s.mm(o_[0:32, :], L[:, 0:32], R_, inc=False)
                                s.mm(o_[32:64, 32:64], L[:, 32:64], R_[:, 32:64], inc=(hp == 1))
                            else:
                                s.mm(o_[32:64, :], L[:, 32:64], R_, inc=False)
                                s.mm(o_[0:32, 0:32], L[:, 0:32], R_[:, 0:32], inc=(hp == 1))
                    for par, pA in ((0, pX), (1, pY)):
                        dv = dst[:, 2 * par:2 * par + 2, :]
                        pv_ = pA[0:64, 0:128].rearrange("p (h n) -> p h n", h=2)
                        mk_ = mask[:, 0:2, :]
                        if not quad:
                            s.tt(dv, pv_, mk_, ALU.mult, eng=eng)
                        elif dd == 0:
                            s.tt(dv[0:32], pv_[0:32], mk_[0:32], ALU.mult, eng=eng)
                            s.tt(dv[32:64, :, 32:64], pv_[32:64, :, 32:64], mk_[32:64, :, 32:64], ALU.mult, eng=eng)
                        else:
                            s.tt(dv[32:64], pv_[32:64], mk_[32:64], ALU.mult, eng=eng)
                            s.tt(dv[0:32, :, 0:32], pv_[0:32, :, 0:32], mk_[0:32, :, 0:32], ALU.mult, eng=eng)
                amat(ArkT[dd][b], 'kTm', 'rTm', mInc, quad=not delta)
                if delta:
                    amat(ArbT[dd][b], 'bTm', 'rTm', mInc)
                    amat(AkkT[dd][b], 'kTm', 'kkTm', mStr)
                    amat(Xq[dd][0], 'bTm', 'kkTm', mStr)
                    amat(Xt[dd][0], 'kkTm', 'bTm', mStrT)
            if delta:
                for dd in DD:
                    s.tt(Pm[dd][0], I4, Xq[dd][0], ALU.subtract, eng='pool')
                for j in range(5):
                    yield
                    a_, b_ = j % 2, (j + 1) % 2
                    for dd in DD:
                        pQ, pQ2 = nb(), nb()
                        for h in range(4):
                            s.mm(pQ[0:64, h * 64:(h + 1) * 64], Xt[dd][a_][:, h, :], Xq[dd][a_][:, h, :], inc=(h == 3))
                        for h in range(4):
                            s.mm(pQ2[0:64, h * 64:(h + 1) * 64], Xq[dd][a_][:, h, :], Xt[dd][a_][:, h, :], inc=(h == 3))
                        s.copy(Xq[dd][b_], pQ[0:64, 0:256].rearrange("p (h n) -> p h n", h=4), eng='act')
                        s.copy(Xt[dd][b_], pQ2[0:64, 0:256].rearrange("p (h n) -> p h n", h=4))
                    for dd in DD:
                        pP = nb()
                        for h in range(4):
                            s.mm(pP[0:64, h * 64:(h + 1) * 64], Xt[dd][b_][:, h, :], Pm[dd][a_][:, h, :], inc=(h == 3))
                        s.tt(Pm[dd][b_], Pm[dd][a_], pP[0:64, 0:256].rearrange("p (h n) -> p h n", h=4), ALU.add)
                MT = [Pm[dd][1] for dd in DD]
                yield
                for dd in DD:
                    pX = nb()
                    for h in range(4):
                        s.mm(pX[0:64, h * 64:(h + 1) * 64], AkkT[dd][b][:, hq(h), :], Vb[dd][b][:, h * 64:(h + 1) * 64], inc=(h == 3))
                    s.copy(X0[dd][0], pX[0:64, 0:256].rearrange("p (h n) -> p h n", h=4), eng='act')
                    pK0, pK1 = nb(), nb()
                    for hp in range(2):
                        for par, pK in ((0, pK0), (1, pK1)):
                            h = 2 * hp + par
                            pr = 64 * par
                            s.mm(pK[pr:pr + 64, hp * 64:(hp + 1) * 64], kkin[dd][b][:, h * 64:(h + 1) * 64], MT[dd][:, hq(h), :], inc=(hp == 1))
                    s.copy(KtT[dd][b][0:64], pK0[0:64, 0:128].rearrange("p (h n) -> p h n", h=2))
                    s.copy(KtT[dd][b][64:128], pK1[64:128, 0:128].rearrange("p (h n) -> p h n", h=2), eng='act')
                for dd in DD:
                    pU = nb()
                    for h in range(4):
                        s.mm(pU[0:64, h * 64:(h + 1) * 64], MT[dd][:, hq(h), :], X0[dd][0][:, h, :], inc=(h == 3))
                    s.ts(U0[dd][b], pU[0:64, 0:256], -1.0, None, ALU.mult)
            yield
            if delta:
                for dd in DD:
                    pU = nb()
                    for hp in range(2):
                        s.mm(pU[0:64, hp * 128:(hp + 1) * 128], KtT[dd][b][:, hp, :], H2[dd][hp], inc=(hp == 1))
                    s.tt(U[dd][0], U0[dd][b], pU[0:64, 0:256], ALU.subtract)
            yield
            for dd in DD:
                pO = nb()
                for hp in range(2):
                    s.mm(pO[0:64, hp * 128:(hp + 1) * 128], fm['rTin'][dd][b][:, hp, :], H2[dd][hp], start=(hp == 0), stop=False)
                for h in range(4):
                    last = (h == 3) and not delta
                    s.mm(pO[0:64, h * 64:(h + 1) * 64], ArkT[dd][b][:, hq(h), :], Vb[dd][b][:, h * 64:(h + 1) * 64],
                         start=False, stop=last, inc=last)
                if delta:
                    for h in range(4):
                        s.mm(pO[0:64, h * 64:(h + 1) * 64], ArbT[dd][b][:, hq(h), :], U[dd][0][:, h * 64:(h + 1) * 64],
                             start=False, stop=(h == 3), inc=(h == 3))
                s.copy(Osb[dd][b], pO[0:64, 0:256], eng='act')
                r0 = cks[dd] * 64
                s.dma(OUTD[dd][r0:r0 + 64, :], Osb[dd][b], writes=[('par', OUTD[dd])], q='pool')
            yield
            for dd in DD:
                pH = nb()
                for hp in range(2):
                    cs_ = slice(hp * 128, (hp + 1) * 128)
                    s.mm(pH[:, cs_], kout[dd][b][:, cs_], T['V'][dd][b][:, cs_], start=True, stop=not delta, inc=(hp == 1 and not delta))
                    if delta:
                        s.mm(pH[:, cs_], bout[dd][b][:, cs_], U[dd][0][:, cs_], start=False, stop=True, inc=(hp == 1))
                for hp in range(2):
                    s.tt(tmpH[dd][0], pH[:, hp * 128:(hp + 1) * 128], BD, ALU.mult)
                    s.stt(H2[dd][hp], H2[dd][hp], eS[dd][b][:, hp, 1:2], tmpH[dd][0], ALU.mult, ALU.add)
            yield


def stage_scans(C, l):
    with Stage(C, f"scans{l}") as t:
        banks_r = [C.ps[0], C.ps[1], C.ps[2], C.ps[3]]
        banks_h = [C.ps[4], C.ps[5], C.psb[0].bitcast(F32), C.psb[1].bitcast(F32)]
        gens = [scan_gen(C, t, 'r', banks_r, C.SCR, RWC, C.ORW, True),
                scan_gen(C, t, 'h', banks_h, C.SCH, HGC, C.OHG, False)]
        if l == 0:
            gens.append(precast_gen(C, t))
        live = list(gens)
        rnd = 0
        while live:
            rnd += 1
            for gi, g in enumerate(list(live)):
                if g is gens[-1] and len(gens) == 3 and len(live) > 1 and rnd % 6 != 0:
                    continue
                try:
                    next(g)
                except StopIteration:
                    live.remove(g)


def stage_hg_read(C, l):
    s, d = C.s, C.d
    with Stage(C, f"hgr{l}") as t:
        NG = bcast_load(C, t, d['hg_norm_g'][l], 256, "ng")
        ofs = [t.sb([128, 256], F32, f"of{i}") for i in range(2)]
        obs = [t.sb([128, 256], F32, f"ob{i}") for i in range(2)]
        gs = [t.sb([128, 256], F32, f"g{i}") for i in range(2)]
        sq = t.sb([128, 256], F32, "sq")
        r4 = t.sb([128, 4], F32, "r4")
        for i in range(NT):
            of, ob, g = ofs[i % 2], obs[i % 2], gs[i % 2]
            r0 = prow(i * 128)
            s.dma(of, C.OHG[0][i * 128:(i + 1) * 128, :])
            s.dma(ob, C.OHG[1][i * 128:(i + 1) * 128, :])
            s.dma(g, C.P[r0:r0 + 128, 1024:1280])
            s.tt(of, of, ob, ALU.add)
            s.tt(sq, of, of, ALU.mult, eng='pool')
            s.reduce(r4, sq.rearrange("p (h n) -> p h n", h=4), ALU.add)
            s.act(r4, r4, AF.Sqrt, bias=EPS, scale=1.0 / 64)
            s.op('dve', lambda e: e.reciprocal(r4, r4), reads=[r4], writes=[r4])
            ov = of.rearrange("p (h n) -> p h n", h=4)
            s.tt(ov, ov, r4.unsqueeze(2).to_broadcast([128, 4, 64]), ALU.mult)
            s.act(g, g, AF.Silu)
            s.tt(of, of, NG, ALU.mult, eng='pool')
            s.tt(of, of, g, ALU.mult)
            s.dma(C.CAT[i * 128:(i + 1) * 128, 0:256], of, writes=[('par', C.CAT)], q='pool')


def stage_rw_read(C, l):
    s, d = C.s, C.d
    with Stage(C, f"rwr{l}") as t:
        LG = bcast_load(C, t, d['rw_ln_g'][l], 256, "lg")
        LB_ = bcast_load(C, t, d['rw_ln_b'][l], 256, "lb")
        ofs = [t.sb([128, 256], F32, f"of{i}") for i in range(2)]
        obs = [t.sb([128, 256], F32, f"ob{i}") for i in range(2)]
        gbs = [t.sb([128, 512], F32, f"gb{i}") for i in range(2)]
        sq = t.sb([128, 256], F32, "sq")
        r4 = t.sb([128, 4], F32, "r4")
        for i in range(NT):
            of, ob, gb = ofs[i % 2], obs[i % 2], gbs[i % 2]
            s.dma(of, C.ORW[0][i * 128:(i + 1) * 128, :])
            s.dma(ob, C.ORW[1][i * 128:(i + 1) * 128, :])
            s.dma(gb, C.SCR[i * 128:(i + 1) * 128, RWC['g']:RWC['g'] + 512])
            s.tt(of, of, ob, ALU.add)
            ov = of.rearrange("p (h n) -> p h n", h=4)
            s.reduce(r4, ov, ALU.add)
            s.ts(r4, r4, 1.0 / 64, None, ALU.mult)
            s.tt(ov, ov, r4.unsqueeze(2).to_broadcast([128, 4, 64]), ALU.subtract)
            s.tt(sq, of, of, ALU.mult, eng='pool')
            s.reduce(r4, sq.rearrange("p (h n) -> p h n", h=4), ALU.add)
            s.act(r4, r4, AF.Sqrt, bias=64e-5, scale=1.0 / 64)
            s.op('dve', lambda e: e.reciprocal(r4, r4), reads=[r4], writes=[r4])
            s.tt(ov, ov, r4.unsqueeze(2).to_broadcast([128, 4, 64]), ALU.mult)
            s.tt(of, of, LG, ALU.mult, eng='pool')
            s.tt(of, of, LB_, ALU.add)
            s.tt(of, of, gb[:, 256:512], ALU.add, eng='pool')
            s.tt(of, of, gb[:, 0:256], ALU.mult)
            s.dma(C.CAT[i * 128:(i + 1) * 128, 256:512], of, writes=[('par', C.CAT)], q='pool')


_NC_CACHE = {}


def kernel(**inputs):
    ncores = 4
    if 'nc' not in _NC_CACHE:
        _NC_CACHE['nc'] = build()
    nc = _NC_CACHE['nc']
    in_maps = [host_inputs(inputs, b) for b in range(ncores)]
    res = run_bass_kernel_spmd(nc, in_maps, core_ids=list(range(ncores)))
    out = np.stack([np.asarray(res.results[b]['out'], dtype=np.float32) for b in range(4)], axis=0)
    return out
```

```python
import numpy as np
import concourse.bass as bass
import concourse.mybir as mybir
from concourse.bass_utils import run_bass_kernel_spmd

F32 = mybir.dt.float32
BF16 = mybir.dt.bfloat16
ALU = mybir.AluOpType
AF = mybir.ActivationFunctionType
AX = mybir.AxisListType


class Sched:
    NDMA = 40
    ROT = 20000

    def __init__(self, nc):
        self.nc = nc
        self.e = dict(pe=nc.tensor, act=nc.scalar, dve=nc.vector, pool=nc.gpsimd, sp=nc.sync)
        self.sem = {}
        self.cnt = {}
        self.nsem = {}
        for k in ('pe', 'act', 'dve', 'pool'):
            self.nsem[k] = 0
            self.sem[k] = nc.alloc_semaphore(f"s_{k}_0")
            self.cnt[k] = 0
        self.seen = {k: {} for k in self.e}
        self.dsem = [nc.alloc_semaphore(f"sd{i}") for i in range(self.NDMA)]
        self.dcnt = [0] * self.NDMA
        self.drr = 0
        self.res = {}
        self.ninst = 0
        self.out_evs = {}

    @staticmethod
    def _merge(dst, src):
        for n, (h, v) in src.items():
            if n not in dst or dst[n][1] < v:
                dst[n] = (h, v)

    def _st(self, key):
        s = self.res.get(key)
        if s is None:
            s = dict(w={}, r={}, old={}, phase='w')
            self.res[key] = s
        return s

    def _key(self, x):
        if isinstance(x, str):
            return x, False
        if isinstance(x, tuple):
            return self._key(x[1])[0], True
        return x.name, False

    def _need(self, reads, writes):
        need = {}
        for r in reads:
            k, _ = self._key(r)
            self._merge(need, self._st(k)['w'])
            if k.startswith('ps'):
                self._merge(need, self._st(k)['r'])
        for w in writes:
            k, par = self._key(w)
            s = self._st(k)
            if par:
                if s['phase'] == 'r':
                    old = {}
                    self._merge(old, s['w'])
                    self._merge(old, s['r'])
                    s['old'] = old
                    s['w'] = {}
                    s['r'] = {}
                    s['phase'] = 'w'
                self._merge(need, s['old'])
            else:
                self._merge(need, s['w'])
                self._merge(need, s['r'])
                self._merge(need, s['old'])
        return need

    def _commit(self, reads, writes, ev):
        for r in reads:
            k, _ = self._key(r)
            s = self._st(k)
            self._merge(s['r'], ev)
            s['phase'] = 'r'
        for w in writes:
            k, par = self._key(w)
            s = self._st(k)
            if par:
                self._merge(s['w'], ev)
            else:
                s['w'] = dict(ev)
                s['r'] = {}
                s['old'] = {}
                s['phase'] = 'w'

    def _wait(self, eng, need):
        seen = self.seen[eng]
        for n, (h, v) in need.items():
            if eng == 'pe' and n.startswith('s_pe_'):
                continue
            if seen.get(n, 0) >= v:
                continue
            for k in ('pe', 'act', 'dve', 'pool'):
                if n == f"s_{k}_{self.nsem[k]}":
                    assert v <= self.cnt[k], (eng, n, v, self.cnt[k])
            self.e[eng].wait_ge(h, v)
            self.ninst += 1
            seen[n] = v

    def op(self, eng, fn, reads=(), writes=(), inc=True):
        need = self._need(reads, writes)
        self._wait(eng, need)
        ins = fn(self.e[eng])
        self.ninst += 1
        if inc:
            if self.cnt[eng] >= self.ROT:
                self.nsem[eng] += 1
                self.sem[eng] = self.nc.alloc_semaphore(f"s_{eng}_{self.nsem[eng]}")
                self.cnt[eng] = 0
            self.cnt[eng] += 1
            ins.then_inc(self.sem[eng], 1)
            ev = {self.sem[eng].name: (self.sem[eng], self.cnt[eng])}
        else:
            assert self.cnt[eng] < self.ROT - 64
            ev = {self.sem[eng].name: (self.sem[eng], self.cnt[eng] + 1)}
        self._commit(reads, writes, ev)
        return ev

    def dma(self, out, in_, reads=None, writes=None, q='sp', **kw):
        reads = [in_] if reads is None else reads
        writes = [out] if writes is None else writes
        i = self.drr
        self.drr = (self.drr + 1) % self.NDMA
        h = self.dsem[i]
        need = self._need(reads, writes)
        if self.dcnt[i] > 0:
            self._merge(need, {h.name: (h, 16 * self.dcnt[i])})
        self._wait(q, need)
        self.e[q].dma_start(out=out, in_=in_, **kw).then_inc(h, 16)
        self.ninst += 1
        self.dcnt[i] += 1
        ev = {h.name: (h, 16 * self.dcnt[i])}
        self._commit(reads, writes, ev)
        return ev

    def finish(self, keys, eng='sp'):
        need = {}
        for k in keys:
            kk, _ = self._key(k)
            self._merge(need, self._st(kk)['w'])
        self._wait(eng, need)

    def mm(self, out, lhsT, rhs, start=True, stop=True, inc=None, extra_r=()):
        inc = stop if inc is None else inc
        return self.op('pe', lambda e: e.matmul(out, lhsT, rhs, start=start, stop=stop),
                       reads=[lhsT, rhs, *extra_r], writes=[out], inc=inc)

    def tr(self, out, in_, ident, inc=True):
        return self.op('pe', lambda e: e.transpose(out, in_, ident), reads=[in_, ident], writes=[out], inc=inc)

    def act(self, out, in_, func, bias=None, scale=None, accum_out=None, eng='act'):
        kw = {}
        reads = [in_]
        writes = [out]
        if bias is not None:
            kw['bias'] = bias
            if not isinstance(bias, (int, float)):
                reads.append(bias)
        if scale is not None:
            kw['scale'] = scale
            if not isinstance(scale, (int, float)):
                reads.append(scale)
        if accum_out is not None:
            kw['accum_out'] = accum_out
            writes.append(accum_out)
        return self.op('act', lambda e: e.activation(out, in_, func, **kw), reads=reads, writes=writes)

    def tt(self, out, in0, in1, op, eng='dve'):
        return self.op(eng, lambda e: e.tensor_tensor(out, in0, in1, op), reads=[in0, in1], writes=[out])

    def ts(self, out, in0, s1, s2, op0, op1=None, eng='dve', accum_out=None):
        reads = [in0] + [s for s in (s1, s2) if s is not None and not isinstance(s, (int, float))]
        writes = [out] + ([accum_out] if accum_out is not None else [])
        kw = {}
        if accum_out is not None:
            kw['accum_out'] = accum_out
        if op1 is None:
            return self.op(eng, lambda e: e.tensor_scalar(out, in0, s1, None, op0, **kw), reads=reads, writes=writes)
        return self.op(eng, lambda e: e.tensor_scalar(out, in0, s1, s2, op0, op1, **kw), reads=reads, writes=writes)

    def stt(self, out, in0, scalar, in1, op0, op1, eng='dve'):
        reads = [in0, in1] + ([scalar] if not isinstance(scalar, (int, float)) else [])
        return self.op(eng, lambda e: e.scalar_tensor_tensor(out, in0, scalar, in1, op0, op1), reads=reads, writes=[out])

    def copy(self, out, in_, eng='dve'):
        if eng == 'act':
            return self.op('act', lambda e: e.copy(out, in_), reads=[in_], writes=[out])
        return self.op(eng, lambda e: e.tensor_copy(out, in_), reads=[in_], writes=[out])

    def memset(self, ap, val, eng='dve'):
        return self.op(eng, lambda e: e.memset(ap, val), reads=[], writes=[ap])

    def reduce(self, out, in_, op, eng='dve'):
        return self.op(eng, lambda e: e.tensor_reduce(out, in_, AX.X, op), reads=[in_], writes=[out])


from contextlib import ExitStack

D = 1024
NT = 34
TOK = 4352
INC = 3232
EPS = 1e-6
PROWS = 4356


def prow(t):
    return 1 + t if t < 256 else 3 + t


class Ctx:
    pass


def barrier(C):
    s = C.s
    need = {}
    for k in ('pe', 'act', 'dve', 'pool'):
        if s.cnt[k] > 0:
            need[s.sem[k].name] = (s.sem[k], s.cnt[k])
    for i, h in enumerate(s.dsem):
        if s.dcnt[i] > 0:
            need[h.name] = (h, 16 * s.dcnt[i])
    for eng in ('pe', 'act', 'dve', 'pool', 'sp'):
        seen = s.seen[eng]
        for n, (h, v) in need.items():
            if seen.get(n, 0) >= v:
                continue
            s.e[eng].wait_ge(h, v)
            s.ninst += 1
            seen[n] = v
    s.res = {}


class Stage:
    def __init__(self, C, name):
        self.C = C
        self.name = name
        self.es = ExitStack()
        self.n = 0

    def __enter__(self):
        self.es.__enter__()
        return self

    def sb(self, shape, dtype=F32, name=None):
        self.n += 1
        nm = f"{self.name}_{name or 't'}{self.n}"
        t = self.es.enter_context(self.C.nc.sbuf_tensor(nm, list(shape), dtype))
        return t.ap()

    def __exit__(self, *a):
        barrier(self.C)
        return self.es.__exit__(*a)


def bcast_load(C, st, vec_ap, n, name, q='sp'):
    t = st.sb([128, n], F32, name)
    C.s.dma(t, vec_ap.partition_broadcast(128), q=q)
    return t


def stage_consts(C):
    nc, s = C.nc, C.s
    C.ident = nc.alloc_sbuf_tensor("ident_sb", [128, 128], F32).ap()
    C.identb = nc.alloc_sbuf_tensor("identb", [128, 128], BF16).ap()
    C.zero = nc.alloc_sbuf_tensor("zero_sb", [128, 808], F32).ap()
    C.ones = nc.alloc_sbuf_tensor("ones_sb", [128, 128], F32).ap()
    s.dma(C.ident, C.d['ident'])
    s.copy(C.identb, C.ident)
    s.memset(C.zero, 0.0)
    s.memset(C.ones, 1.0)
    for r in (0, 257, 258, 4355):
        for q4 in range(4):
            s.dma(C.P[r:r + 1, q4 * 808:(q4 + 1) * 808], C.zero[0:1, :], writes=[('par', C.P)], q='pool')
    C.ps = [nc.alloc_psum_tensor(f"ps{i}", [128, 512], F32).ap() for i in range(6)]
    C.psb = [nc.alloc_psum_tensor(f"psb{i}", [128, 1024], BF16).ap() for i in range(2)]
    barrier(C)


def stage_mod(C, l):
    nc, s, d = C.nc, C.s, C.d
    with Stage(C, f"mod{l}") as t:
        MOD = t.sb([128, 2, 6144], F32, "MOD")
        cT = t.sb([128, 16], F32)
        cs = t.sb([128, 16], F32)
        cb = t.sb([128, 16, 128], F32)
        brow = t.sb([1, 6144], F32)
        s.dma(cT, d['cT'])
        s.dma(brow, d['b_ada'][l:l + 1, :])
        s.act(cs, cT, AF.Silu)
        for j in range(16):
            s.copy(cb[:, j, :], cs[:, j:j + 1].to_broadcast([128, 128]), eng='pool' if j % 2 else 'dve')
        wts = [t.sb([128, 8, 512], F32, f"wa{i}") for i in range(2)]
        wv = d['w_ada'][l].rearrange("(c p) n -> p c n", p=128)
        for n in range(12):
            w = wts[n % 2]
            s.dma(w, wv[:, :, n * 512:(n + 1) * 512])
            for r in range(2):
                ps = C.ps[(2 * n + r) % 4]
                for k in range(8):
                    s.mm(ps, cb[:, r * 8 + k, :], w[:, k, :], start=(k == 0), stop=False)
                s.mm(ps, C.ones[0:1, :], brow[0:1, n * 512:(n + 1) * 512], start=False, stop=True)
                s.copy(MOD[:, r, n * 512:(n + 1) * 512], ps, eng='act' if r else 'dve')
        g1 = bcast_load(C, t, d['norm1_g'][l], 1024, "g1")
        g2 = bcast_load(C, t, d['norm2_g'][l], 1024, "g2")
        for r in range(2):
            for (seg, g) in ((1, g1), (4, g2)):
                sl = MOD[:, r, seg * 1024:(seg + 1) * 1024]
                s.stt(sl, sl, 1.0, g, ALU.add, ALU.mult)
        for r in range(2):
            s.dma(C.MODD[l][r:r + 1, :], MOD[0:1, r, :], writes=[('par', C.MODD[l])], q='pool')


def rmsnorm_mod_T(C, st, xt, G, sh, hT, tmp):
    s = C.s
    s.act(tmp['junk'], xt, AF.Square, accum_out=tmp['ss'])
    s.act(tmp['rs'], tmp['ss'], AF.Sqrt, bias=EPS, scale=1.0 / D)
    s.op('dve', lambda e: e.reciprocal(tmp['rs'], tmp['rs']), reads=[tmp['rs']], writes=[tmp['rs']])
    s.stt(tmp['t1'], xt, tmp['rs'], G, ALU.mult, ALU.mult)
    s.tt(tmp['hb'], tmp['t1'], sh, ALU.add, eng='pool')
    pb = C.psb[tmp['i'] % 2]
    tmp['i'] += 1
    for k in range(8):
        s.tr(pb[:, k * 128:(k + 1) * 128], tmp['hb'][:, k * 128:(k + 1) * 128], C.identb, inc=(k == 7))
    s.copy(hT, pb.rearrange("p (c n) -> p c n", c=8), eng='act')


def norm_tmp(st):
    return dict(junk=st.sb([128, 1024], BF16), ss=st.sb([128, 1], F32), rs=st.sb([128, 1], F32),
                t1=st.sb([128, 1024], F32), hb=st.sb([128, 1024], BF16), i=0)


def load_cast_w(C, st, dst_bf, src_rows, stg, q='sp', eng='pool'):
    C.s.dma(stg, src_rows, q=q)
    C.s.copy(dst_bf, stg, eng=eng)


def stage_win(C, l, MODD, xsrc):
    nc, s, d = C.nc, C.s, C.d
    with Stage(C, f"win{l}") as t:
        wb = t.sb([128, 8, INC], BF16, "wb")
        stg = [t.sb([128, INC], F32, f"stg{i}") for i in range(2)]
        for k in range(8):
            load_cast_w(C, t, wb[:, k, :], d['w_in'][l, k * 128:(k + 1) * 128, :], stg[k % 2], eng='pool')
        tmp = norm_tmp(t)
        seg = {(r, sg): bcast_load(C, t, MODD[r, sg * 1024:(sg + 1) * 1024], 1024, f"seg{r}{sg}") for r in (0, 1) for sg in (0, 1)}
        xts = [t.sb([128, 1024], F32, f"x{i}") for i in range(2)]
        hTs = [t.sb([128, 8, 128], BF16, f"hT{i}") for i in range(2)]
        pts = [t.sb([128, INC], F32, f"p{i}") for i in range(2)]
        for i in range(NT):
            r = 0 if i < 2 else 1
            xt, hT, pt = xts[i % 2], hTs[i % 2], pts[i % 2]
            s.dma(xt, xsrc[i * 128:(i + 1) * 128, :])
            rmsnorm_mod_T(C, t, xt, seg[(r, 1)], seg[(r, 0)], hT, tmp)
            for n in range(7):
                c0, c1 = n * 512, min(INC, (n + 1) * 512)
                ps = C.ps[n % 4]
                for k in range(8):
                    s.mm(ps[:, 0:c1 - c0], hT[:, k, :], wb[:, k, c0:c1], start=(k == 0), stop=(k == 7))
                s.copy(pt[:, c0:c1], ps[:, 0:c1 - c0], eng='act' if n % 2 else 'dve')
            r0 = prow(i * 128)
            s.dma(C.P[r0:r0 + 128, :], pt, writes=[('par', C.P)], q='pool')


WSHAPES = dict(
    w_ada=(2, 1024, 6144), b_ada=(2, 6144), norm1_g=(2, 1024), norm2_g=(2, 1024), w_in=(2, 1024, 3232),
    hg_lb=(2, 2, 256), hg_norm_g=(2, 256), rw_mu=(2, 2, 1088), rw_w0=(2, 2, 256), rw_w2=(2, 2, 64, 256),
    rw_a0=(2, 256), rw_a2=(2, 64, 256), rw_g2=(2, 128, 256), rw_k_k=(2, 256), rw_k_a=(2, 256),
    rw_r_k=(2, 4, 64), rw_ln_g=(2, 256), rw_ln_b=(2, 256), swa_sink=(2, 4), mla_qnorm_g=(2, 192),
    mla_wuq=(2, 192, 384), mla_kvnorm_g=(2, 128), mla_wukv=(2, 128, 512), w_out=(2, 1024, 1024),
    ffn_w1=(1, 1024, 2816), ffn_w3=(1, 1024, 2816), ffn_w2=(1, 2816, 1024), moe_router=(1, 1024, 8),
    moe_w1=(1, 8, 1024, 1408), moe_w3=(1, 8, 1024, 1408), moe_w2=(1, 8, 1408, 1024), final_norm_g=(1024,),
)
CSHAPES = dict(ident=(128, 128), cT=(128, 16), cosH=(4096, 32), sinH=(4096, 32), cosM=(4096, 16), sinM=(4096, 16), maskP=(128, 128), maskN=(128, 128), scn=(2, 64, 450), bdm=(128, 128))


def build(upto=None, dbg=(), din=()):
    nc = bass.Bass("TRN2", target_bir_lowering=False)
    C = Ctx()
    C.nc = nc
    C.s = Sched(nc)
    C.d = {}
    C.d['xin'] = nc.dram_tensor("xin", [TOK, D], F32, kind="ExternalInput").ap()
    for k, shp in {**WSHAPES, **CSHAPES}.items():
        C.d[k] = nc.dram_tensor(k, list(shp), F32, kind="ExternalInput").ap()
    C.out = nc.dram_tensor("out", [2048, D], F32, kind="ExternalOutput").ap()

    def scratch(name, shape):
        kind = "ExternalOutput" if name in dbg else "Internal"
        return nc.dram_tensor(name, list(shape), F32, kind=kind).ap()
    C.P = scratch("P", [PROWS, INC])
    C.scratch = scratch
    C.MODD = [scratch(f"MODD{l}", [2, 6144]) for l in range(2)]
    C.CAT = scratch("CAT", [TOK, D]) if "CAT" not in din else nc.dram_tensor("CAT", [TOK, D], F32, kind="ExternalInput").ap()
    C.XMID = scratch("XMID", [TOK, D])
    C.XS = scratch("XS", [TOK, D])

    def bscratch(name, shape):
        return nc.dram_tensor(name, list(shape), BF16).ap()
    C.WB = dict(f1=bscratch("WBf1", [1024, 2816]), f3=bscratch("WBf3", [1024, 2816]), f2=bscratch("WBf2", [2816, 1024]),
                m1=[bscratch(f"WBm1_{e}", [1024, 1408]) for e in range(8)],
                m3=[bscratch(f"WBm3_{e}", [1024, 1408]) for e in range(8)],
                m2=[bscratch(f"WBm2_{e}", [1408, 1024]) for e in range(8)])
    C.SCH = scratch("SCH", [TOK, HGC['n']])
    C.SCR = scratch("SCR", [TOK, RWC['n']])
    C.OHG = [scratch(f"OHG{i}", [TOK, 256]) for i in range(2)]
    C.ORW = [scratch(f"ORW{i}", [TOK, 256]) for i in range(2)]
    stage_consts(C)
    program(C, upto)
    C.s.finish([C.out] + [n for n in dbg], 'sp')
    print("ninst", C.s.ninst, "sems", {k: v for k, v in C.s.nsem.items()})
    return nc


def program(C, upto):
    if upto in ('ffn', 'moe'):
        with Stage(C, "precast") as t_:
            for _ in precast_gen(C, t_):
                pass
    for l in range(2):
        xsrc = C.d['xin'] if l == 0 else C.XS
        ctx_out = l < 1
        stage_mod(C, l)
        stage_win(C, l, C.MODD[l], xsrc)
        if upto == 'win':
            return
        if upto in (None, 'scans'):
            stage_hg_prep(C, l)
            stage_rw_prep(C, l)
            stage_scans(C, l)
            stage_hg_read(C, l)
            stage_rw_read(C, l)
            if upto == 'scans':
                return
        if upto in (None, 'attn'):
            stage_swa(C, l, ctx_out)
            stage_mla(C, l, ctx_out)
            if upto == 'attn':
                return
        stage_out_ffn(C, l, C.MODD[l], xsrc, moe=(l % 2 == 1))
        if upto == 'ffn':
            return
        if upto == 'L0':
            return
    stage_final(C)


def host_inputs(inputs, b, rev=False):
    m = {}
    cx, xx = np.asarray(inputs['ctx'][b], np.float32), np.asarray(inputs['x'][b], np.float32)
    if rev:
        cx, xx = cx[::-1], xx[::-1]
    m['xin'] = np.ascontiguousarray(np.concatenate([cx, xx], axis=0), dtype=np.float32)
    cc = np.stack([np.asarray(inputs['c_ctx'], np.float32), np.asarray(inputs['c'][b], np.float32)], 0)
    m['cT'] = np.ascontiguousarray(cc.reshape(2, 8, 128).transpose(2, 0, 1).reshape(128, 16))
    m['ident'] = np.eye(128, dtype=np.float32)
    m.update(rope_consts())
    m.update(scan_consts())
    for k in WSHAPES:
        m[k] = np.ascontiguousarray(inputs[k], dtype=np.float32)
    if rev:
        for k in ('cosH', 'sinH', 'cosM', 'sinM'):
            m[k] = np.ascontiguousarray(m[k][::-1])
        perm = np.arange(INC)
        perm[256:512], perm[512:768] = np.arange(512, 768), np.arange(256, 512)
        o = 1280 + 768
        perm[o:o + 64], perm[o + 64:o + 128] = np.arange(o + 64, o + 128), np.arange(o, o + 64)
        m['w_in'] = np.ascontiguousarray(m['w_in'][:, :, perm])
        mu = m['rw_mu'][:, ::-1, :]
        m['rw_mu'] = np.ascontiguousarray(mu[:, :, perm[1280:2368] - 1280])
        m['hg_lb'] = np.ascontiguousarray(m['hg_lb'][:, ::-1, :])
        m['rw_w0'] = np.ascontiguousarray(m['rw_w0'][:, ::-1, :])
        m['rw_w2'] = np.ascontiguousarray(m['rw_w2'][:, ::-1, :, :])
    return m


def precast_gen(C, t):
    nc, s, d = C.nc, C.s, C.d
    jobs = []
    for nm, src in (('f1', d['ffn_w1'][0]), ('f3', d['ffn_w3'][0]), ('f2', d['ffn_w2'][0])):
        jobs.append((C.WB[nm], src))
    for e in range(8):
        jobs.append((C.WB['m1'][e], d['moe_w1'][0, e]))
        jobs.append((C.WB['m3'][e], d['moe_w3'][0, e]))
        jobs.append((C.WB['m2'][e], d['moe_w2'][0, e]))
    NB = 2
    stg = [t.sb([128, 2816], F32, f"pcs{i}") for i in range(NB)]
    wbf = [t.sb([128, 2816], BF16, f"pcw{i}") for i in range(NB)]
    it = 0
    for dst, src in jobs:
        rows, cols = src.shape
        per = max(1, 2816 // cols)
        nchunk = rows // 128
        c0 = 0
        while c0 < nchunk:
            n = min(per, nchunk - c0)
            sg = stg[it % NB]
            wb = wbf[it % NB]
            sv = sg[:, 0:n * cols].rearrange("p (a n) -> p a n", a=n)
            wv = wb[:, 0:n * cols].rearrange("p (a n) -> p a n", a=n)
            s.dma(sv, src[c0 * 128:(c0 + n) * 128, :].rearrange("(a p) n -> p a n", p=128), reads=[src], writes=[sg])
            s.copy(wv, sv, eng='act')
            s.dma(dst[c0 * 128:(c0 + n) * 128, :].rearrange("(a p) n -> p a n", p=128), wv, reads=[wb], writes=[('par', dst)], q='pool')
            c0 += n
            it += 1
            yield


def stage_out_ffn(C, l, MODD, xsrc, moe):
    nc, s, d = C.nc, C.s, C.d
    G = 8
    nlat = 32 if l == 0 else 16
    groups = ([[0, 1]] if l == 0 else []) + [list(range(2 + g * G, 2 + (g + 1) * G)) for g in range(nlat // G)]
    WB = C.WB
    if moe:
        experts = [(WB['m1'][e], WB['m3'][e], WB['m2'][e]) for e in range(8)]
    else:
        experts = [(WB['f1'][:, h * 1408:(h + 1) * 1408], WB['f3'][:, h * 1408:(h + 1) * 1408],
                    WB['f2'][h * 1408:(h + 1) * 1408, :]) for h in range(2)]
    with Stage(C, f"ffn{l}") as t:
        wob = t.sb([128, 8, 1024], BF16, "wob")
        stg = [t.sb([128, 1024], F32, f"stg{i}") for i in range(2)]
        for k in range(8):
            load_cast_w(C, t, wob[:, k, :], d['w_out'][l, k * 128:(k + 1) * 128, :], stg[k % 2])
        segt = {sg: t.sb([128, 1024], F32, f"seg{sg}") for sg in (2, 3, 4, 5)}
        seg = {(r, sg): segt[sg] for r in (0, 1) for sg in (2, 3, 4, 5)}
        cur_r = [None]
        if moe:
            rt = t.sb([128, 8, 8], F32, "router")
            s.dma(rt, d['moe_router'][0].rearrange("(c p) e -> p c e", p=128))
        tmp = norm_tmp(t)
        cts = [t.sb([128, 1024], F32, f"cat{i}") for i in range(1)]
        xts = [t.sb([128, 1024], F32, f"x{i}") for i in range(1)]
        catb = t.sb([128, 1024], BF16, "catb")
        catT = t.sb([128, 8, 128], BF16, "catT")
        xm = t.sb([128, 1024], F32, "xm")
        h2T = t.sb([128, 8, G * 128], BF16, "h2T")
        Y = t.sb([128, G, 1024], F32, "Y")
        actT = t.sb([128, 11, 512], BF16, "actT")
        sa = [t.sb([128, 512], BF16, f"sa{i}") for i in range(2)]
        w1b = t.sb([128, 8, 1408], BF16, "w1b")
        w3b = t.sb([128, 8, 1408], BF16, "w3b")
        w2b = t.sb([128, 11, 1024], BF16, "w2b")
        comb = t.sb([128, G, 8], F32, "comb")
        if moe:
            h2f = t.sb([128, 1024], F32, "h2f")
            h2Tf = t.sb([128, 8, 128], F32, "h2Tf")
            lg = t.sb([128, 8], F32, "lg")
            mx = t.sb([128, 8], F32, "mx")
            nv1 = t.sb([128, 1], F32, "nv1")
            msk = t.sb([128, 8], F32, "msk")
            ex = t.sb([128, 8], F32, "ex")
            sm = t.sb([128, 1], F32, "sm")
        ti = 0
        for grp in groups:
            r = 0 if grp[0] < 2 else 1
            ng = len(grp)
            if cur_r[0] != r:
                cur_r[0] = r
                for sg in (2, 3, 4, 5):
                    s.dma(segt[sg], MODD[r, sg * 1024:(sg + 1) * 1024].partition_broadcast(128))
            for j, i in enumerate(grp):
                ct, xt = cts[0], xts[0]
                ti += 1
                s.dma(ct, C.CAT[i * 128:(i + 1) * 128, :])
                s.dma(xt, xsrc[i * 128:(i + 1) * 128, :])
                s.copy(catb, ct, eng='pool')
                pb = C.psb[tmp['i'] % 2]
                tmp['i'] += 1
                for k in range(8):
                    s.tr(pb[:, k * 128:(k + 1) * 128], catb[:, k * 128:(k + 1) * 128], C.identb, inc=(k == 7))
                s.copy(catT, pb.rearrange("p (c n) -> p c n", c=8), eng='act')
                for hf in range(2):
                    ps = C.ps[hf]
                    for k in range(8):
                        s.mm(ps, catT[:, k, :], wob[:, k, hf * 512:(hf + 1) * 512], start=(k == 0), stop=(k == 7))
                    s.tt(xm[:, hf * 512:(hf + 1) * 512], ps, seg[(r, 2)][:, hf * 512:(hf + 1) * 512], ALU.mult)
                s.tt(xm, xm, xt, ALU.add, eng='pool')
                s.dma(C.XMID[i * 128:(i + 1) * 128, :], xm, writes=[('par', C.XMID)], q='pool')
                rmsnorm_mod_T(C, t, xm, seg[(r, 4)], seg[(r, 3)], h2T[:, :, j * 128:(j + 1) * 128], tmp)
                if moe:
                    s.tt(h2f, tmp['t1'], seg[(r, 3)], ALU.add)
                    for hf in range(2):
                        ps = C.ps[2 + hf]
                        for k in range(4):
                            kk = hf * 4 + k
                            s.tr(ps[:, k * 128:(k + 1) * 128], h2f[:, kk * 128:(kk + 1) * 128], C.ident, inc=(k == 3))
                        s.copy(h2Tf[:, hf * 4:(hf + 1) * 4, :], ps.rearrange("p (c n) -> p c n", c=4), eng='act')
                    ps = C.ps[4]
                    for k in range(8):
                        s.mm(ps[:, 0:8], h2Tf[:, k, :], rt[:, k, :], start=(k == 0), stop=(k == 7))
                    s.copy(lg, ps[:, 0:8])
                    s.op('dve', lambda e: e.max(out=mx, in_=lg), reads=[lg], writes=[mx])
                    s.ts(nv1, mx[:, 0:1], -1.0, None, ALU.mult)
                    s.ts(msk, lg, mx[:, 1:2], None, ALU.is_ge)
                    s.act(ex, lg, AF.Exp, bias=nv1)
                    s.tt(ex, ex, msk, ALU.mult)
                    s.reduce(sm, ex, ALU.add)
                    s.op('dve', lambda e: e.reciprocal(sm, sm), reads=[sm], writes=[sm])
                    s.ts(comb[:, j, :], ex, sm, None, ALU.mult)
            nh = (ng * 128 + 511) // 512
            for ei, (w1, w3, w2) in enumerate(experts):
                s.dma(w1b, w1.rearrange("(k p) n -> p k n", p=128))
                s.dma(w3b, w3.rearrange("(k p) n -> p k n", p=128))
                s.dma(w2b, w2.rearrange("(k p) n -> p k n", p=128))
                for hf in range(nh):
                    t0, t1 = hf * 512, min(ng * 128, (hf + 1) * 512)
                    for c in range(11):
                        pa, pbk = C.ps[(2 * c) % 4], C.ps[(2 * c + 1) % 4]
                        for k in range(8):
                            s.mm(pa[:, 0:t1 - t0], w1b[:, k, c * 128:(c + 1) * 128], h2T[:, k, t0:t1], start=(k == 0), stop=(k == 7))
                        for k in range(8):
                            s.mm(pbk[:, 0:t1 - t0], w3b[:, k, c * 128:(c + 1) * 128], h2T[:, k, t0:t1], start=(k == 0), stop=(k == 7))
                        sx = sa[c % 2]
                        s.act(sx[:, 0:t1 - t0], pa[:, 0:t1 - t0], AF.Silu)
                        s.tt(actT[:, c, 0:t1 - t0], sx[:, 0:t1 - t0], pbk[:, 0:t1 - t0], ALU.mult)
                    for j in range(hf * 4, min(ng, hf * 4 + 4)):
                        jl = j - hf * 4
                        for h2_ in range(2):
                            ps = C.ps[4 + (2 * j + h2_) % 2]
                            for c in range(11):
                                s.mm(ps, actT[:, c, jl * 128:(jl + 1) * 128], w2b[:, c, h2_ * 512:(h2_ + 1) * 512], start=(c == 0), stop=(c == 10))
                            ys = Y[:, j, h2_ * 512:(h2_ + 1) * 512]
                            if moe:
                                if ei == 0:
                                    s.ts(ys, ps, comb[:, j, ei:ei + 1], None, ALU.mult)
                                else:
                                    s.stt(ys, ps, comb[:, j, ei:ei + 1], ys, ALU.mult, ALU.add)
                            else:
                                if ei == 0:
                                    s.copy(ys, ps, eng='act')
                                else:
                                    s.tt(ys, ys, ps, ALU.add)
            for j, i in enumerate(grp):
                xt = xts[0]
                ti += 1
                s.dma(xt, C.XMID[i * 128:(i + 1) * 128, :])
                s.tt(Y[:, j, :], Y[:, j, :], seg[(r, 5)], ALU.mult, eng='pool')
                s.tt(Y[:, j, :], Y[:, j, :], xt, ALU.add)
                s.dma(C.XS[i * 128:(i + 1) * 128, :], Y[:, j, :], writes=[('par', C.XS)], q='pool')


def stage_final(C):
    s, d = C.s, C.d
    with Stage(C, "final") as t:
        g = bcast_load(C, t, d['final_norm_g'], 1024, "fg")
        xts = [t.sb([128, 1024], F32, f"x{i}") for i in range(2)]
        junk = t.sb([128, 1024], BF16)
        ss = t.sb([128, 1], F32)
        rs = t.sb([128, 1], F32)
        for i in range(16):
            xt = xts[i % 2]
            s.dma(xt, C.XS[(i + 2) * 128:(i + 3) * 128, :])
            s.act(junk, xt, AF.Square, accum_out=ss)
            s.act(rs, ss, AF.Sqrt, bias=EPS, scale=1.0 / D)
            s.op('dve', lambda e: e.reciprocal(rs, rs), reads=[rs], writes=[rs])
            s.stt(xt, xt, rs, g, ALU.mult, ALU.mult)
            s.dma(C.out[i * 128:(i + 1) * 128, :], xt, writes=[('par', C.out)], q='pool')


def rope_tm(C, out, x, cos, sin, H, n, ta, tb):
    s = C.s
    xv = x.rearrange("p (h a f n) -> p h a f n", h=H, a=2, f=2)
    ov = out.rearrange("p (h a f n) -> p h a f n", h=H, a=2, f=2)
    x1, x2 = xv[:, :, :, 0, :], xv[:, :, :, 1, :]
    cb = cos.rearrange("p (a n) -> p a n", a=2).unsqueeze(1).to_broadcast([128, H, 2, n])
    sb_ = sin.rearrange("p (a n) -> p a n", a=2).unsqueeze(1).to_broadcast([128, H, 2, n])
    tav = ta[:, 0:H * 2 * n].rearrange("p (h a n) -> p h a n", h=H, a=2)
    tbv = tb[:, 0:H * 2 * n].rearrange("p (h a n) -> p h a n", h=H, a=2)
    s.tt(tav, x1, cb, ALU.mult)
    s.tt(tbv, x2, sb_, ALU.mult, eng='pool')
    s.tt(ov[:, :, :, 0, :], tav, tbv, ALU.subtract)
    s.tt(tav, x1, sb_, ALU.mult)
    s.tt(tbv, x2, cb, ALU.mult, eng='pool')
    s.tt(ov[:, :, :, 1, :], tav, tbv, ALU.add)


def kmax_bcast(C, t, KS, nkm):
    s = C.s
    m1 = t.sb([128, 1], F32)
    row = t.sb([1, 128], F32)
    v = t.sb([1, 1], F32)
    s.reduce(m1, KS, ALU.max)
    ps = C.ps[5]
    s.tr(ps[0:1, 0:128], m1, C.ident)
    s.copy(row, ps[0:1, 0:128])
    s.reduce(v, row, ALU.max)
    s.act(v, v, AF.Sqrt)
    s.ts(v, v, -1.0, None, ALU.mult)
    s.mm(ps[:, 0:1], C.ones[0:1, :], v[0:1, 0:1])
    s.copy(nkm, ps[:, 0:1])


def attend(C, OT, ncols, q_rhs, blocks, scale, PTs, cnt, tail=None):
    s = C.s
    nb = len(blocks)
    LOOK = 2
    pss = {}

    def emit_scores(bi):
        ps = C.ps[(cnt[0] + bi) % 3]
        s.mm(ps[:, 0:ncols], blocks[bi][0], q_rhs)
        pss[bi] = ps
    for bi in range(min(LOOK, nb)):
        emit_scores(bi)
    for bi, (kT, vA, mask) in enumerate(blocks):
        if bi + LOOK < nb:
            emit_scores(bi + LOOK)
        ps = pss.pop(bi)
        PT = PTs[(cnt[0] + bi) % len(PTs)]
        s.act(PT[:, 0:ncols], ps[:, 0:ncols], AF.Exp, scale=scale)
        if mask is not None:
            nrep = ncols // 128
            pv = PT[:, 0:ncols].rearrange("p (r n) -> p r n", r=nrep)
            s.tt(pv, pv, mask.unsqueeze(1).to_broadcast([128, nrep, 128]), ALU.mult, eng='pool')
        s.mm(OT[0:65, 0:ncols], vA, PT[:, 0:ncols], start=(bi == 0), stop=(bi == nb - 1 and tail is None))
    cnt[0] += nb
    if tail is not None:
        tail()


def stage_swa(C, l, ctx_out):
    nc, s, d = C.nc, C.s, C.d
    scale = 64 ** -0.5
    with Stage(C, f"swa{l}") as t:
        KT = t.sb([65, 2, TOK], BF16, "KT")
        VA = t.sb([128, NT, 2, 65], BF16, "VA")
        QT = t.sb([65, 4, TOK], BF16, "QT")
        QR = t.sb([128, NT, 4, 65], BF16, "QR")
        SSQ = t.sb([128, NT, 4], F32, "SSQ")
        KS = t.sb([128, NT], F32, "KS")
        nkm = t.sb([128, 1], F32, "nkm")
        mP = t.sb([128, 128], BF16, "mP")
        mN = t.sb([128, 128], BF16, "mN")
        mf = t.sb([128, 128], F32, "mf")
        s.dma(mf, d['maskP'])
        s.copy(mP, mf)
        s.dma(mf, d['maskN'])
        s.copy(mN, mf)
        SINK = t.sb([65, 4], F32, "SINK")
        s.dma(SINK[64:65, :], d['swa_sink'][l:l + 1, :])
        E64 = t.sb([65, 65], BF16, "E64")
        s.memset(E64, 0.0)
        s.memset(E64[64:65, 64:65], 1.0)
        es = t.sb([65, 256], BF16, "es")
        s.memset(VA[:, :, :, 64:65], 1.0)
        kaug = t.sb([128, 2, 65], BF16, "kaug")
        s.memset(kaug[:, :, 64:65], 1.0)
        pqs = [t.sb([128, 512], F32, f"pq{i}") for i in range(2)]
        cs = [t.sb([128, 32], F32, f"cos{i}") for i in range(2)]
        sn = [t.sb([128, 32], F32, f"sin{i}") for i in range(2)]
        rot = t.sb([128, 384], F32, "rot")
        ta = t.sb([128, 384], F32, "ta")
        tb = t.sb([128, 384], F32, "tb")
        ssk = t.sb([128, 6], F32, "ssk")
        for i in range(NT):
            pq = pqs[i % 2]
            r0 = prow(i * 128)
            s.dma(pq, C.P[r0:r0 + 128, 2368:2880])
            if i >= 2:
                s.dma(cs[i % 2], d['cosH'][(i - 2) * 128:(i - 1) * 128, :])
                s.dma(sn[i % 2], d['sinH'][(i - 2) * 128:(i - 1) * 128, :])
                rope_tm(C, rot, pq[:, 0:384], cs[i % 2], sn[i % 2], 6, 16, ta, tb)
                src = rot
            else:
                src = pq[:, 0:384]
            s.tt(ta, pq[:, 0:384], pq[:, 0:384], ALU.mult)
            s.reduce(ssk, ta.rearrange("p (h n) -> p h n", h=6), ALU.add)
            s.copy(SSQ[:, i, :], ssk[:, 0:4], eng='pool')
            s.reduce(KS[:, i:i + 1], ssk[:, 4:6], ALU.max)
            s.copy(QR[:, i, :, 0:64], src[:, 0:256].rearrange("p (h n) -> p h n", h=4), eng='pool')
            s.copy(kaug[:, :, 0:64], src[:, 256:384].rearrange("p (h n) -> p h n", h=2))
            s.copy(VA[:, i, :, 0:64], pq[:, 384:512].rearrange("p (h n) -> p h n", h=2), eng='pool')
            pb = C.psb[i % 2]
            for j in range(2):
                s.tr(pb[0:65, j * 128:(j + 1) * 128], kaug[:, j, :], C.identb, inc=(j == 1))
            s.copy(KT[:, :, i * 128:(i + 1) * 128], pb[0:65, 0:256].rearrange("p (j n) -> p j n", j=2), eng='act')
        kmax_bcast(C, t, KS, nkm)
        nq = t.sb([128, 4], F32, "nq")
        for i in range(NT):
            if i < 2 and not ctx_out:
                continue
            s.act(nq, SSQ[:, i, :], AF.Sqrt)
            s.ts(QR[:, i, :, 64], nq, nkm, None, ALU.mult)
            pb = C.psb[i % 2]
            for h in range(4):
                s.tr(pb[0:65, h * 128:(h + 1) * 128], QR[:, i, h, :], C.identb, inc=(h == 3))
            s.copy(QT[:, :, i * 128:(i + 1) * 128], pb[0:65, 0:512].rearrange("p (j n) -> p j n", j=4), eng='act')
        PTs = [t.sb([128, 256], BF16, f"PT{i}") for i in range(3)]
        osb = t.sb([65, 256], F32, "osb")
        rden = t.sb([128, 2], F32, "rden")
        otile = [t.sb([128, 256], F32, f"ot{i}") for i in range(2)]
        cnt = [0]
        qblocks = ([0, 1] if ctx_out else []) + list(range(2, NT if l == 0 else 18))
        for qi, n in enumerate(qblocks):
            ot = otile[qi % 2]
            for j in range(2):
                if n < 2:
                    kbs = [(0, None), (1, None)]
                else:
                    kbs = [(0, None), (1, None)]
                    if n - 1 >= 2:
                        kbs.append((n - 1, mP))
                    kbs.append((n, None))
                    if n + 1 < NT:
                        kbs.append((n + 1, mN))
                blocks = [(KT[0:65, j, kb * 128:(kb + 1) * 128], VA[:, kb, j, :], m) for kb, m in kbs]
                OT = C.ps[3 + (qi * 2 + j) % 2]
                q_rhs = QT[0:65, 2 * j:2 * j + 2, n * 128:(n + 1) * 128]

                def tail(OT=OT, j=j, n=n):
                    for hh in range(2):
                        s.act(es[64:65, hh * 128:(hh + 1) * 128], QT[64:65, 2 * j + hh, n * 128:(n + 1) * 128], AF.Exp,
                              bias=SINK[64:65, 2 * j + hh:2 * j + hh + 1], scale=scale)
                    s.mm(OT[0:65, 0:256], E64[64:65, :], es[64:65, :], start=False, stop=True)
                attend(C, OT, 256, q_rhs, blocks, scale, PTs, cnt, tail)
                s.copy(osb, OT[0:65, 0:256])
                ps = C.ps[5]
                for hh in range(2):
                    s.tr(ps[:, hh * 65:(hh + 1) * 65], osb[:, hh * 128:(hh + 1) * 128], C.ident[0:65, 0:65], inc=(hh == 1))
                pv = ps[:, 0:130].rearrange("p (h n) -> p h n", h=2)
                s.op('dve', lambda e, pv=pv: e.reciprocal(rden, pv[:, :, 64]), reads=[ps], writes=[rden])
                s.tt(ot[:, j * 128:(j + 1) * 128].rearrange("p (h n) -> p h n", h=2), pv[:, :, 0:64],
                     rden.unsqueeze(2).to_broadcast([128, 2, 64]), ALU.mult)
            s.dma(C.CAT[n * 128:(n + 1) * 128, 512:768], ot, writes=[('par', C.CAT)], q='pool')


def stage_mla(C, l, ctx_out):
    nc, s, d = C.nc, C.s, C.d
    scale = 96 ** -0.5
    with Stage(C, f"mla{l}") as t:
        KT = t.sb([97, 4, TOK], BF16, "KT")
        VA = t.sb([128, NT, 4, 65], BF16, "VA")
        QT = t.sb([97, 4, TOK], BF16, "QT")
        QR = t.sb([128, NT, 4, 97], BF16, "QR")
        SSQ = t.sb([128, NT, 4], F32, "SSQ")
        KS = t.sb([128, NT], F32, "KS")
        nkm = t.sb([128, 1], F32, "nkm")
        s.memset(VA[:, :, :, 64:65], 1.0)
        kaug = t.sb([128, 4, 97], BF16, "kaug")
        s.memset(kaug[:, :, 96:97], 1.0)
        stg = t.sb([128, 512], F32, "stg")
        wq0 = t.sb([128, 384], BF16, "wq0")
        wq1 = t.sb([64, 384], BF16, "wq1")
        wkv = t.sb([128, 512], BF16, "wkv")
        s.dma(stg[:, 0:384], d['mla_wuq'][l, 0:128, :])
        s.copy(wq0, stg[:, 0:384])
        s.dma(stg[0:64, 0:384], d['mla_wuq'][l, 128:192, :])
        s.copy(wq1, stg[0:64, 0:384])
        s.dma(stg, d['mla_wukv'][l])
        s.copy(wkv, stg)
        gq = bcast_load(C, t, d['mla_qnorm_g'][l], 192, "gq")
        gkv = bcast_load(C, t, d['mla_kvnorm_g'][l], 128, "gkv")
        pms = [t.sb([128, 352], F32, f"pm{i}") for i in range(2)]
        cs = [t.sb([128, 16], F32, f"cos{i}") for i in range(2)]
        sn = [t.sb([128, 16], F32, f"sin{i}") for i in range(2)]
        junk = t.sb([128, 192], F32, "junk")
        ss2 = t.sb([128, 2], F32, "ss2")
        cn = t.sb([128, 320], BF16, "cn")
        cT = t.sb([128, 3, 128], BF16, "cT")
        qf = t.sb([128, 384], F32, "qf")
        kvf = t.sb([128, 512], F32, "kvf")
        qr_in = t.sb([128, 128], F32, "qr_in")
        qr_out = t.sb([128, 128], F32, "qr_out")
        kr_out = t.sb([128, 32], F32, "kr_out")
        ta = t.sb([128, 64], F32, "ta")
        tb = t.sb([128, 64], F32, "tb")
        sq = t.sb([128, 512], F32, "sq")
        s4 = t.sb([128, 4], F32, "s4")
        s4b = t.sb([128, 4], F32, "s4b")
        s1 = t.sb([128, 1], F32, "s1")
        for i in range(NT):
            pm = pms[i % 2]
            r0 = prow(i * 128)
            s.dma(pm, C.P[r0:r0 + 128, 2880:3232])
            s.act(junk[:, 0:192], pm[:, 0:192], AF.Square, accum_out=ss2[:, 0:1])
            s.act(junk[:, 0:128], pm[:, 192:320], AF.Square, accum_out=ss2[:, 1:2])
            s.act(ss2[:, 0:1], ss2[:, 0:1], AF.Sqrt, bias=EPS, scale=1.0 / 192)
            s.act(ss2[:, 1:2], ss2[:, 1:2], AF.Sqrt, bias=EPS, scale=1.0 / 128)
            s.op('dve', lambda e: e.reciprocal(ss2, ss2), reads=[ss2], writes=[ss2])
            s.stt(cn[:, 0:192], pm[:, 0:192], ss2[:, 0:1], gq, ALU.mult, ALU.mult)
            s.stt(cn[:, 192:320], pm[:, 192:320], ss2[:, 1:2], gkv, ALU.mult, ALU.mult)
            pb = C.psb[i % 2]
            s.tr(pb[:, 0:128], cn[:, 0:128], C.identb, inc=False)
            s.tr(pb[0:64, 128:256], cn[:, 128:192], C.identb, inc=False)
            s.tr(pb[:, 256:384], cn[:, 192:320], C.identb)
            s.copy(cT, pb[:, 0:384].rearrange("p (c n) -> p c n", c=3), eng='act')
            pq, pk = C.ps[0], C.ps[1]
            s.mm(pq[:, 0:384], cT[:, 0, :], wq0, start=True, stop=False)
            s.mm(pq[:, 0:384], cT[0:64, 1, :], wq1, start=False, stop=True)
            s.mm(pk, cT[:, 2, :], wkv)
            s.copy(qf, pq[:, 0:384], eng='act')
            s.copy(kvf, pk)
            qv = qf.rearrange("p (h n) -> p h n", h=4)
            kvv = kvf.rearrange("p (h n) -> p h n", h=4)
            s.tt(sq[:, 0:384], qf, qf, ALU.mult, eng='pool')
            s.reduce(SSQ[:, i, :], sq[:, 0:384].rearrange("p (h n) -> p h n", h=4), ALU.add)
            s.tt(sq, kvf, kvf, ALU.mult, eng='pool')
            s.reduce(s4, sq.rearrange("p (h n) -> p h n", h=4)[:, :, 0:64], ALU.add)
            s.tt(ta[:, 0:32], pm[:, 320:352], pm[:, 320:352], ALU.mult)
            s.reduce(s1, ta[:, 0:32], ALU.add)
            s.ts(s4b, s4, s1, None, ALU.add)
            s.reduce(KS[:, i:i + 1], s4b, ALU.max)
            if i >= 2:
                s.dma(cs[i % 2], d['cosM'][(i - 2) * 128:(i - 1) * 128, :])
                s.dma(sn[i % 2], d['sinM'][(i - 2) * 128:(i - 1) * 128, :])
                s.copy(qr_in.rearrange("p (h n) -> p h n", h=4), qv[:, :, 64:96], eng='pool')
                rope_tm(C, qr_out, qr_in, cs[i % 2], sn[i % 2], 4, 8, ta, tb)
                rope_tm(C, kr_out, pm[:, 320:352], cs[i % 2], sn[i % 2], 1, 8, ta, tb)
                qr_src = qr_out.rearrange("p (h n) -> p h n", h=4)
                kr_src = kr_out
            else:
                qr_src = qv[:, :, 64:96]
                kr_src = pm[:, 320:352]
            s.copy(QR[:, i, :, 0:64], qv[:, :, 0:64], eng='pool')
            s.copy(QR[:, i, :, 64:96], qr_src, eng='pool')
            s.copy(kaug[:, :, 0:64], kvv[:, :, 0:64])
            s.copy(kaug[:, :, 64:96], kr_src.unsqueeze(1).to_broadcast([128, 4, 32]))
            s.copy(VA[:, i, :, 0:64], kvv[:, :, 64:128], eng='pool')
            pb = C.psb[(i + 1) % 2]
            for h in range(4):
                s.tr(pb[0:97, h * 128:(h + 1) * 128], kaug[:, h, :], C.identb, inc=(h == 3))
            s.copy(KT[:, :, i * 128:(i + 1) * 128], pb[0:97, 0:512].rearrange("p (j n) -> p j n", j=4), eng='act')
        kmax_bcast(C, t, KS, nkm)
        nq = t.sb([128, 4], F32, "nq")
        for i in range(NT):
            if i < 2 and not ctx_out:
                continue
            s.act(nq, SSQ[:, i, :], AF.Sqrt)
            s.ts(QR[:, i, :, 96], nq, nkm, None, ALU.mult)
            pb = C.psb[i % 2]
            for h in range(4):
                s.tr(pb[0:97, h * 128:(h + 1) * 128], QR[:, i, h, :], C.identb, inc=(h == 3))
            s.copy(QT[:, :, i * 128:(i + 1) * 128], pb[0:97, 0:512].rearrange("p (j n) -> p j n", j=4), eng='act')
        PTs = [t.sb([128, 512], BF16, f"PT{i}") for i in range(3)]
        osb = t.sb([65, 512], F32, "osb")
        rden = t.sb([128, 4], F32, "rden")
        otile = [t.sb([128, 4, 256], F32, f"ot{i}") for i in range(2)]
        cnt = [0]
        qtiles = ([(0, 256)] if ctx_out else []) + [(256 + q * 512, 512) for q in range(8 if l == 0 else 4)]
        for qi, (q0, qn) in enumerate(qtiles):
            ot = otile[qi % 2]
            kbs = [0, 1] if q0 < 256 else list(range(NT))
            for h in range(4):
                blocks = [(KT[0:97, h, kb * 128:(kb + 1) * 128], VA[:, kb, h, :], None) for kb in kbs]
                OT = C.ps[3 + (qi * 4 + h) % 2]
                attend(C, OT, qn, QT[0:97, h, q0:q0 + qn], blocks, scale, PTs, cnt)
                s.copy(osb[:, 0:qn], OT[0:65, 0:qn])
                ps = C.ps[5]
                nsub = qn // 128
                for sb_ in range(nsub):
                    s.tr(ps[:, sb_ * 65:(sb_ + 1) * 65], osb[:, sb_ * 128:(sb_ + 1) * 128], C.ident[0:65, 0:65], inc=(sb_ == nsub - 1))
                pv = ps[:, 0:nsub * 65].rearrange("p (h n) -> p h n", h=nsub)
                s.op('dve', lambda e, pv=pv, nsub=nsub: e.reciprocal(rden[:, 0:nsub], pv[:, :, 64]), reads=[ps], writes=[rden])
                s.tt(ot[:, 0:nsub, h * 64:(h + 1) * 64], pv[:, :, 0:64],
                     rden[:, 0:nsub].unsqueeze(2).to_broadcast([128, nsub, 64]), ALU.mult)
            for sb_ in range(qn // 128):
                s.dma(C.CAT[q0 + sb_ * 128:q0 + (sb_ + 1) * 128, 768:1024], ot[:, sb_, :], writes=[('par', C.CAT)], q='pool')


def rope_consts():
    out = {}
    row = np.repeat(np.arange(64, dtype=np.float32), 64)
    col = np.tile(np.arange(64, dtype=np.float32), 64)
    for nm, rot in (('H', 64), ('M', 32)):
        nf = rot // 4
        inv = (np.float32(10000.0) ** (-np.arange(nf, dtype=np.float32) / np.float32(nf))).astype(np.float32)
        ang = np.stack([row[:, None] * inv, col[:, None] * inv], axis=1).astype(np.float32)
        out['cos' + nm] = np.cos(ang).reshape(4096, 2 * nf).astype(np.float32)
        out['sin' + nm] = np.sin(ang).reshape(4096, 2 * nf).astype(np.float32)
    j = np.arange(128)[:, None]
    i = np.arange(128)[None, :]
    out['maskP'] = (j >= i).astype(np.float32)
    out['maskN'] = (j <= i).astype(np.float32)
    return out


HGC = dict(r=0, kf=256, kb=512, v=768, lwf=1024, lwb=1280, n=1536)
RWC = dict(r=0, kf=256, kb=256, v=512, lwf=768, lwb=1024, kk=1280, b=1536, g=1792, bonus=2048, n=2304)
NEG_EXP_HALF = -0.6065306597126334


def stage_hg_prep(C, l):
    s, d = C.s, C.d
    with Stage(C, f"hgp{l}") as t:
        LB = t.sb([128, 512], F32, "LB")
        OML = t.sb([128, 512], F32, "OML")
        if l == 0:
            s.memset(LB, 0.0)
        else:
            a0 = bcast_load(C, t, d['hg_lb'][0].rearrange("a n -> (a n)"), 512, "a0")
            a1 = bcast_load(C, t, d['hg_lb'][1].rearrange("a n -> (a n)"), 512, "a1")
            s.tt(a1, a1, a0, ALU.subtract)
            s.act(LB, a1, AF.Sigmoid)
        s.ts(OML, LB, -1.0, 1.0, ALU.mult, ALU.add)
        pzs = [t.sb([128, 1024], F32, f"pz{i}") for i in range(2)]
        scs = [t.sb([128, HGC['n']], F32, f"sc{i}") for i in range(2)]
        sg = t.sb([128, 512], F32, "sg")
        for i in range(NT):
            pz, sc = pzs[i % 2], scs[i % 2]
            r0 = prow(i * 128)
            s.dma(pz, C.P[r0:r0 + 128, 0:1024])
            s.act(sc[:, 0:256], pz[:, 0:256], AF.Silu)
            s.copy(sc[:, 768:1024], pz[:, 768:1024], eng='pool')
            s.act(sg, pz[:, 256:768], AF.Sigmoid)
            s.tt(sg, sg, OML, ALU.mult)
            s.tt(sg, sg, LB, ALU.add, eng='pool')
            s.ts(sg, sg, 1e-30, None, ALU.max)
            s.ts(sc[:, 256:768], sg, -1.0, 1.0, ALU.mult, ALU.add, eng='pool')
            s.act(sc[:, 1024:1536], sg, AF.Ln)
            s.dma(C.SCH[i * 128:(i + 1) * 128, :], sc, writes=[('par', C.SCH)], q='pool')


def stage_rw_prep(C, l):
    s, d = C.s, C.d
    with Stage(C, f"rwp{l}") as t:
        MU0 = bcast_load(C, t, d['rw_mu'][l, 0], 1088, "mu0")
        MU1 = bcast_load(C, t, d['rw_mu'][l, 1], 1088, "mu1")
        W0 = bcast_load(C, t, d['rw_w0'][l].rearrange("a n -> (a n)"), 512, "w0")
        A0 = bcast_load(C, t, d['rw_a0'][l], 256, "a0")
        KKb = bcast_load(C, t, d['rw_k_k'][l], 256, "kkb")
        KA = bcast_load(C, t, d['rw_k_a'][l], 256, "ka")
        RK = bcast_load(C, t, d['rw_r_k'][l].rearrange("a n -> (a n)"), 256, "rk")
        OMKA = t.sb([128, 256], F32, "omka")
        s.ts(OMKA, KA, -1.0, 1.0, ALU.mult, ALU.add)
        W2 = t.sb([128, 256], F32, "W2")
        s.dma(W2, d['rw_w2'][l].rearrange("a k n -> (a k) n"))
        A2 = t.sb([64, 256], F32, "A2")
        s.dma(A2, d['rw_a2'][l])
        G2 = t.sb([128, 256], F32, "G2")
        s.dma(G2, d['rw_g2'][l])
        p0s = [t.sb([128, 1088], F32, f"p0{i}") for i in range(2)]
        pms = [t.sb([128, 1088], F32, f"pm{i}") for i in range(2)]
        pns = [t.sb([128, 1088], F32, f"pn{i}") for i in range(2)]
        scs = [t.sb([128, RWC['n']], F32, f"sc{i}") for i in range(2)]
        xs = t.sb([128, 1088], F32, "xs")
        thT = t.sb([128, 128], F32, "thT")
        yaT = t.sb([64, 128], F32, "yaT")
        sgT = t.sb([128, 128], F32, "sgT")
        a = t.sb([128, 256], F32, "a")
        tq = t.sb([128, 256], F32, "tq")
        tq2 = t.sb([128, 256], F32, "tq2")
        r4 = t.sb([128, 4], F32, "r4")
        for i in range(NT):
            p0, pm, pn, sc = p0s[i % 2], pms[i % 2], pns[i % 2], scs[i % 2]
            r0 = prow(i * 128)
            s.dma(p0, C.P[r0:r0 + 128, 1280:2368])
            s.dma(pm, C.P[r0 - 1:r0 + 127, 1280:2368])
            s.dma(pn, C.P[r0 + 1:r0 + 129, 1280:2368])
            s.tt(pm, pm, p0, ALU.subtract)
            s.tt(pn, pn, p0, ALU.subtract, eng='pool')
            s.tt(pm, pm, MU0, ALU.mult)
            s.tt(pn, pn, MU1, ALU.mult, eng='pool')
            s.tt(xs, p0, pm, ALU.add)
            s.tt(xs, xs, pn, ALU.add)
            ps = C.ps[0]
            s.tr(ps[:, 0:128], xs[:, 768:896], C.ident)
            s.act(thT, ps[:, 0:128], AF.Tanh)
            ps = C.ps[1]
            s.tr(ps[0:64, 0:128], xs[:, 896:960], C.ident)
            s.copy(yaT, ps[0:64, 0:128])
            ps = C.ps[2]
            s.tr(ps[:, 0:128], xs[:, 960:1088], C.ident)
            s.act(sgT, ps[:, 0:128], AF.Sigmoid)
            pw = C.ps[3]
            pw2 = C.ps[5]
            s.mm(pw[:, 0:256], thT[0:64, :], W2[0:64, :])
            s.mm(pw2[:, 0:256], thT[64:128, :], W2[64:128, :])
            pa = C.ps[4]
            s.mm(pa[:, 0:256], yaT, A2)
            s.mm(pa[:, 256:512], sgT, G2)
            lw = sc[:, RWC['lwf']:RWC['lwf'] + 512]
            s.tt(lw[:, 0:256], pw[:, 0:256], W0[:, 0:256], ALU.add)
            s.tt(lw[:, 256:512], pw2[:, 0:256], W0[:, 256:512], ALU.add)
            s.act(lw, lw, AF.Sigmoid)
            s.ts(lw, lw, NEG_EXP_HALF, None, ALU.mult, eng='pool')
            s.tt(a, pa[:, 0:256], A0, ALU.add)
            s.act(a, a, AF.Sigmoid)
            s.copy(sc[:, RWC['g']:RWC['g'] + 256], pa[:, 256:512], eng='act')
            s.copy(sc[:, 0:256], xs[:, 0:256], eng='pool')
            s.copy(sc[:, 512:768], xs[:, 512:768], eng='pool')
            kk = sc[:, RWC['kk']:RWC['kk'] + 256]
            s.tt(kk, xs[:, 256:512], KKb, ALU.mult)
            s.tt(tq, kk, kk, ALU.mult)
            s.reduce(r4, tq.rearrange("p (h n) -> p h n", h=4), ALU.add)
            s.act(r4, r4, AF.Sqrt)
            s.ts(r4, r4, 1e-12, None, ALU.max)
            s.op('dve', lambda e: e.reciprocal(r4, r4), reads=[r4], writes=[r4])
            kkv = kk.rearrange("p (h n) -> p h n", h=4)
            s.tt(kkv, kkv, r4.unsqueeze(2).to_broadcast([128, 4, 64]), ALU.mult)
            s.tt(tq, a, KA, ALU.mult, eng='pool')
            s.tt(tq, tq, OMKA, ALU.add, eng='pool')
            s.tt(sc[:, 256:512], xs[:, 256:512], tq, ALU.mult)
            s.tt(sc[:, RWC['b']:RWC['b'] + 256], kk, a, ALU.mult, eng='pool')
            s.tt(tq2, xs[:, 0:256], sc[:, 256:512], ALU.mult)
            s.tt(tq2, tq2, RK, ALU.mult)
            s.reduce(r4, tq2.rearrange("p (h n) -> p h n", h=4), ALU.add)
            s.tt(sc[:, RWC['bonus']:RWC['bonus'] + 256].rearrange("p (h n) -> p h n", h=4),
                 xs[:, 512:768].rearrange("p (h n) -> p h n", h=4), r4.unsqueeze(2).to_broadcast([128, 4, 64]), ALU.mult)
            s.dma(C.SCR[i * 128:(i + 1) * 128, :], sc, writes=[('par', C.SCR)], q='pool')


def scan_consts():
    out = np.zeros((2, 64, 450), np.float32)
    sidx = np.arange(64)[:, None]
    tidx = np.arange(64)[None, :]
    for dd in range(2):
        if dd == 0:
            tri = (sidx <= tidx)
            tris = (sidx < tidx)
            mid = 31
        else:
            tri = (sidx >= tidx)
            tris = (sidx > tidx)
            mid = 32
        tri = tri.astype(np.float32)
        tris = tris.astype(np.float32)
        cm = tri[:, mid:mid + 1]
        out[dd, :, 0:64] = tri - cm
        out[dd, :, 64:128] = tris - cm
        out[dd, :, 128] = cm[:, 0]
        out[dd, :, 129] = 1.0
        out[dd, :, 130:194] = tris.T
        out[dd, :, 194:258] = tris
        out[dd, :, 258:322] = tri
        out[dd, :, 322:386] = tris
        out[dd, :, 386:450] = tris.T
    bd = np.zeros((128, 128), np.float32)
    bd[:64, :64] = 1.0
    bd[64:, 64:] = 1.0
    return dict(scn=out, bdm=bd)


def hq(h):
    return 2 * (h % 2) + h // 2


def scan_gen(C, t, tag, banks, SC, cols, OUTD, delta):
    s, d = C.s, C.d
    nsteps = TOK // 64
    if True:
        K_ = t.sb([64, 2, 450], F32, "scn" + tag)
        s.dma(K_, d['scn'].rearrange("a s n -> s a n"))
        BD = t.sb([128, 128], F32, "BD" + tag)
        s.dma(BD, d['bdm'])
        I4 = t.sb([64, 4, 64], BF16, "I4" + tag)
        for h in range(4):
            s.copy(I4[:, h, :], C.ident[0:64, 0:64])
        H2 = [[t.sb([128, 128], F32, f"H{tag}{dd}{hp}") for hp in range(2)] for dd in range(2)]
        for dd in range(2):
            for hp in range(2):
                s.memset(H2[dd][hp], 0.0)
        bank = [0]

        def nb():
            bank[0] = (bank[0] + 1) % len(banks)
            return banks[bank[0]]

        def mk(name, shape, n=2, dt=F32):
            return [[t.sb(shape, dt, f"{name}{tag}{dd}{k}") for k in range(n)] for dd in range(2)]
        names_tok = ['LW', 'R', 'K', 'V'] + (['KK', 'B'] if delta else [])
        T = {nm: mk(nm, [64, 256]) for nm in names_tok}
        eP = mk('eP', [128, 2, 128])
        eM = mk('eM', [128, 2, 64])
        eS = mk('eS', [128, 2, 2])
        eT = mk('eT', [64, 512])
        fm = {nm: mk(nm, [128, 2, 64], dt=(F32 if nm == 'rTin' else BF16))
              for nm in (['rTm', 'kTm', 'rTin'] + (['kkTm', 'bTm'] if delta else []))}
        ePin = mk('ePin', [128, 2, 64])
        Vb = mk('Vb', [64, 256], dt=BF16)
        kout = mk('kout', [64, 256])
        ArkT = mk('ArkT', [64, 4, 64], dt=BF16)
        for dd in range(2):
            for k_ in range(2):
                s.memset(ArkT[dd][k_], 0.0)
        Osb = mk('Osb', [64, 256])
        tmpH = mk('tmpH', [128, 128], 1)
        if delta:
            bout = mk('bout', [64, 256], dt=BF16)
            kkin = mk('kkin', [64, 256], dt=BF16)
            ArbT = mk('ArbT', [64, 4, 64], dt=BF16)
            AkkT = mk('AkkT', [64, 4, 64], dt=BF16)
            Xq = mk('Xq', [64, 4, 64], 2, dt=BF16)
            Xt = mk('Xt', [64, 4, 64], 2, dt=BF16)
            Pm = mk('Pm', [64, 4, 64], 2, dt=BF16)
            X0 = mk('X0', [64, 4, 64], 1, dt=BF16)
            U0 = mk('U0', [64, 256])
            KtT = mk('KtT', [128, 2, 64])
            U = mk('U', [64, 256], 1, dt=BF16)

        def chunk_of(dd, i):
            if dd == 0:
                return i
            return 3 - i if i < 4 else 71 - i

        for i in range(nsteps):
            b = i % 2
            cks = [chunk_of(0, i), chunk_of(1, i)]
            DD = (0, 1)
            for dd in DD:
                r0 = cks[dd] * 64
                lwc = cols['lwf'] if dd == 0 else cols['lwb']
                kc = cols['kf'] if dd == 0 else cols['kb']
                s.dma(T['LW'][dd][b], SC[r0:r0 + 64, lwc:lwc + 256])
                s.dma(T['R'][dd][b], SC[r0:r0 + 64, cols['r']:cols['r'] + 256])
                s.dma(T['K'][dd][b], SC[r0:r0 + 64, kc:kc + 256])
                s.dma(T['V'][dd][b], SC[r0:r0 + 64, cols['v']:cols['v'] + 256])
                if delta:
                    s.dma(T['KK'][dd][b], SC[r0:r0 + 64, cols['kk']:cols['kk'] + 256])
                    s.dma(T['B'][dd][b], SC[r0:r0 + 64, cols['b']:cols['b'] + 256])
            yield
            for dd in DD:
                LW = T['LW'][dd][b]
                pE = nb()
                for hp in range(2):
                    s.mm(pE[:, hp * 130:(hp + 1) * 130], LW[:, hp * 128:(hp + 1) * 128], K_[:, dd, 0:130], inc=(hp == 1))
                pEv = pE[:, 0:260].rearrange("p (h n) -> p h n", h=2)
                s.act(eP[dd][b], pEv[:, :, 0:128], AF.Exp)
                s.act(eM[dd][b], pEv[:, :, 0:64], AF.Exp, scale=-1.0)
                s.act(eS[dd][b], pEv[:, :, 128:130], AF.Exp)
                pT = nb()
                s.mm(pT[0:64, 0:256], K_[:, dd, 130:194], LW, inc=False)
                s.mm(pT[0:64, 256:512], K_[:, dd, 194:258], LW)
                s.act(eT[dd][b], pT[0:64, :], AF.Exp)
            yield
            for dd in DD:
                pR = nb()
                srcs = ['R', 'K'] + (['KK', 'B'] if delta else [])
                for si, nm in enumerate(srcs):
                    for hp in range(2):
                        last = (si == len(srcs) - 1 and hp == 1)
                        s.tr(pR[:, (si * 2 + hp) * 64:(si * 2 + hp + 1) * 64], T[nm][dd][b][:, hp * 128:(hp + 1) * 128],
                             C.ident[0:64, 0:64], inc=last)
                pv = pR.rearrange("p (a h n) -> p a h n", a=4, h=2)
                s.tt(fm['rTm'][dd][b], pv[:, 0], eP[dd][b][:, :, 0:64], ALU.mult)
                s.tt(fm['kTm'][dd][b], pv[:, 1], eM[dd][b], ALU.mult)
                if delta:
                    s.tt(fm['kkTm'][dd][b], pv[:, 2], eP[dd][b][:, :, 64:128], ALU.mult)
                    s.tt(fm['bTm'][dd][b], pv[:, 3], eM[dd][b], ALU.mult)
                s.tt(ePin[dd][b], eP[dd][b][:, :, 0:64], eS[dd][b][:, :, 0:1].to_broadcast([128, 2, 64]), ALU.mult)
                s.tt(fm['rTin'][dd][b], pv[:, 0], ePin[dd][b], ALU.mult)
                s.copy(Vb[dd][b], T['V'][dd][b], eng='pool')
                s.tt(kout[dd][b], T['K'][dd][b], eT[dd][b][:, 0:256], ALU.mult, eng='pool')
                if delta:
                    s.tt(bout[dd][b], T['B'][dd][b], eT[dd][b][:, 0:256], ALU.mult, eng='pool')
                    s.tt(kkin[dd][b], T['KK'][dd][b], eT[dd][b][:, 256:512], ALU.mult, eng='pool')
            yield
            for dd in DD:
                mInc = K_[:, dd, 258:322].unsqueeze(1).to_broadcast([64, 4, 64])
                mStr = K_[:, dd, 322:386].unsqueeze(1).to_broadcast([64, 4, 64])
                mStrT = K_[:, dd, 386:450].unsqueeze(1).to_broadcast([64, 4, 64])

                def amat(dst, lname, rname, mask, eng='dve', quad=False):
                    pX, pY = nb(), nb()
                    for hp in range(2):
                        for par, pA in ((0, pX), (1, pY)):
                            pr = 64 * par
                            L = fm[lname][dd][b][pr:pr + 64, hp, :]
                            R_ = fm[rname][dd][b][pr:pr + 64, hp, :]
                            o_ = pA[0:64, hp * 64:(hp + 1) * 64]
                            if not quad:
                                s.mm(o_, L, R_, inc=(hp == 1))
                            elif dd == 0:
                                s.mm(o_[0:32, :], L[:, 0:32], R_, inc=False)
                                s.mm(o_[32:64, 32:64], L[:, 32:64], R_[:, 32:64], inc=(hp == 1))
                            else:
                                s.mm(o_[32:64, :], L[:, 32:64], R_, inc=False)
                                s.mm(o_[0:32, 0:32], L[:, 0:32], R_[:, 0:32], inc=(hp == 1))
                    for par, pA in ((0, pX), (1, pY)):
                        dv = dst[:, 2 * par:2 * par + 2, :]
                        pv_ = pA[0:64, 0:128].rearrange("p (h n) -> p h n", h=2)
                        mk_ = mask[:, 0:2, :]
                        if not quad:
                            s.tt(dv, pv_, mk_, ALU.mult, eng=eng)
                        elif dd == 0:
                            s.tt(dv[0:32], pv_[0:32], mk_[0:32], ALU.mult, eng=eng)
                            s.tt(dv[32:64, :, 32:64], pv_[32:64, :, 32:64], mk_[32:64, :, 32:64], ALU.mult, eng=eng)
                        else:
                            s.tt(dv[32:64], pv_[32:64], mk_[32:64], ALU.mult, eng=eng)
                            s.tt(dv[0:32, :, 0:32], pv_[0:32, :, 0:32], mk_[0:32, :, 0:32], ALU.mult, eng=eng)
                amat(ArkT[dd][b], 'kTm', 'rTm', mInc, quad=not delta)
                if delta:
                    amat(ArbT[dd][b], 'bTm', 'rTm', mInc)
                    amat(AkkT[dd][b], 'kTm', 'kkTm', mStr)
                    amat(Xq[dd][0], 'bTm', 'kkTm', mStr)
                    amat(Xt[dd][0], 'kkTm', 'bTm', mStrT)
            if delta:
                for dd in DD:
                    s.tt(Pm[dd][0], I4, Xq[dd][0], ALU.subtract, eng='pool')
                for j in range(5):
                    yield
                    a_, b_ = j % 2, (j + 1) % 2
                    for dd in DD:
                        pQ, pQ2 = nb(), nb()
                        for h in range(4):
                            s.mm(pQ[0:64, h * 64:(h + 1) * 64], Xt[dd][a_][:, h, :], Xq[dd][a_][:, h, :], inc=(h == 3))
                        for h in range(4):
                            s.mm(pQ2[0:64, h * 64:(h + 1) * 64], Xq[dd][a_][:, h, :], Xt[dd][a_][:, h, :], inc=(h == 3))
                        s.copy(Xq[dd][b_], pQ[0:64, 0:256].rearrange("p (h n) -> p h n", h=4), eng='act')
                        s.copy(Xt[dd][b_], pQ2[0:64, 0:256].rearrange("p (h n) -> p h n", h=4))
                    for dd in DD:
                        pP = nb()
                        for h in range(4):
                            s.mm(pP[0:64, h * 64:(h + 1) * 64], Xt[dd][b_][:, h, :], Pm[dd][a_][:, h, :], inc=(h == 3))
                        s.tt(Pm[dd][b_], Pm[dd][a_], pP[0:64, 0:256].rearrange("p (h n) -> p h n", h=4), ALU.add)
                MT = [Pm[dd][1] for dd in DD]
                yield
                for dd in DD:
                    pX = nb()
                    for h in range(4):
                        s.mm(pX[0:64, h * 64:(h + 1) * 64], AkkT[dd][b][:, hq(h), :], Vb[dd][b][:, h * 64:(h + 1) * 64], inc=(h == 3))
                    s.copy(X0[dd][0], pX[0:64, 0:256].rearrange("p (h n) -> p h n", h=4), eng='act')
                    pK0, pK1 = nb(), nb()
                    for hp in range(2):
                        for par, pK in ((0, pK0), (1, pK1)):
                            h = 2 * hp + par
                            pr = 64 * par
                            s.mm(pK[pr:pr + 64, hp * 64:(hp + 1) * 64], kkin[dd][b][:, h * 64:(h + 1) * 64], MT[dd][:, hq(h), :], inc=(hp == 1))
                    s.copy(KtT[dd][b][0:64], pK0[0:64, 0:128].rearrange("p (h n) -> p h n", h=2))
                    s.copy(KtT[dd][b][64:128], pK1[64:128, 0:128].rearrange("p (h n) -> p h n", h=2), eng='act')
                for dd in DD:
                    pU = nb()
                    for h in range(4):
                        s.mm(pU[0:64, h * 64:(h + 1) * 64], MT[dd][:, hq(h), :], X0[dd][0][:, h, :], inc=(h == 3))
                    s.ts(U0[dd][b], pU[0:64, 0:256], -1.0, None, ALU.mult)
            yield
            if delta:
                for dd in DD:
                    pU = nb()
                    for hp in range(2):
                        s.mm(pU[0:64, hp * 128:(hp + 1) * 128], KtT[dd][b][:, hp, :], H2[dd][hp], inc=(hp == 1))
                    s.tt(U[dd][0], U0[dd][b], pU[0:64, 0:256], ALU.subtract)
            yield
            for dd in DD:
                pO = nb()
                for hp in range(2):
                    s.mm(pO[0:64, hp * 128:(hp + 1) * 128], fm['rTin'][dd][b][:, hp, :], H2[dd][hp], start=(hp == 0), stop=False)
                for h in range(4):
                    last = (h == 3) and not delta
                    s.mm(pO[0:64, h * 64:(h + 1) * 64], ArkT[dd][b][:, hq(h), :], Vb[dd][b][:, h * 64:(h + 1) * 64],
                         start=False, stop=last, inc=last)
                if delta:
                    for h in range(4):
                        s.mm(pO[0:64, h * 64:(h + 1) * 64], ArbT[dd][b][:, hq(h), :], U[dd][0][:, h * 64:(h + 1) * 64],
                             start=False, stop=(h == 3), inc=(h == 3))
                s.copy(Osb[dd][b], pO[0:64, 0:256], eng='act')
                r0 = cks[dd] * 64
                s.dma(OUTD[dd][r0:r0 + 64, :], Osb[dd][b], writes=[('par', OUTD[dd])], q='pool')
            yield
            for dd in DD:
                pH = nb()
                for hp in range(2):
                    cs_ = slice(hp * 128, (hp + 1) * 128)
                    s.mm(pH[:, cs_], kout[dd][b][:, cs_], T['V'][dd][b][:, cs_], start=True, stop=not delta, inc=(hp == 1 and not delta))
                    if delta:
                        s.mm(pH[:, cs_], bout[dd][b][:, cs_], U[dd][0][:, cs_], start=False, stop=True, inc=(hp == 1))
                for hp in range(2):
                    s.tt(tmpH[dd][0], pH[:, hp * 128:(hp + 1) * 128], BD, ALU.mult)
                    s.stt(H2[dd][hp], H2[dd][hp], eS[dd][b][:, hp, 1:2], tmpH[dd][0], ALU.mult, ALU.add)
            yield


def stage_scans(C, l):
    with Stage(C, f"scans{l}") as t:
        banks_r = [C.ps[0], C.ps[1], C.ps[2], C.ps[3]]
        banks_h = [C.ps[4], C.ps[5], C.psb[0].bitcast(F32), C.psb[1].bitcast(F32)]
        gens = [scan_gen(C, t, 'r', banks_r, C.SCR, RWC, C.ORW, True),
                scan_gen(C, t, 'h', banks_h, C.SCH, HGC, C.OHG, False)]
        if l == 0:
            gens.append(precast_gen(C, t))
        live = list(gens)
        rnd = 0
        while live:
            rnd += 1
            for gi, g in enumerate(list(live)):
                if g is gens[-1] and len(gens) == 3 and len(live) > 1 and rnd % 6 != 0:
                    continue
                try:
                    next(g)
                except StopIteration:
                    live.remove(g)


def stage_hg_read(C, l):
    s, d = C.s, C.d
    with Stage(C, f"hgr{l}") as t:
        NG = bcast_load(C, t, d['hg_norm_g'][l], 256, "ng")
        ofs = [t.sb([128, 256], F32, f"of{i}") for i in range(2)]
        obs = [t.sb([128, 256], F32, f"ob{i}") for i in range(2)]
        gs = [t.sb([128, 256], F32, f"g{i}") for i in range(2)]
        sq = t.sb([128, 256], F32, "sq")
        r4 = t.sb([128, 4], F32, "r4")
        for i in range(NT):
            of, ob, g = ofs[i % 2], obs[i % 2], gs[i % 2]
            r0 = prow(i * 128)
            s.dma(of, C.OHG[0][i * 128:(i + 1) * 128, :])
            s.dma(ob, C.OHG[1][i * 128:(i + 1) * 128, :])
            s.dma(g, C.P[r0:r0 + 128, 1024:1280])
            s.tt(of, of, ob, ALU.add)
            s.tt(sq, of, of, ALU.mult, eng='pool')
            s.reduce(r4, sq.rearrange("p (h n) -> p h n", h=4), ALU.add)
            s.act(r4, r4, AF.Sqrt, bias=EPS, scale=1.0 / 64)
            s.op('dve', lambda e: e.reciprocal(r4, r4), reads=[r4], writes=[r4])
            ov = of.rearrange("p (h n) -> p h n", h=4)
            s.tt(ov, ov, r4.unsqueeze(2).to_broadcast([128, 4, 64]), ALU.mult)
            s.act(g, g, AF.Silu)
            s.tt(of, of, NG, ALU.mult, eng='pool')
            s.tt(of, of, g, ALU.mult)
            s.dma(C.CAT[i * 128:(i + 1) * 128, 0:256], of, writes=[('par', C.CAT)], q='pool')


def stage_rw_read(C, l):
    s, d = C.s, C.d
    with Stage(C, f"rwr{l}") as t:
        LG = bcast_load(C, t, d['rw_ln_g'][l], 256, "lg")
        LB_ = bcast_load(C, t, d['rw_ln_b'][l], 256, "lb")
        ofs = [t.sb([128, 256], F32, f"of{i}") for i in range(2)]
        obs = [t.sb([128, 256], F32, f"ob{i}") for i in range(2)]
        gbs = [t.sb([128, 512], F32, f"gb{i}") for i in range(2)]
        sq = t.sb([128, 256], F32, "sq")
        r4 = t.sb([128, 4], F32, "r4")
        for i in range(NT):
            of, ob, gb = ofs[i % 2], obs[i % 2], gbs[i % 2]
            s.dma(of, C.ORW[0][i * 128:(i + 1) * 128, :])
            s.dma(ob, C.ORW[1][i * 128:(i + 1) * 128, :])
            s.dma(gb, C.SCR[i * 128:(i + 1) * 128, RWC['g']:RWC['g'] + 512])
            s.tt(of, of, ob, ALU.add)
            ov = of.rearrange("p (h n) -> p h n", h=4)
            s.reduce(r4, ov, ALU.add)
            s.ts(r4, r4, 1.0 / 64, None, ALU.mult)
            s.tt(ov, ov, r4.unsqueeze(2).to_broadcast([128, 4, 64]), ALU.subtract)
            s.tt(sq, of, of, ALU.mult, eng='pool')
            s.reduce(r4, sq.rearrange("p (h n) -> p h n", h=4), ALU.add)
            s.act(r4, r4, AF.Sqrt, bias=64e-5, scale=1.0 / 64)
            s.op('dve', lambda e: e.reciprocal(r4, r4), reads=[r4], writes=[r4])
            s.tt(ov, ov, r4.unsqueeze(2).to_broadcast([128, 4, 64]), ALU.mult)
            s.tt(of, of, LG, ALU.mult, eng='pool')
            s.tt(of, of, LB_, ALU.add)
            s.tt(of, of, gb[:, 256:512], ALU.add, eng='pool')
            s.tt(of, of, gb[:, 0:256], ALU.mult)
            s.dma(C.CAT[i * 128:(i + 1) * 128, 256:512], of, writes=[('par', C.CAT)], q='pool')


_NC_CACHE = {}


def kernel(**inputs):
    if 'nc' not in _NC_CACHE:
        _NC_CACHE['nc'] = build()
    nc = _NC_CACHE['nc']
    in_maps = []
    for b in range(4):
        in_maps.append(host_inputs(inputs, b, rev=False))
        in_maps.append(host_inputs(inputs, b, rev=True))
    res = run_bass_kernel_spmd(nc, in_maps, core_ids=list(range(8)))
    out = np.empty((4, 4096, D), np.float32)
    for b in range(4):
        out[b, 0:2048] = np.asarray(res.results[2 * b]['out'], dtype=np.float32)
        out[b, 2048:4096] = np.asarray(res.results[2 * b + 1]['out'], dtype=np.float32)[::-1]
    return out
```

```python
import numpy as np
import concourse.bass as bass
import concourse.mybir as mybir
from concourse.bass_utils import run_bass_kernel_spmd

F32 = mybir.dt.float32
BF16 = mybir.dt.bfloat16
ALU = mybir.AluOpType
AF = mybir.ActivationFunctionType
AX = mybir.AxisListType


class Sched:
    NDMA = 40
    ROT = 20000

    def __init__(self, nc):
        self.nc = nc
        self.e = dict(pe=nc.tensor, act=nc.scalar, dve=nc.vector, pool=nc.gpsimd, sp=nc.sync)
        self.sem = {}
        self.cnt = {}
        self.nsem = {}
        for k in ('pe', 'act', 'dve', 'pool'):
            self.nsem[k] = 0
            self.sem[k] = nc.alloc_semaphore(f"s_{k}_0")
            self.cnt[k] = 0
        self.seen = {k: {} for k in self.e}
        self.dsem = [nc.alloc_semaphore(f"sd{i}") for i in range(self.NDMA)]
        self.dcnt = [0] * self.NDMA
        self.drr = 0
        self.res = {}
        self.ninst = 0
        self.out_evs = {}

    @staticmethod
    def _merge(dst, src):
        for n, (h, v) in src.items():
            if n not in dst or dst[n][1] < v:
                dst[n] = (h, v)

    def _st(self, key):
        s = self.res.get(key)
        if s is None:
            s = dict(w={}, r={}, old={}, phase='w')
            self.res[key] = s
        return s

    def _key(self, x):
        if isinstance(x, str):
            return x, False
        if isinstance(x, tuple):
            return self._key(x[1])[0], True
        return x.name, False

    def _need(self, reads, writes):
        need = {}
        for r in reads:
            k, _ = self._key(r)
            self._merge(need, self._st(k)['w'])
            if k.startswith('ps'):
                self._merge(need, self._st(k)['r'])
        for w in writes:
            k, par = self._key(w)
            s = self._st(k)
            if par:
                if s['phase'] == 'r':
                    old = {}
                    self._merge(old, s['w'])
                    self._merge(old, s['r'])
                    s['old'] = old
                    s['w'] = {}
                    s['r'] = {}
                    s['phase'] = 'w'
                self._merge(need, s['old'])
            else:
                self._merge(need, s['w'])
                self._merge(need, s['r'])
                self._merge(need, s['old'])
        return need

    def _commit(self, reads, writes, ev):
        for r in reads:
            k, _ = self._key(r)
            s = self._st(k)
            self._merge(s['r'], ev)
            s['phase'] = 'r'
        for w in writes:
            k, par = self._key(w)
            s = self._st(k)
            if par:
                self._merge(s['w'], ev)
            else:
                s['w'] = dict(ev)
                s['r'] = {}
                s['old'] = {}
                s['phase'] = 'w'

    def _wait(self, eng, need):
        seen = self.seen[eng]
        for n, (h, v) in need.items():
            if eng == 'pe' and n.startswith('s_pe_'):
                continue
            if seen.get(n, 0) >= v:
                continue
            for k in ('pe', 'act', 'dve', 'pool'):
                if n == f"s_{k}_{self.nsem[k]}":
                    assert v <= self.cnt[k], (eng, n, v, self.cnt[k])
            self.e[eng].wait_ge(h, v)
            self.ninst += 1
            seen[n] = v

    def op(self, eng, fn, reads=(), writes=(), inc=True):
        need = self._need(reads, writes)
        self._wait(eng, need)
        ins = fn(self.e[eng])
        self.ninst += 1
        if inc:
            if self.cnt[eng] >= self.ROT:
                self.nsem[eng] += 1
                self.sem[eng] = self.nc.alloc_semaphore(f"s_{eng}_{self.nsem[eng]}")
                self.cnt[eng] = 0
            self.cnt[eng] += 1
            ins.then_inc(self.sem[eng], 1)
            ev = {self.sem[eng].name: (self.sem[eng], self.cnt[eng])}
        else:
            assert self.cnt[eng] < self.ROT - 64
            ev = {self.sem[eng].name: (self.sem[eng], self.cnt[eng] + 1)}
        self._commit(reads, writes, ev)
        return ev

    def dma(self, out, in_, reads=None, writes=None, q='sp', **kw):
        reads = [in_] if reads is None else reads
        writes = [out] if writes is None else writes
        i = self.drr
        self.drr = (self.drr + 1) % self.NDMA
        h = self.dsem[i]
        need = self._need(reads, writes)
        if self.dcnt[i] > 0:
            self._merge(need, {h.name: (h, 16 * self.dcnt[i])})
        self._wait(q, need)
        self.e[q].dma_start(out=out, in_=in_, **kw).then_inc(h, 16)
        self.ninst += 1
        self.dcnt[i] += 1
        ev = {h.name: (h, 16 * self.dcnt[i])}
        self._commit(reads, writes, ev)
        return ev

    def finish(self, keys, eng='sp'):
        need = {}
        for k in keys:
            kk, _ = self._key(k)
            self._merge(need, self._st(kk)['w'])
        self._wait(eng, need)

    def mm(self, out, lhsT, rhs, start=True, stop=True, inc=None, extra_r=()):
        inc = stop if inc is None else inc
        return self.op('pe', lambda e: e.matmul(out, lhsT, rhs, start=start, stop=stop),
                       reads=[lhsT, rhs, *extra_r], writes=[out], inc=inc)

    def tr(self, out, in_, ident, inc=True):
        return self.op('pe', lambda e: e.transpose(out, in_, ident), reads=[in_, ident], writes=[out], inc=inc)

    def act(self, out, in_, func, bias=None, scale=None, accum_out=None, eng='act'):
        kw = {}
        reads = [in_]
        writes = [out]
        if bias is not None:
            kw['bias'] = bias
            if not isinstance(bias, (int, float)):
                reads.append(bias)
        if scale is not None:
            kw['scale'] = scale
            if not isinstance(scale, (int, float)):
                reads.append(scale)
        if accum_out is not None:
            kw['accum_out'] = accum_out
            writes.append(accum_out)
        return self.op('act', lambda e: e.activation(out, in_, func, **kw), reads=reads, writes=writes)

    def tt(self, out, in0, in1, op, eng='dve'):
        return self.op(eng, lambda e: e.tensor_tensor(out, in0, in1, op), reads=[in0, in1], writes=[out])

    def ts(self, out, in0, s1, s2, op0, op1=None, eng='dve', accum_out=None):
        reads = [in0] + [s for s in (s1, s2) if s is not None and not isinstance(s, (int, float))]
        writes = [out] + ([accum_out] if accum_out is not None else [])
        kw = {}
        if accum_out is not None:
            kw['accum_out'] = accum_out
        if op1 is None:
            return self.op(eng, lambda e: e.tensor_scalar(out, in0, s1, None, op0, **kw), reads=reads, writes=writes)
        return self.op(eng, lambda e: e.tensor_scalar(out, in0, s1, s2, op0, op1, **kw), reads=reads, writes=writes)

    def stt(self, out, in0, scalar, in1, op0, op1, eng='dve'):
        reads = [in0, in1] + ([scalar] if not isinstance(scalar, (int, float)) else [])
        return self.op(eng, lambda e: e.scalar_tensor_tensor(out, in0, scalar, in1, op0, op1), reads=reads, writes=[out])

    def copy(self, out, in_, eng='dve'):
        if eng == 'act':
            return self.op('act', lambda e: e.copy(out, in_), reads=[in_], writes=[out])
        return self.op(eng, lambda e: e.tensor_copy(out, in_), reads=[in_], writes=[out])

    def memset(self, ap, val, eng='dve'):
        return self.op(eng, lambda e: e.memset(ap, val), reads=[], writes=[ap])

    def reduce(self, out, in_, op, eng='dve'):
        return self.op(eng, lambda e: e.tensor_reduce(out, in_, AX.X, op), reads=[in_], writes=[out])


from contextlib import ExitStack

D = 1024
NT = 34
TOK = 4352
INC = 3232
EPS = 1e-6
PROWS = 4356


def prow(t):
    return 1 + t if t < 256 else 3 + t


class Ctx:
    pass


def barrier(C):
    s = C.s
    need = {}
    for k in ('pe', 'act', 'dve', 'pool'):
        if s.cnt[k] > 0:
            need[s.sem[k].name] = (s.sem[k], s.cnt[k])
    for i, h in enumerate(s.dsem):
        if s.dcnt[i] > 0:
            need[h.name] = (h, 16 * s.dcnt[i])
    for eng in ('pe', 'act', 'dve', 'pool', 'sp'):
        seen = s.seen[eng]
        for n, (h, v) in need.items():
            if seen.get(n, 0) >= v:
                continue
            s.e[eng].wait_ge(h, v)
            s.ninst += 1
            seen[n] = v
    s.res = {}


class Stage:
    def __init__(self, C, name):
        self.C = C
        self.name = name
        self.es = ExitStack()
        self.n = 0

    def __enter__(self):
        self.es.__enter__()
        return self

    def sb(self, shape, dtype=F32, name=None):
        self.n += 1
        nm = f"{self.name}_{name or 't'}{self.n}"
        t = self.es.enter_context(self.C.nc.sbuf_tensor(nm, list(shape), dtype))
        return t.ap()

    def __exit__(self, *a):
        barrier(self.C)
        return self.es.__exit__(*a)


def bcast_load(C, st, vec_ap, n, name, q='sp'):
    t = st.sb([128, n], F32, name)
    C.s.dma(t, vec_ap.partition_broadcast(128), q=q)
    return t


def stage_consts(C):
    nc, s = C.nc, C.s
    C.ident = nc.alloc_sbuf_tensor("ident_sb", [128, 128], F32).ap()
    C.identb = nc.alloc_sbuf_tensor("identb", [128, 128], BF16).ap()
    C.zero = nc.alloc_sbuf_tensor("zero_sb", [128, 808], F32).ap()
    C.ones = nc.alloc_sbuf_tensor("ones_sb", [128, 128], F32).ap()
    s.dma(C.ident, C.d['ident'])
    s.copy(C.identb, C.ident)
    s.memset(C.zero, 0.0)
    s.memset(C.ones, 1.0)
    for r in (0, 257, 258, 4355):
        for q4 in range(4):
            s.dma(C.P[r:r + 1, q4 * 808:(q4 + 1) * 808], C.zero[0:1, :], writes=[('par', C.P)], q='pool')
    C.ps = [nc.alloc_psum_tensor(f"ps{i}", [128, 512], F32).ap() for i in range(6)]
    C.psb = [nc.alloc_psum_tensor(f"psb{i}", [128, 1024], BF16).ap() for i in range(2)]
    barrier(C)


def stage_mod(C, l):
    nc, s, d = C.nc, C.s, C.d
    with Stage(C, f"mod{l}") as t:
        MOD = t.sb([128, 2, 6144], F32, "MOD")
        cT = t.sb([128, 16], F32)
        cs = t.sb([128, 16], F32)
        cb = t.sb([128, 16, 128], F32)
        brow = t.sb([1, 6144], F32)
        s.dma(cT, d['cT'])
        s.dma(brow, d['b_ada'][l:l + 1, :])
        s.act(cs, cT, AF.Silu)
        for j in range(16):
            s.copy(cb[:, j, :], cs[:, j:j + 1].to_broadcast([128, 128]), eng='pool' if j % 2 else 'dve')
        wts = [t.sb([128, 8, 512], F32, f"wa{i}") for i in range(2)]
        wv = d['w_ada'][l].rearrange("(c p) n -> p c n", p=128)
        for n in range(12):
            w = wts[n % 2]
            s.dma(w, wv[:, :, n * 512:(n + 1) * 512])
            for r in range(2):
                ps = C.ps[(2 * n + r) % 4]
                for k in range(8):
                    s.mm(ps, cb[:, r * 8 + k, :], w[:, k, :], start=(k == 0), stop=False)
                s.mm(ps, C.ones[0:1, :], brow[0:1, n * 512:(n + 1) * 512], start=False, stop=True)
                s.copy(MOD[:, r, n * 512:(n + 1) * 512], ps, eng='act' if r else 'dve')
        g1 = bcast_load(C, t, d['norm1_g'][l], 1024, "g1")
        g2 = bcast_load(C, t, d['norm2_g'][l], 1024, "g2")
        for r in range(2):
            for (seg, g) in ((1, g1), (4, g2)):
                sl = MOD[:, r, seg * 1024:(seg + 1) * 1024]
                s.stt(sl, sl, 1.0, g, ALU.add, ALU.mult)
        for r in range(2):
            s.dma(C.MODD[l][r:r + 1, :], MOD[0:1, r, :], writes=[('par', C.MODD[l])], q='pool')


def rmsnorm_mod_T(C, st, xt, G, sh, hT, tmp):
    s = C.s
    s.act(tmp['junk'], xt, AF.Square, accum_out=tmp['ss'])
    s.act(tmp['rs'], tmp['ss'], AF.Sqrt, bias=EPS, scale=1.0 / D)
    s.op('dve', lambda e: e.reciprocal(tmp['rs'], tmp['rs']), reads=[tmp['rs']], writes=[tmp['rs']])
    s.stt(tmp['t1'], xt, tmp['rs'], G, ALU.mult, ALU.mult)
    s.tt(tmp['hb'], tmp['t1'], sh, ALU.add, eng='pool')
    pb = C.psb[tmp['i'] % 2]
    tmp['i'] += 1
    for k in range(8):
        s.tr(pb[:, k * 128:(k + 1) * 128], tmp['hb'][:, k * 128:(k + 1) * 128], C.identb, inc=(k == 7))
    s.copy(hT, pb.rearrange("p (c n) -> p c n", c=8), eng='act')


def norm_tmp(st):
    return dict(junk=st.sb([128, 1024], BF16), ss=st.sb([128, 1], F32), rs=st.sb([128, 1], F32),
                t1=st.sb([128, 1024], F32), hb=st.sb([128, 1024], BF16), i=0)


def load_cast_w(C, st, dst_bf, src_rows, stg, q='sp', eng='pool'):
    C.s.dma(stg, src_rows, q=q)
    C.s.copy(dst_bf, stg, eng=eng)


def stage_win(C, l, MODD, xsrc):
    nc, s, d = C.nc, C.s, C.d
    with Stage(C, f"win{l}") as t:
        wb = t.sb([128, 8, INC], BF16, "wb")
        stg = [t.sb([128, INC], F32, f"stg{i}") for i in range(2)]
        for k in range(8):
            load_cast_w(C, t, wb[:, k, :], d['w_in'][l, k * 128:(k + 1) * 128, :], stg[k % 2], eng='pool')
        tmp = norm_tmp(t)
        seg = {(r, sg): bcast_load(C, t, MODD[r, sg * 1024:(sg + 1) * 1024], 1024, f"seg{r}{sg}") for r in (0, 1) for sg in (0, 1)}
        xts = [t.sb([128, 1024], F32, f"x{i}") for i in range(2)]
        hTs = [t.sb([128, 8, 128], BF16, f"hT{i}") for i in range(2)]
        pts = [t.sb([128, INC], F32, f"p{i}") for i in range(2)]
        for i in range(NT):
            r = 0 if i < 2 else 1
            xt, hT, pt = xts[i % 2], hTs[i % 2], pts[i % 2]
            s.dma(xt, xsrc[i * 128:(i + 1) * 128, :])
            rmsnorm_mod_T(C, t, xt, seg[(r, 1)], seg[(r, 0)], hT, tmp)
            for n in range(7):
                c0, c1 = n * 512, min(INC, (n + 1) * 512)
                ps = C.ps[n % 4]
                for k in range(8):
                    s.mm(ps[:, 0:c1 - c0], hT[:, k, :], wb[:, k, c0:c1], start=(k == 0), stop=(k == 7))
                s.copy(pt[:, c0:c1], ps[:, 0:c1 - c0], eng='act' if n % 2 else 'dve')
            r0 = prow(i * 128)
            s.dma(C.P[r0:r0 + 128, :], pt, writes=[('par', C.P)], q='pool')


WSHAPES = dict(
    w_ada=(2, 1024, 6144), b_ada=(2, 6144), norm1_g=(2, 1024), norm2_g=(2, 1024), w_in=(2, 1024, 3232),
    hg_lb=(2, 2, 256), hg_norm_g=(2, 256), rw_mu=(2, 2, 1088), rw_w0=(2, 2, 256), rw_w2=(2, 2, 64, 256),
    rw_a0=(2, 256), rw_a2=(2, 64, 256), rw_g2=(2, 128, 256), rw_k_k=(2, 256), rw_k_a=(2, 256),
    rw_r_k=(2, 4, 64), rw_ln_g=(2, 256), rw_ln_b=(2, 256), swa_sink=(2, 4), mla_qnorm_g=(2, 192),
    mla_wuq=(2, 192, 384), mla_kvnorm_g=(2, 128), mla_wukv=(2, 128, 512), w_out=(2, 1024, 1024),
    ffn_w1=(1, 1024, 2816), ffn_w3=(1, 1024, 2816), ffn_w2=(1, 2816, 1024), moe_router=(1, 1024, 8),
    moe_w1=(1, 8, 1024, 1408), moe_w3=(1, 8, 1024, 1408), moe_w2=(1, 8, 1408, 1024), final_norm_g=(1024,),
)
CSHAPES = dict(ident=(128, 128), cT=(128, 16), cosH=(4096, 32), sinH=(4096, 32), cosM=(4096, 16), sinM=(4096, 16), maskP=(128, 128), maskN=(128, 128), scn=(2, 64, 450), bdm=(128, 128))


def build(upto=None, dbg=(), din=()):
    nc = bass.Bass("TRN2", target_bir_lowering=False)
    C = Ctx()
    C.nc = nc
    C.s = Sched(nc)
    C.d = {}
    C.d['xin'] = nc.dram_tensor("xin", [TOK, D], F32, kind="ExternalInput").ap()
    for k, shp in {**WSHAPES, **CSHAPES}.items():
        C.d[k] = nc.dram_tensor(k, list(shp), F32, kind="ExternalInput").ap()
    C.out = nc.dram_tensor("out", [2048, D], F32, kind="ExternalOutput").ap()

    def scratch(name, shape):
        kind = "ExternalOutput" if name in dbg else "Internal"
        return nc.dram_tensor(name, list(shape), F32, kind=kind).ap()
    C.P = scratch("P", [PROWS, INC])
    C.scratch = scratch
    C.MODD = [scratch(f"MODD{l}", [2, 6144]) for l in range(2)]
    C.CAT = scratch("CAT", [TOK, D]) if "CAT" not in din else nc.dram_tensor("CAT", [TOK, D], F32, kind="ExternalInput").ap()
    C.XMID = scratch("XMID", [TOK, D])
    C.XS = scratch("XS", [TOK, D])

    def bscratch(name, shape):
        return nc.dram_tensor(name, list(shape), BF16).ap()
    C.WB = dict(f1=bscratch("WBf1", [1024, 2816]), f3=bscratch("WBf3", [1024, 2816]), f2=bscratch("WBf2", [2816, 1024]),
                m1=[bscratch(f"WBm1_{e}", [1024, 1408]) for e in range(8)],
                m3=[bscratch(f"WBm3_{e}", [1024, 1408]) for e in range(8)],
                m2=[bscratch(f"WBm2_{e}", [1408, 1024]) for e in range(8)])
    C.SCH = scratch("SCH", [TOK, HGC['n']])
    C.SCR = scratch("SCR", [TOK, RWC['n']])
    C.OHG = [scratch(f"OHG{i}", [TOK, 256]) for i in range(2)]
    C.ORW = [scratch(f"ORW{i}", [TOK, 256]) for i in range(2)]
    stage_consts(C)
    program(C, upto)
    C.s.finish([C.out] + [n for n in dbg], 'sp')
    print("ninst", C.s.ninst, "sems", {k: v for k, v in C.s.nsem.items()})
    return nc


def program(C, upto):
    if upto in ('ffn', 'moe'):
        with Stage(C, "precast") as t_:
            for _ in precast_gen(C, t_):
                pass
    for l in range(2):
        xsrc = C.d['xin'] if l == 0 else C.XS
        ctx_out = l < 1
        stage_mod(C, l)
        stage_win(C, l, C.MODD[l], xsrc)
        if upto == 'win':
            return
        if upto in (None, 'scans'):
            stage_hg_prep(C, l)
            stage_rw_prep(C, l)
            stage_scans(C, l)
            stage_hg_read(C, l)
            stage_rw_read(C, l)
            if upto == 'scans':
                return
        if upto in (None, 'attn'):
            stage_swa(C, l, ctx_out)
            stage_mla(C, l, ctx_out)
            if upto == 'attn':
                return
        stage_out_ffn(C, l, C.MODD[l], xsrc, moe=(l % 2 == 1))
        if upto == 'ffn':
            return
        if upto == 'L0':
            return
    stage_final(C)


def host_inputs(inputs, b, rev=False):
    m = {}
    cx, xx = np.asarray(inputs['ctx'][b], np.float32), np.asarray(inputs['x'][b], np.float32)
    if rev:
        cx, xx = cx[::-1], xx[::-1]
    m['xin'] = np.ascontiguousarray(np.concatenate([cx, xx], axis=0), dtype=np.float32)
    cc = np.stack([np.asarray(inputs['c_ctx'], np.float32), np.asarray(inputs['c'][b], np.float32)], 0)
    m['cT'] = np.ascontiguousarray(cc.reshape(2, 8, 128).transpose(2, 0, 1).reshape(128, 16))
    m['ident'] = np.eye(128, dtype=np.float32)
    m.update(rope_consts())
    m.update(scan_consts())
    for k in WSHAPES:
        m[k] = np.ascontiguousarray(inputs[k], dtype=np.float32)
    if rev:
        for k in ('cosH', 'sinH', 'cosM', 'sinM'):
            m[k] = np.ascontiguousarray(m[k][::-1])
        perm = np.arange(INC)
        perm[256:512], perm[512:768] = np.arange(512, 768), np.arange(256, 512)
        o = 1280 + 768
        perm[o:o + 64], perm[o + 64:o + 128] = np.arange(o + 64, o + 128), np.arange(o, o + 64)
        m['w_in'] = np.ascontiguousarray(m['w_in'][:, :, perm])
        mu = m['rw_mu'][:, ::-1, :]
        m['rw_mu'] = np.ascontiguousarray(mu[:, :, perm[1280:2368] - 1280])
        m['hg_lb'] = np.ascontiguousarray(m['hg_lb'][:, ::-1, :])
        m['rw_w0'] = np.ascontiguousarray(m['rw_w0'][:, ::-1, :])
        m['rw_w2'] = np.ascontiguousarray(m['rw_w2'][:, ::-1, :, :])
    return m


def precast_gen(C, t):
    nc, s, d = C.nc, C.s, C.d
    jobs = []
    for nm, src in (('f1', d['ffn_w1'][0]), ('f3', d['ffn_w3'][0]), ('f2', d['ffn_w2'][0])):
        jobs.append((C.WB[nm], src))
    for e in range(8):
        jobs.append((C.WB['m1'][e], d['moe_w1'][0, e]))
        jobs.append((C.WB['m3'][e], d['moe_w3'][0, e]))
        jobs.append((C.WB['m2'][e], d['moe_w2'][0, e]))
    NB = 2
    stg = [t.sb([128, 2816], F32, f"pcs{i}") for i in range(NB)]
    wbf = [t.sb([128, 2816], BF16, f"pcw{i}") for i in range(NB)]
    it = 0
    for dst, src in jobs:
        rows, cols = src.shape
        per = max(1, 2816 // cols)
        nchunk = rows // 128
        c0 = 0
        while c0 < nchunk:
            n = min(per, nchunk - c0)
            sg = stg[it % NB]
            wb = wbf[it % NB]
            sv = sg[:, 0:n * cols].rearrange("p (a n) -> p a n", a=n)
            wv = wb[:, 0:n * cols].rearrange("p (a n) -> p a n", a=n)
            s.dma(sv, src[c0 * 128:(c0 + n) * 128, :].rearrange("(a p) n -> p a n", p=128), reads=[src], writes=[sg])
            s.copy(wv, sv, eng='act')
            s.dma(dst[c0 * 128:(c0 + n) * 128, :].rearrange("(a p) n -> p a n", p=128), wv, reads=[wb], writes=[('par', dst)], q='pool')
            c0 += n
            it += 1
            yield


def stage_out_ffn(C, l, MODD, xsrc, moe):
    nc, s, d = C.nc, C.s, C.d
    G = 8
    nlat = 32 if l == 0 else 16
    groups = ([[0, 1]] if l == 0 else []) + [list(range(2 + g * G, 2 + (g + 1) * G)) for g in range(nlat // G)]
    WB = C.WB
    if moe:
        experts = [(WB['m1'][e], WB['m3'][e], WB['m2'][e]) for e in range(8)]
    else:
        experts = [(WB['f1'][:, h * 1408:(h + 1) * 1408], WB['f3'][:, h * 1408:(h + 1) * 1408],
                    WB['f2'][h * 1408:(h + 1) * 1408, :]) for h in range(2)]
    with Stage(C, f"ffn{l}") as t:
        wob = t.sb([128, 8, 1024], BF16, "wob")
        stg = [t.sb([128, 1024], F32, f"stg{i}") for i in range(2)]
        for k in range(8):
            load_cast_w(C, t, wob[:, k, :], d['w_out'][l, k * 128:(k + 1) * 128, :], stg[k % 2])
        segt = {sg: t.sb([128, 1024], F32, f"seg{sg}") for sg in (2, 3, 4, 5)}
        seg = {(r, sg): segt[sg] for r in (0, 1) for sg in (2, 3, 4, 5)}
        cur_r = [None]
        if moe:
            rt = t.sb([128, 8, 8], F32, "router")
            s.dma(rt, d['moe_router'][0].rearrange("(c p) e -> p c e", p=128))
        tmp = norm_tmp(t)
        cts = [t.sb([128, 1024], F32, f"cat{i}") for i in range(1)]
        xts = [t.sb([128, 1024], F32, f"x{i}") for i in range(1)]
        catb = t.sb([128, 1024], BF16, "catb")
        catT = t.sb([128, 8, 128], BF16, "catT")
        xm = t.sb([128, 1024], F32, "xm")
        h2T = t.sb([128, 8, G * 128], BF16, "h2T")
        Y = t.sb([128, G, 1024], F32, "Y")
        actT = t.sb([128, 11, 512], BF16, "actT")
        sa = [t.sb([128, 512], BF16, f"sa{i}") for i in range(2)]
        w1b = t.sb([128, 8, 1408], BF16, "w1b")
        w3b = t.sb([128, 8, 1408], BF16, "w3b")
        w2b = t.sb([128, 11, 1024], BF16, "w2b")
        comb = t.sb([128, G, 8], F32, "comb")
        if moe:
            h2f = t.sb([128, 1024], F32, "h2f")
            h2Tf = t.sb([128, 8, 128], F32, "h2Tf")
            lg = t.sb([128, 8], F32, "lg")
            mx = t.sb([128, 8], F32, "mx")
            nv1 = t.sb([128, 1], F32, "nv1")
            msk = t.sb([128, 8], F32, "msk")
            ex = t.sb([128, 8], F32, "ex")
            sm = t.sb([128, 1], F32, "sm")
        ti = 0
        for grp in groups:
            r = 0 if grp[0] < 2 else 1
            ng = len(grp)
            if cur_r[0] != r:
                cur_r[0] = r
                for sg in (2, 3, 4, 5):
                    s.dma(segt[sg], MODD[r, sg * 1024:(sg + 1) * 1024].partition_broadcast(128))
            for j, i in enumerate(grp):
                ct, xt = cts[0], xts[0]
                ti += 1
                s.dma(ct, C.CAT[i * 128:(i + 1) * 128, :])
                s.dma(xt, xsrc[i * 128:(i + 1) * 128, :])
                s.copy(catb, ct, eng='pool')
                pb = C.psb[tmp['i'] % 2]
                tmp['i'] += 1
                for k in range(8):
                    s.tr(pb[:, k * 128:(k + 1) * 128], catb[:, k * 128:(k + 1) * 128], C.identb, inc=(k == 7))
                s.copy(catT, pb.rearrange("p (c n) -> p c n", c=8), eng='act')
                for hf in range(2):
                    ps = C.ps[hf]
                    for k in range(8):
                        s.mm(ps, catT[:, k, :], wob[:, k, hf * 512:(hf + 1) * 512], start=(k == 0), stop=(k == 7))
                    s.tt(xm[:, hf * 512:(hf + 1) * 512], ps, seg[(r, 2)][:, hf * 512:(hf + 1) * 512], ALU.mult)
                s.tt(xm, xm, xt, ALU.add, eng='pool')
                s.dma(C.XMID[i * 128:(i + 1) * 128, :], xm, writes=[('par', C.XMID)], q='pool')
                rmsnorm_mod_T(C, t, xm, seg[(r, 4)], seg[(r, 3)], h2T[:, :, j * 128:(j + 1) * 128], tmp)
                if moe:
                    s.tt(h2f, tmp['t1'], seg[(r, 3)], ALU.add)
                    for hf in range(2):
                        ps = C.ps[2 + hf]
                        for k in range(4):
                            kk = hf * 4 + k
                            s.tr(ps[:, k * 128:(k + 1) * 128], h2f[:, kk * 128:(kk + 1) * 128], C.ident, inc=(k == 3))
                        s.copy(h2Tf[:, hf * 4:(hf + 1) * 4, :], ps.rearrange("p (c n) -> p c n", c=4), eng='act')
                    ps = C.ps[4]
                    for k in range(8):
                        s.mm(ps[:, 0:8], h2Tf[:, k, :], rt[:, k, :], start=(k == 0), stop=(k == 7))
                    s.copy(lg, ps[:, 0:8])
                    s.op('dve', lambda e: e.max(out=mx, in_=lg), reads=[lg], writes=[mx])
                    s.ts(nv1, mx[:, 0:1], -1.0, None, ALU.mult)
                    s.ts(msk, lg, mx[:, 1:2], None, ALU.is_ge)
                    s.act(ex, lg, AF.Exp, bias=nv1)
                    s.tt(ex, ex, msk, ALU.mult)
                    s.reduce(sm, ex, ALU.add)
                    s.op('dve', lambda e: e.reciprocal(sm, sm), reads=[sm], writes=[sm])
                    s.ts(comb[:, j, :], ex, sm, None, ALU.mult)
            nh = (ng * 128 + 511) // 512
            for ei, (w1, w3, w2) in enumerate(experts):
                s.dma(w1b, w1.rearrange("(k p) n -> p k n", p=128))
                s.dma(w3b, w3.rearrange("(k p) n -> p k n", p=128))
                s.dma(w2b, w2.rearrange("(k p) n -> p k n", p=128))
                for hf in range(nh):
                    t0, t1 = hf * 512, min(ng * 128, (hf + 1) * 512)
                    for c in range(11):
                        pa, pbk = C.ps[(2 * c) % 4], C.ps[(2 * c + 1) % 4]
                        for k in range(8):
                            s.mm(pa[:, 0:t1 - t0], w1b[:, k, c * 128:(c + 1) * 128], h2T[:, k, t0:t1], start=(k == 0), stop=(k == 7))
                        for k in range(8):
                            s.mm(pbk[:, 0:t1 - t0], w3b[:, k, c * 128:(c + 1) * 128], h2T[:, k, t0:t1], start=(k == 0), stop=(k == 7))
                        sx = sa[c % 2]
                        s.act(sx[:, 0:t1 - t0], pa[:, 0:t1 - t0], AF.Silu)
                        s.tt(actT[:, c, 0:t1 - t0], sx[:, 0:t1 - t0], pbk[:, 0:t1 - t0], ALU.mult)
                    for j in range(hf * 4, min(ng, hf * 4 + 4)):
                        jl = j - hf * 4
                        for h2_ in range(2):
                            ps = C.ps[4 + (2 * j + h2_) % 2]
                            for c in range(11):
                                s.mm(ps, actT[:, c, jl * 128:(jl + 1) * 128], w2b[:, c, h2_ * 512:(h2_ + 1) * 512], start=(c == 0), stop=(c == 10))
                            ys = Y[:, j, h2_ * 512:(h2_ + 1) * 512]
                            if moe:
                                if ei == 0:
                                    s.ts(ys, ps, comb[:, j, ei:ei + 1], None, ALU.mult)
                                else:
                                    s.stt(ys, ps, comb[:, j, ei:ei + 1], ys, ALU.mult, ALU.add)
                            else:
                                if ei == 0:
                                    s.copy(ys, ps, eng='act')
                                else:
                                    s.tt(ys, ys, ps, ALU.add)
            for j, i in enumerate(grp):
                xt = xts[0]
                ti += 1
                s.dma(xt, C.XMID[i * 128:(i + 1) * 128, :])
                s.tt(Y[:, j, :], Y[:, j, :], seg[(r, 5)], ALU.mult, eng='pool')
                s.tt(Y[:, j, :], Y[:, j, :], xt, ALU.add)
                s.dma(C.XS[i * 128:(i + 1) * 128, :], Y[:, j, :], writes=[('par', C.XS)], q='pool')


def stage_final(C):
    s, d = C.s, C.d
    with Stage(C, "final") as t:
        g = bcast_load(C, t, d['final_norm_g'], 1024, "fg")
        xts = [t.sb([128, 1024], F32, f"x{i}") for i in range(2)]
        junk = t.sb([128, 1024], BF16)
        ss = t.sb([128, 1], F32)
        rs = t.sb([128, 1], F32)
        for i in range(16):
            xt = xts[i % 2]
            s.dma(xt, C.XS[(i + 2) * 128:(i + 3) * 128, :])
            s.act(junk, xt, AF.Square, accum_out=ss)
            s.act(rs, ss, AF.Sqrt, bias=EPS, scale=1.0 / D)
            s.op('dve', lambda e: e.reciprocal(rs, rs), reads=[rs], writes=[rs])
            s.stt(xt, xt, rs, g, ALU.mult, ALU.mult)
            s.dma(C.out[i * 128:(i + 1) * 128, :], xt, writes=[('par', C.out)], q='pool')


def rope_tm(C, out, x, cos, sin, H, n, ta, tb):
    s = C.s
    xv = x.rearrange("p (h a f n) -> p h a f n", h=H, a=2, f=2)
    ov = out.rearrange("p (h a f n) -> p h a f n", h=H, a=2, f=2)
    x1, x2 = xv[:, :, :, 0, :], xv[:, :, :, 1, :]
    cb = cos.rearrange("p (a n) -> p a n", a=2).unsqueeze(1).to_broadcast([128, H, 2, n])
    sb_ = sin.rearrange("p (a n) -> p a n", a=2).unsqueeze(1).to_broadcast([128, H, 2, n])
    tav = ta[:, 0:H * 2 * n].rearrange("p (h a n) -> p h a n", h=H, a=2)
    tbv = tb[:, 0:H * 2 * n].rearrange("p (h a n) -> p h a n", h=H, a=2)
    s.tt(tav, x1, cb, ALU.mult)
    s.tt(tbv, x2, sb_, ALU.mult, eng='pool')
    s.tt(ov[:, :, :, 0, :], tav, tbv, ALU.subtract)
    s.tt(tav, x1, sb_, ALU.mult)
    s.tt(tbv, x2, cb, ALU.mult, eng='pool')
    s.tt(ov[:, :, :, 1, :], tav, tbv, ALU.add)


def kmax_bcast(C, t, KS, nkm):
    s = C.s
    m1 = t.sb([128, 1], F32)
    row = t.sb([1, 128], F32)
    v = t.sb([1, 1], F32)
    s.reduce(m1, KS, ALU.max)
    ps = C.ps[5]
    s.tr(ps[0:1, 0:128], m1, C.ident)
    s.copy(row, ps[0:1, 0:128])
    s.reduce(v, row, ALU.max)
    s.act(v, v, AF.Sqrt)
    s.ts(v, v, -1.0, None, ALU.mult)
    s.mm(ps[:, 0:1], C.ones[0:1, :], v[0:1, 0:1])
    s.copy(nkm, ps[:, 0:1])


def attend(C, OT, ncols, q_rhs, blocks, scale, PTs, cnt, tail=None):
    s = C.s
    nb = len(blocks)
    LOOK = 2
    pss = {}

    def emit_scores(bi):
        ps = C.ps[(cnt[0] + bi) % 3]
        s.mm(ps[:, 0:ncols], blocks[bi][0], q_rhs)
        pss[bi] = ps
    for bi in range(min(LOOK, nb)):
        emit_scores(bi)
    for bi, (kT, vA, mask) in enumerate(blocks):
        if bi + LOOK < nb:
            emit_scores(bi + LOOK)
        ps = pss.pop(bi)
        PT = PTs[(cnt[0] + bi) % len(PTs)]
        s.act(PT[:, 0:ncols], ps[:, 0:ncols], AF.Exp, scale=scale)
        if mask is not None:
            nrep = ncols // 128
            pv = PT[:, 0:ncols].rearrange("p (r n) -> p r n", r=nrep)
            s.tt(pv, pv, mask.unsqueeze(1).to_broadcast([128, nrep, 128]), ALU.mult, eng='pool')
        s.mm(OT[0:65, 0:ncols], vA, PT[:, 0:ncols], start=(bi == 0), stop=(bi == nb - 1 and tail is None))
    cnt[0] += nb
    if tail is not None:
        tail()


def stage_swa(C, l, ctx_out):
    nc, s, d = C.nc, C.s, C.d
    scale = 64 ** -0.5
    with Stage(C, f"swa{l}") as t:
        KT = t.sb([65, 2, TOK], BF16, "KT")
        VA = t.sb([128, NT, 2, 65], BF16, "VA")
        QT = t.sb([65, 4, TOK], BF16, "QT")
        QR = t.sb([128, NT, 4, 65], BF16, "QR")
        SSQ = t.sb([128, NT, 4], F32, "SSQ")
        KS = t.sb([128, NT], F32, "KS")
        nkm = t.sb([128, 1], F32, "nkm")
        mP = t.sb([128, 128], BF16, "mP")
        mN = t.sb([128, 128], BF16, "mN")
        mf = t.sb([128, 128], F32, "mf")
        s.dma(mf, d['maskP'])
        s.copy(mP, mf)
        s.dma(mf, d['maskN'])
        s.copy(mN, mf)
        SINK = t.sb([65, 4], F32, "SINK")
        s.dma(SINK[64:65, :], d['swa_sink'][l:l + 1, :])
        E64 = t.sb([65, 65], BF16, "E64")
        s.memset(E64, 0.0)
        s.memset(E64[64:65, 64:65], 1.0)
        es = t.sb([65, 256], BF16, "es")
        s.memset(VA[:, :, :, 64:65], 1.0)
        kaug = t.sb([128, 2, 65], BF16, "kaug")
        s.memset(kaug[:, :, 64:65], 1.0)
        pqs = [t.sb([128, 512], F32, f"pq{i}") for i in range(2)]
        cs = [t.sb([128, 32], F32, f"cos{i}") for i in range(2)]
        sn = [t.sb([128, 32], F32, f"sin{i}") for i in range(2)]
        rot = t.sb([128, 384], F32, "rot")
        ta = t.sb([128, 384], F32, "ta")
        tb = t.sb([128, 384], F32, "tb")
        ssk = t.sb([128, 6], F32, "ssk")
        for i in range(NT):
            pq = pqs[i % 2]
            r0 = prow(i * 128)
            s.dma(pq, C.P[r0:r0 + 128, 2368:2880])
            if i >= 2:
                s.dma(cs[i % 2], d['cosH'][(i - 2) * 128:(i - 1) * 128, :])
                s.dma(sn[i % 2], d['sinH'][(i - 2) * 128:(i - 1) * 128, :])
                rope_tm(C, rot, pq[:, 0:384], cs[i % 2], sn[i % 2], 6, 16, ta, tb)
                src = rot
            else:
                src = pq[:, 0:384]
            s.tt(ta, pq[:, 0:384], pq[:, 0:384], ALU.mult)
            s.reduce(ssk, ta.rearrange("p (h n) -> p h n", h=6), ALU.add)
            s.copy(SSQ[:, i, :], ssk[:, 0:4], eng='pool')
            s.reduce(KS[:, i:i + 1], ssk[:, 4:6], ALU.max)
            s.copy(QR[:, i, :, 0:64], src[:, 0:256].rearrange("p (h n) -> p h n", h=4), eng='pool')
            s.copy(kaug[:, :, 0:64], src[:, 256:384].rearrange("p (h n) -> p h n", h=2))
            s.copy(VA[:, i, :, 0:64], pq[:, 384:512].rearrange("p (h n) -> p h n", h=2), eng='pool')
            pb = C.psb[i % 2]
            for j in range(2):
                s.tr(pb[0:65, j * 128:(j + 1) * 128], kaug[:, j, :], C.identb, inc=(j == 1))
            s.copy(KT[:, :, i * 128:(i + 1) * 128], pb[0:65, 0:256].rearrange("p (j n) -> p j n", j=2), eng='act')
        kmax_bcast(C, t, KS, nkm)
        nq = t.sb([128, 4], F32, "nq")
        for i in range(NT):
            if i < 2 and not ctx_out:
                continue
            s.act(nq, SSQ[:, i, :], AF.Sqrt)
            s.ts(QR[:, i, :, 64], nq, nkm, None, ALU.mult)
            pb = C.psb[i % 2]
            for h in range(4):
                s.tr(pb[0:65, h * 128:(h + 1) * 128], QR[:, i, h, :], C.identb, inc=(h == 3))
            s.copy(QT[:, :, i * 128:(i + 1) * 128], pb[0:65, 0:512].rearrange("p (j n) -> p j n", j=4), eng='act')
        PTs = [t.sb([128, 256], BF16, f"PT{i}") for i in range(3)]
        osb = t.sb([65, 256], F32, "osb")
        rden = t.sb([128, 2], F32, "rden")
        otile = [t.sb([128, 256], F32, f"ot{i}") for i in range(2)]
        cnt = [0]
        qblocks = ([0, 1] if ctx_out else []) + list(range(2, NT if l == 0 else 18))
        for qi, n in enumerate(qblocks):
            ot = otile[qi % 2]
            for j in range(2):
                if n < 2:
                    kbs = [(0, None), (1, None)]
                else:
                    kbs = [(0, None), (1, None)]
                    if n - 1 >= 2:
                        kbs.append((n - 1, mP))
                    kbs.append((n, None))
                    if n + 1 < NT:
                        kbs.append((n + 1, mN))
                blocks = [(KT[0:65, j, kb * 128:(kb + 1) * 128], VA[:, kb, j, :], m) for kb, m in kbs]
                OT = C.ps[3 + (qi * 2 + j) % 2]
                q_rhs = QT[0:65, 2 * j:2 * j + 2, n * 128:(n + 1) * 128]

                def tail(OT=OT, j=j, n=n):
                    for hh in range(2):
                        s.act(es[64:65, hh * 128:(hh + 1) * 128], QT[64:65, 2 * j + hh, n * 128:(n + 1) * 128], AF.Exp,
                              bias=SINK[64:65, 2 * j + hh:2 * j + hh + 1], scale=scale)
                    s.mm(OT[0:65, 0:256], E64[64:65, :], es[64:65, :], start=False, stop=True)
                attend(C, OT, 256, q_rhs, blocks, scale, PTs, cnt, tail)
                s.copy(osb, OT[0:65, 0:256])
                ps = C.ps[5]
                for hh in range(2):
                    s.tr(ps[:, hh * 65:(hh + 1) * 65], osb[:, hh * 128:(hh + 1) * 128], C.ident[0:65, 0:65], inc=(hh == 1))
                pv = ps[:, 0:130].rearrange("p (h n) -> p h n", h=2)
                s.op('dve', lambda e, pv=pv: e.reciprocal(rden, pv[:, :, 64]), reads=[ps], writes=[rden])
                s.tt(ot[:, j * 128:(j + 1) * 128].rearrange("p (h n) -> p h n", h=2), pv[:, :, 0:64],
                     rden.unsqueeze(2).to_broadcast([128, 2, 64]), ALU.mult)
            s.dma(C.CAT[n * 128:(n + 1) * 128, 512:768], ot, writes=[('par', C.CAT)], q='pool')


def stage_mla(C, l, ctx_out):
    nc, s, d = C.nc, C.s, C.d
    scale = 96 ** -0.5
    with Stage(C, f"mla{l}") as t:
        KT = t.sb([97, 4, TOK], BF16, "KT")
        VA = t.sb([128, NT, 4, 65], BF16, "VA")
        QT = t.sb([97, 4, TOK], BF16, "QT")
        QR = t.sb([128, NT, 4, 97], BF16, "QR")
        SSQ = t.sb([128, NT, 4], F32, "SSQ")
        KS = t.sb([128, NT], F32, "KS")
        nkm = t.sb([128, 1], F32, "nkm")
        s.memset(VA[:, :, :, 64:65], 1.0)
        kaug = t.sb([128, 4, 97], BF16, "kaug")
        s.memset(kaug[:, :, 96:97], 1.0)
        stg = t.sb([128, 512], F32, "stg")
        wq0 = t.sb([128, 384], BF16, "wq0")
        wq1 = t.sb([64, 384], BF16, "wq1")
        wkv = t.sb([128, 512], BF16, "wkv")
        s.dma(stg[:, 0:384], d['mla_wuq'][l, 0:128, :])
        s.copy(wq0, stg[:, 0:384])
        s.dma(stg[0:64, 0:384], d['mla_wuq'][l, 128:192, :])
        s.copy(wq1, stg[0:64, 0:384])
        s.dma(stg, d['mla_wukv'][l])
        s.copy(wkv, stg)
        gq = bcast_load(C, t, d['mla_qnorm_g'][l], 192, "gq")
        gkv = bcast_load(C, t, d['mla_kvnorm_g'][l], 128, "gkv")
        pms = [t.sb([128, 352], F32, f"pm{i}") for i in range(2)]
        cs = [t.sb([128, 16], F32, f"cos{i}") for i in range(2)]
        sn = [t.sb([128, 16], F32, f"sin{i}") for i in range(2)]
        junk = t.sb([128, 192], F32, "junk")
        ss2 = t.sb([128, 2], F32, "ss2")
        cn = t.sb([128, 320], BF16, "cn")
        cT = t.sb([128, 3, 128], BF16, "cT")
        qf = t.sb([128, 384], F32, "qf")
        kvf = t.sb([128, 512], F32, "kvf")
        qr_in = t.sb([128, 128], F32, "qr_in")
        qr_out = t.sb([128, 128], F32, "qr_out")
        kr_out = t.sb([128, 32], F32, "kr_out")
        ta = t.sb([128, 64], F32, "ta")
        tb = t.sb([128, 64], F32, "tb")
        sq = t.sb([128, 512], F32, "sq")
        s4 = t.sb([128, 4], F32, "s4")
        s4b = t.sb([128, 4], F32, "s4b")
        s1 = t.sb([128, 1], F32, "s1")
        for i in range(NT):
            pm = pms[i % 2]
            r0 = prow(i * 128)
            s.dma(pm, C.P[r0:r0 + 128, 2880:3232])
            s.act(junk[:, 0:192], pm[:, 0:192], AF.Square, accum_out=ss2[:, 0:1])
            s.act(junk[:, 0:128], pm[:, 192:320], AF.Square, accum_out=ss2[:, 1:2])
            s.act(ss2[:, 0:1], ss2[:, 0:1], AF.Sqrt, bias=EPS, scale=1.0 / 192)
            s.act(ss2[:, 1:2], ss2[:, 1:2], AF.Sqrt, bias=EPS, scale=1.0 / 128)
            s.op('dve', lambda e: e.reciprocal(ss2, ss2), reads=[ss2], writes=[ss2])
            s.stt(cn[:, 0:192], pm[:, 0:192], ss2[:, 0:1], gq, ALU.mult, ALU.mult)
            s.stt(cn[:, 192:320], pm[:, 192:320], ss2[:, 1:2], gkv, ALU.mult, ALU.mult)
            pb = C.psb[i % 2]
            s.tr(pb[:, 0:128], cn[:, 0:128], C.identb, inc=False)
            s.tr(pb[0:64, 128:256], cn[:, 128:192], C.identb, inc=False)
            s.tr(pb[:, 256:384], cn[:, 192:320], C.identb)
            s.copy(cT, pb[:, 0:384].rearrange("p (c n) -> p c n", c=3), eng='act')
            pq, pk = C.ps[0], C.ps[1]
            s.mm(pq[:, 0:384], cT[:, 0, :], wq0, start=True, stop=False)
            s.mm(pq[:, 0:384], cT[0:64, 1, :], wq1, start=False, stop=True)
            s.mm(pk, cT[:, 2, :], wkv)
            s.copy(qf, pq[:, 0:384], eng='act')
            s.copy(kvf, pk)
            qv = qf.rearrange("p (h n) -> p h n", h=4)
            kvv = kvf.rearrange("p (h n) -> p h n", h=4)
            s.tt(sq[:, 0:384], qf, qf, ALU.mult, eng='pool')
            s.reduce(SSQ[:, i, :], sq[:, 0:384].rearrange("p (h n) -> p h n", h=4), ALU.add)
            s.tt(sq, kvf, kvf, ALU.mult, eng='pool')
            s.reduce(s4, sq.rearrange("p (h n) -> p h n", h=4)[:, :, 0:64], ALU.add)
            s.tt(ta[:, 0:32], pm[:, 320:352], pm[:, 320:352], ALU.mult)
            s.reduce(s1, ta[:, 0:32], ALU.add)
            s.ts(s4b, s4, s1, None, ALU.add)
            s.reduce(KS[:, i:i + 1], s4b, ALU.max)
            if i >= 2:
                s.dma(cs[i % 2], d['cosM'][(i - 2) * 128:(i - 1) * 128, :])
                s.dma(sn[i % 2], d['sinM'][(i - 2) * 128:(i - 1) * 128, :])
                s.copy(qr_in.rearrange("p (h n) -> p h n", h=4), qv[:, :, 64:96], eng='pool')
                rope_tm(C, qr_out, qr_in, cs[i % 2], sn[i % 2], 4, 8, ta, tb)
                rope_tm(C, kr_out, pm[:, 320:352], cs[i % 2], sn[i % 2], 1, 8, ta, tb)
                qr_src = qr_out.rearrange("p (h n) -> p h n", h=4)
                kr_src = kr_out
            else:
                qr_src = qv[:, :, 64:96]
                kr_src = pm[:, 320:352]
            s.copy(QR[:, i, :, 0:64], qv[:, :, 0:64], eng='pool')
            s.copy(QR[:, i, :, 64:96], qr_src, eng='pool')
            s.copy(kaug[:, :, 0:64], kvv[:, :, 0:64])
            s.copy(kaug[:, :, 64:96], kr_src.unsqueeze(1).to_broadcast([128, 4, 32]))
            s.copy(VA[:, i, :, 0:64], kvv[:, :, 64:128], eng='pool')
            pb = C.psb[(i + 1) % 2]
            for h in range(4):
                s.tr(pb[0:97, h * 128:(h + 1) * 128], kaug[:, h, :], C.identb, inc=(h == 3))
            s.copy(KT[:, :, i * 128:(i + 1) * 128], pb[0:97, 0:512].rearrange("p (j n) -> p j n", j=4), eng='act')
        kmax_bcast(C, t, KS, nkm)
        nq = t.sb([128, 4], F32, "nq")
        for i in range(NT):
            if i < 2 and not ctx_out:
                continue
            s.act(nq, SSQ[:, i, :], AF.Sqrt)
            s.ts(QR[:, i, :, 96], nq, nkm, None, ALU.mult)
            pb = C.psb[i % 2]
            for h in range(4):
                s.tr(pb[0:97, h * 128:(h + 1) * 128], QR[:, i, h, :], C.identb, inc=(h == 3))
            s.copy(QT[:, :, i * 128:(i + 1) * 128], pb[0:97, 0:512].rearrange("p (j n) -> p j n", j=4), eng='act')
        PTs = [t.sb([128, 512], BF16, f"PT{i}") for i in range(3)]
        osb = t.sb([65, 512], F32, "osb")
        rden = t.sb([128, 4], F32, "rden")
        otile = [t.sb([128, 4, 256], F32, f"ot{i}") for i in range(2)]
        cnt = [0]
        qtiles = ([(0, 256)] if ctx_out else []) + [(256 + q * 512, 512) for q in range(8 if l == 0 else 4)]
        for qi, (q0, qn) in enumerate(qtiles):
            ot = otile[qi % 2]
            kbs = [0, 1] if q0 < 256 else list(range(NT))
            for h in range(4):
                blocks = [(KT[0:97, h, kb * 128:(kb + 1) * 128], VA[:, kb, h, :], None) for kb in kbs]
                OT = C.ps[3 + (qi * 4 + h) % 2]
                attend(C, OT, qn, QT[0:97, h, q0:q0 + qn], blocks, scale, PTs, cnt)
                s.copy(osb[:, 0:qn], OT[0:65, 0:qn])
                ps = C.ps[5]
                nsub = qn // 128
                for sb_ in range(nsub):
                    s.tr(ps[:, sb_ * 65:(sb_ + 1) * 65], osb[:, sb_ * 128:(sb_ + 1) * 128], C.ident[0:65, 0:65], inc=(sb_ == nsub - 1))
                pv = ps[:, 0:nsub * 65].rearrange("p (h n) -> p h n", h=nsub)
                s.op('dve', lambda e, pv=pv, nsub=nsub: e.reciprocal(rden[:, 0:nsub], pv[:, :, 64]), reads=[ps], writes=[rden])
                s.tt(ot[:, 0:nsub, h * 64:(h + 1) * 64], pv[:, :, 0:64],
                     rden[:, 0:nsub].unsqueeze(2).to_broadcast([128, nsub, 64]), ALU.mult)
            for sb_ in range(qn // 128):
                s.dma(C.CAT[q0 + sb_ * 128:q0 + (sb_ + 1) * 128, 768:1024], ot[:, sb_, :], writes=[('par', C.CAT)], q='pool')


def rope_consts():
    out = {}
    row = np.repeat(np.arange(64, dtype=np.float32), 64)
    col = np.tile(np.arange(64, dtype=np.float32), 64)
    for nm, rot in (('H', 64), ('M', 32)):
        nf = rot // 4
        inv = (np.float32(10000.0) ** (-np.arange(nf, dtype=np.float32) / np.float32(nf))).astype(np.float32)
        ang = np.stack([row[:, None] * inv, col[:, None] * inv], axis=1).astype(np.float32)
        out['cos' + nm] = np.cos(ang).reshape(4096, 2 * nf).astype(np.float32)
        out['sin' + nm] = np.sin(ang).reshape(4096, 2 * nf).astype(np.float32)
    j = np.arange(128)[:, None]
    i = np.arange(128)[None, :]
    out['maskP'] = (j >= i).astype(np.float32)
    out['maskN'] = (j <= i).astype(np.float32)
    return out


HGC = dict(r=0, kf=256, kb=512, v=768, lwf=1024, lwb=1280, n=1536)
RWC = dict(r=0, kf=256, kb=256, v=512, lwf=768, lwb=1024, kk=1280, b=1536, g=1792, bonus=2048, n=2304)
NEG_EXP_HALF = -0.6065306597126334


def stage_hg_prep(C, l):
    s, d = C.s, C.d
    with Stage(C, f"hgp{l}") as t:
        LB = t.sb([128, 512], F32, "LB")
        OML = t.sb([128, 512], F32, "OML")
        if l == 0:
            s.memset(LB, 0.0)
        else:
            a0 = bcast_load(C, t, d['hg_lb'][0].rearrange("a n -> (a n)"), 512, "a0")
            a1 = bcast_load(C, t, d['hg_lb'][1].rearrange("a n -> (a n)"), 512, "a1")
            s.tt(a1, a1, a0, ALU.subtract)
            s.act(LB, a1, AF.Sigmoid)
        s.ts(OML, LB, -1.0, 1.0, ALU.mult, ALU.add)
        pzs = [t.sb([128, 1024], F32, f"pz{i}") for i in range(2)]
        scs = [t.sb([128, HGC['n']], F32, f"sc{i}") for i in range(2)]
        sg = t.sb([128, 512], F32, "sg")
        for i in range(NT):
            pz, sc = pzs[i % 2], scs[i % 2]
            r0 = prow(i * 128)
            s.dma(pz, C.P[r0:r0 + 128, 0:1024])
            s.act(sc[:, 0:256], pz[:, 0:256], AF.Silu)
            s.copy(sc[:, 768:1024], pz[:, 768:1024], eng='pool')
            s.act(sg, pz[:, 256:768], AF.Sigmoid)
            s.tt(sg, sg, OML, ALU.mult)
            s.tt(sg, sg, LB, ALU.add, eng='pool')
            s.ts(sg, sg, 1e-30, None, ALU.max)
            s.ts(sc[:, 256:768], sg, -1.0, 1.0, ALU.mult, ALU.add, eng='pool')
            s.act(sc[:, 1024:1536], sg, AF.Ln)
            s.dma(C.SCH[i * 128:(i + 1) * 128, :], sc, writes=[('par', C.SCH)], q='pool')


def stage_rw_prep(C, l):
    s, d = C.s, C.d
    with Stage(C, f"rwp{l}") as t:
        MU0 = bcast_load(C, t, d['rw_mu'][l, 0], 1088, "mu0")
        MU1 = bcast_load(C, t, d['rw_mu'][l, 1], 1088, "mu1")
        W0 = bcast_load(C, t, d['rw_w0'][l].rearrange("a n -> (a n)"), 512, "w0")
        A0 = bcast_load(C, t, d['rw_a0'][l], 256, "a0")
        KKb = bcast_load(C, t, d['rw_k_k'][l], 256, "kkb")
        KA = bcast_load(C, t, d['rw_k_a'][l], 256, "ka")
        RK = bcast_load(C, t, d['rw_r_k'][l].rearrange("a n -> (a n)"), 256, "rk")
        OMKA = t.sb([128, 256], F32, "omka")
        s.ts(OMKA, KA, -1.0, 1.0, ALU.mult, ALU.add)
        W2 = t.sb([128, 256], F32, "W2")
        s.dma(W2, d['rw_w2'][l].rearrange("a k n -> (a k) n"))
        A2 = t.sb([64, 256], F32, "A2")
        s.dma(A2, d['rw_a2'][l])
        G2 = t.sb([128, 256], F32, "G2")
        s.dma(G2, d['rw_g2'][l])
        p0s = [t.sb([128, 1088], F32, f"p0{i}") for i in range(2)]
        pms = [t.sb([128, 1088], F32, f"pm{i}") for i in range(2)]
        pns = [t.sb([128, 1088], F32, f"pn{i}") for i in range(2)]
        scs = [t.sb([128, RWC['n']], F32, f"sc{i}") for i in range(2)]
        xs = t.sb([128, 1088], F32, "xs")
        thT = t.sb([128, 128], F32, "thT")
        yaT = t.sb([64, 128], F32, "yaT")
        sgT = t.sb([128, 128], F32, "sgT")
        a = t.sb([128, 256], F32, "a")
        tq = t.sb([128, 256], F32, "tq")
        tq2 = t.sb([128, 256], F32, "tq2")
        r4 = t.sb([128, 4], F32, "r4")
        for i in range(NT):
            p0, pm, pn, sc = p0s[i % 2], pms[i % 2], pns[i % 2], scs[i % 2]
            r0 = prow(i * 128)
            s.dma(p0, C.P[r0:r0 + 128, 1280:2368])
            s.dma(pm, C.P[r0 - 1:r0 + 127, 1280:2368])
            s.dma(pn, C.P[r0 + 1:r0 + 129, 1280:2368])
            s.tt(pm, pm, p0, ALU.subtract)
            s.tt(pn, pn, p0, ALU.subtract, eng='pool')
            s.tt(pm, pm, MU0, ALU.mult)
            s.tt(pn, pn, MU1, ALU.mult, eng='pool')
            s.tt(xs, p0, pm, ALU.add)
            s.tt(xs, xs, pn, ALU.add)
            ps = C.ps[0]
            s.tr(ps[:, 0:128], xs[:, 768:896], C.ident)
            s.act(thT, ps[:, 0:128], AF.Tanh)
            ps = C.ps[1]
            s.tr(ps[0:64, 0:128], xs[:, 896:960], C.ident)
            s.copy(yaT, ps[0:64, 0:128])
            ps = C.ps[2]
            s.tr(ps[:, 0:128], xs[:, 960:1088], C.ident)
            s.act(sgT, ps[:, 0:128], AF.Sigmoid)
            pw = C.ps[3]
            pw2 = C.ps[5]
            s.mm(pw[:, 0:256], thT[0:64, :], W2[0:64, :])
            s.mm(pw2[:, 0:256], thT[64:128, :], W2[64:128, :])
            pa = C.ps[4]
            s.mm(pa[:, 0:256], yaT, A2)
            s.mm(pa[:, 256:512], sgT, G2)
            lw = sc[:, RWC['lwf']:RWC['lwf'] + 512]
            s.tt(lw[:, 0:256], pw[:, 0:256], W0[:, 0:256], ALU.add)
            s.tt(lw[:, 256:512], pw2[:, 0:256], W0[:, 256:512], ALU.add)
            s.act(lw, lw, AF.Sigmoid)
            s.ts(lw, lw, NEG_EXP_HALF, None, ALU.mult, eng='pool')
            s.tt(a, pa[:, 0:256], A0, ALU.add)
            s.act(a, a, AF.Sigmoid)
            s.copy(sc[:, RWC['g']:RWC['g'] + 256], pa[:, 256:512], eng='act')
            s.copy(sc[:, 0:256], xs[:, 0:256], eng='pool')
            s.copy(sc[:, 512:768], xs[:, 512:768], eng='pool')
            kk = sc[:, RWC['kk']:RWC['kk'] + 256]
            s.tt(kk, xs[:, 256:512], KKb, ALU.mult)
            s.tt(tq, kk, kk, ALU.mult)
            s.reduce(r4, tq.rearrange("p (h n) -> p h n", h=4), ALU.add)
            s.act(r4, r4, AF.Sqrt)
            s.ts(r4, r4, 1e-12, None, ALU.max)
            s.op('dve', lambda e: e.reciprocal(r4, r4), reads=[r4], writes=[r4])
            kkv = kk.rearrange("p (h n) -> p h n", h=4)
            s.tt(kkv, kkv, r4.unsqueeze(2).to_broadcast([128, 4, 64]), ALU.mult)
            s.tt(tq, a, KA, ALU.mult, eng='pool')
            s.tt(tq, tq, OMKA, ALU.add, eng='pool')
            s.tt(sc[:, 256:512], xs[:, 256:512], tq, ALU.mult)
            s.tt(sc[:, RWC['b']:RWC['b'] + 256], kk, a, ALU.mult, eng='pool')
            s.tt(tq2, xs[:, 0:256], sc[:, 256:512], ALU.mult)
            s.tt(tq2, tq2, RK, ALU.mult)
            s.reduce(r4, tq2.rearrange("p (h n) -> p h n", h=4), ALU.add)
            s.tt(sc[:, RWC['bonus']:RWC['bonus'] + 256].rearrange("p (h n) -> p h n", h=4),
                 xs[:, 512:768].rearrange("p (h n) -> p h n", h=4), r4.unsqueeze(2).to_broadcast([128, 4, 64]), ALU.mult)
            s.dma(C.SCR[i * 128:(i + 1) * 128, :], sc, writes=[('par', C.SCR)], q='pool')


def scan_consts():
    out = np.zeros((2, 64, 450), np.float32)
    sidx = np.arange(64)[:, None]
    tidx = np.arange(64)[None, :]
    for dd in range(2):
        if dd == 0:
            tri = (sidx <= tidx)
            tris = (sidx < tidx)
            mid = 31
        else:
            tri = (sidx >= tidx)
            tris = (sidx > tidx)
            mid = 32
        tri = tri.astype(np.float32)
        tris = tris.astype(np.float32)
        cm = tri[:, mid:mid + 1]
        out[dd, :, 0:64] = tri - cm
        out[dd, :, 64:128] = tris - cm
        out[dd, :, 128] = cm[:, 0]
        out[dd, :, 129] = 1.0
        out[dd, :, 130:194] = tris.T
        out[dd, :, 194:258] = tris
        out[dd, :, 258:322] = tri
        out[dd, :, 322:386] = tris
        out[dd, :, 386:450] = tris.T
    bd = np.zeros((128, 128), np.float32)
    bd[:64, :64] = 1.0
    bd[64:, 64:] = 1.0
    return dict(scn=out, bdm=bd)


def hq(h):
    return 2 * (h % 2) + h // 2


def scan_gen(C, t, tag, banks, SC, cols, OUTD, delta, nfwd=None, out_chunks=None):
    s, d = C.s, C.d
    nsteps = TOK // 64
    if True:
        K_ = t.sb([64, 2, 450], F32, "scn" + tag)
        s.dma(K_, d['scn'].rearrange("a s n -> s a n"))
        BD = t.sb([128, 128], F32, "BD" + tag)
        s.dma(BD, d['bdm'])
        I4 = t.sb([64, 4, 64], BF16, "I4" + tag)
        for h in range(4):
            s.copy(I4[:, h, :], C.ident[0:64, 0:64])
        H2 = [[t.sb([128, 128], F32, f"H{tag}{dd}{hp}") for hp in range(2)] for dd in range(2)]
        for dd in range(2):
            for hp in range(2):
                s.memset(H2[dd][hp], 0.0)
        bank = [0]

        def nb():
            bank[0] = (bank[0] + 1) % len(banks)
            return banks[bank[0]]

        def mk(name, shape, n=2, dt=F32):
            return [[t.sb(shape, dt, f"{name}{tag}{dd}{k}") for k in range(n)] for dd in range(2)]
        names_tok = ['LW', 'R', 'K', 'V'] + (['KK', 'B'] if delta else [])
        T = {nm: mk(nm, [64, 256]) for nm in names_tok}
        eP = mk('eP', [128, 2, 128])
        eM = mk('eM', [128, 2, 64])
        eS = mk('eS', [128, 2, 2])
        eT = mk('eT', [64, 512])
        fm = {nm: mk(nm, [128, 2, 64], dt=(F32 if nm == 'rTin' else BF16))
              for nm in (['rTm', 'kTm', 'rTin'] + (['kkTm', 'bTm'] if delta else []))}
        ePin = mk('ePin', [128, 2, 64])
        Vb = mk('Vb', [64, 256], dt=BF16)
        kout = mk('kout', [64, 256])
        ArkT = mk('ArkT', [64, 4, 64], dt=BF16)
        for dd in range(2):
            for k_ in range(2):
                s.memset(ArkT[dd][k_], 0.0)
        Osb = mk('Osb', [64, 256])
        tmpH = mk('tmpH', [128, 128], 1)
        if delta:
            bout = mk('bout', [64, 256], dt=BF16)
            kkin = mk('kkin', [64, 256], dt=BF16)
            ArbT = mk('ArbT', [64, 4, 64], dt=BF16)
            AkkT = mk('AkkT', [64, 4, 64], dt=BF16)
            Xq = mk('Xq', [64, 4, 64], 2, dt=BF16)
            Xt = mk('Xt', [64, 4, 64], 2, dt=BF16)
            Pm = mk('Pm', [64, 4, 64], 2, dt=BF16)
            X0 = mk('X0', [64, 4, 64], 1, dt=BF16)
            U0 = mk('U0', [64, 256])
            KtT = mk('KtT', [128, 2, 64])
            U = mk('U', [64, 256], 1, dt=BF16)

        def chunk_of(dd, i):
            if dd == 0:
                return i
            return 3 - i if i < 4 else 71 - i

        for i in range(nsteps):
            b = i % 2
            cks = [chunk_of(0, i), chunk_of(1, i)]
            DD = (0, 1) if (nfwd is None or i < nfwd) else (1,)
            for dd in DD:
                r0 = cks[dd] * 64
                lwc = cols['lwf'] if dd == 0 else cols['lwb']
                kc = cols['kf'] if dd == 0 else cols['kb']
                s.dma(T['LW'][dd][b], SC[r0:r0 + 64, lwc:lwc + 256])
                s.dma(T['R'][dd][b], SC[r0:r0 + 64, cols['r']:cols['r'] + 256])
                s.dma(T['K'][dd][b], SC[r0:r0 + 64, kc:kc + 256])
                s.dma(T['V'][dd][b], SC[r0:r0 + 64, cols['v']:cols['v'] + 256])
                if delta:
                    s.dma(T['KK'][dd][b], SC[r0:r0 + 64, cols['kk']:cols['kk'] + 256])
                    s.dma(T['B'][dd][b], SC[r0:r0 + 64, cols['b']:cols['b'] + 256])
            yield
            for dd in DD:
                LW = T['LW'][dd][b]
                pE = nb()
                for hp in range(2):
                    s.mm(pE[:, hp * 130:(hp + 1) * 130], LW[:, hp * 128:(hp + 1) * 128], K_[:, dd, 0:130], inc=(hp == 1))
                pEv = pE[:, 0:260].rearrange("p (h n) -> p h n", h=2)
                s.act(eP[dd][b], pEv[:, :, 0:128], AF.Exp)
                s.act(eM[dd][b], pEv[:, :, 0:64], AF.Exp, scale=-1.0)
                s.act(eS[dd][b], pEv[:, :, 128:130], AF.Exp)
                pT = nb()
                s.mm(pT[0:64, 0:256], K_[:, dd, 130:194], LW, inc=False)
                s.mm(pT[0:64, 256:512], K_[:, dd, 194:258], LW)
                s.act(eT[dd][b], pT[0:64, :], AF.Exp)
            yield
            for dd in DD:
                pR = nb()
                srcs = ['R', 'K'] + (['KK', 'B'] if delta else [])
                for si, nm in enumerate(srcs):
                    for hp in range(2):
                        last = (si == len(srcs) - 1 and hp == 1)
                        s.tr(pR[:, (si * 2 + hp) * 64:(si * 2 + hp + 1) * 64], T[nm][dd][b][:, hp * 128:(hp + 1) * 128],
                             C.ident[0:64, 0:64], inc=last)
                pv = pR.rearrange("p (a h n) -> p a h n", a=4, h=2)
                s.tt(fm['rTm'][dd][b], pv[:, 0], eP[dd][b][:, :, 0:64], ALU.mult)
                s.tt(fm['kTm'][dd][b], pv[:, 1], eM[dd][b], ALU.mult)
                if delta:
                    s.tt(fm['kkTm'][dd][b], pv[:, 2], eP[dd][b][:, :, 64:128], ALU.mult)
                    s.tt(fm['bTm'][dd][b], pv[:, 3], eM[dd][b], ALU.mult)
                s.tt(ePin[dd][b], eP[dd][b][:, :, 0:64], eS[dd][b][:, :, 0:1].to_broadcast([128, 2, 64]), ALU.mult)
                s.tt(fm['rTin'][dd][b], pv[:, 0], ePin[dd][b], ALU.mult)
                s.copy(Vb[dd][b], T['V'][dd][b], eng='pool')
                s.tt(kout[dd][b], T['K'][dd][b], eT[dd][b][:, 0:256], ALU.mult, eng='pool')
                if delta:
                    s.tt(bout[dd][b], T['B'][dd][b], eT[dd][b][:, 0:256], ALU.mult, eng='pool')
                    s.tt(kkin[dd][b], T['KK'][dd][b], eT[dd][b][:, 256:512], ALU.mult, eng='pool')
            yield
            for dd in DD:
                mInc = K_[:, dd, 258:322].unsqueeze(1).to_broadcast([64, 4, 64])
                mStr = K_[:, dd, 322:386].unsqueeze(1).to_broadcast([64, 4, 64])
                mStrT = K_[:, dd, 386:450].unsqueeze(1).to_broadcast([64, 4, 64])

                def amat(dst, lname, rname, mask, eng='dve', quad=False):
                    pX, pY = nb(), nb()
                    for hp in range(2):
                        for par, pA in ((0, pX), (1, pY)):
                            pr = 64 * par
                            L = fm[lname][dd][b][pr:pr + 64, hp, :]
                            R_ = fm[rname][dd][b][pr:pr + 64, hp, :]
                            o_ = pA[0:64, hp * 64:(hp + 1) * 64]
                            if not quad:
                                s.mm(o_, L, R_, inc=(hp == 1))
                            elif dd == 0:
                                s.mm(o_[0:32, :], L[:, 0:32], R_, inc=False)
                                s.mm(o_[32:64, 32:64], L[:, 32:64], R_[:, 32:64], inc=(hp == 1))
                            else:
                                s.mm(o_[32:64, :], L[:, 32:64], R_, inc=False)
                                s.mm(o_[0:32, 0:32], L[:, 0:32], R_[:, 0:32], inc=(hp == 1))
                    for par, pA in ((0, pX), (1, pY)):
                        dv = dst[:, 2 * par:2 * par + 2, :]
                        pv_ = pA[0:64, 0:128].rearrange("p (h n) -> p h n", h=2)
                        mk_ = mask[:, 0:2, :]
                        if not quad:
                            s.tt(dv, pv_, mk_, ALU.mult, eng=eng)
                        elif dd == 0:
                            s.tt(dv[0:32], pv_[0:32], mk_[0:32], ALU.mult, eng=eng)
                            s.tt(dv[32:64, :, 32:64], pv_[32:64, :, 32:64], mk_[32:64, :, 32:64], ALU.mult, eng=eng)
                        else:
                            s.tt(dv[32:64], pv_[32:64], mk_[32:64], ALU.mult, eng=eng)
                            s.tt(dv[0:32, :, 0:32], pv_[0:32, :, 0:32], mk_[0:32, :, 0:32], ALU.mult, eng=eng)
                amat(ArkT[dd][b], 'kTm', 'rTm', mInc, quad=not delta)
                if delta:
                    amat(ArbT[dd][b], 'bTm', 'rTm', mInc)
                    amat(AkkT[dd][b], 'kTm', 'kkTm', mStr)
                    amat(Xq[dd][0], 'bTm', 'kkTm', mStr)
                    amat(Xt[dd][0], 'kkTm', 'bTm', mStrT)
            if delta:
                for dd in DD:
                    s.tt(Pm[dd][0], I4, Xq[dd][0], ALU.subtract, eng='pool')
                for j in range(5):
                    yield
                    a_, b_ = j % 2, (j + 1) % 2
                    for dd in DD:
                        pQ, pQ2 = nb(), nb()
                        for h in range(4):
                            s.mm(pQ[0:64, h * 64:(h + 1) * 64], Xt[dd][a_][:, h, :], Xq[dd][a_][:, h, :], inc=(h == 3))
                        for h in range(4):
                            s.mm(pQ2[0:64, h * 64:(h + 1) * 64], Xq[dd][a_][:, h, :], Xt[dd][a_][:, h, :], inc=(h == 3))
                        s.copy(Xq[dd][b_], pQ[0:64, 0:256].rearrange("p (h n) -> p h n", h=4), eng='act')
                        s.copy(Xt[dd][b_], pQ2[0:64, 0:256].rearrange("p (h n) -> p h n", h=4))
                    for dd in DD:
                        pP = nb()
                        for h in range(4):
                            s.mm(pP[0:64, h * 64:(h + 1) * 64], Xt[dd][b_][:, h, :], Pm[dd][a_][:, h, :], inc=(h == 3))
                        s.tt(Pm[dd][b_], Pm[dd][a_], pP[0:64, 0:256].rearrange("p (h n) -> p h n", h=4), ALU.add)
                MT = {dd: Pm[dd][1] for dd in DD}
                yield
                for dd in DD:
                    pX = nb()
                    for h in range(4):
                        s.mm(pX[0:64, h * 64:(h + 1) * 64], AkkT[dd][b][:, hq(h), :], Vb[dd][b][:, h * 64:(h + 1) * 64], inc=(h == 3))
                    s.copy(X0[dd][0], pX[0:64, 0:256].rearrange("p (h n) -> p h n", h=4), eng='act')
                    pK0, pK1 = nb(), nb()
                    for hp in range(2):
                        for par, pK in ((0, pK0), (1, pK1)):
                            h = 2 * hp + par
                            pr = 64 * par
                            s.mm(pK[pr:pr + 64, hp * 64:(hp + 1) * 64], kkin[dd][b][:, h * 64:(h + 1) * 64], MT[dd][:, hq(h), :], inc=(hp == 1))
                    s.copy(KtT[dd][b][0:64], pK0[0:64, 0:128].rearrange("p (h n) -> p h n", h=2))
                    s.copy(KtT[dd][b][64:128], pK1[64:128, 0:128].rearrange("p (h n) -> p h n", h=2), eng='act')
                for dd in DD:
                    pU = nb()
                    for h in range(4):
                        s.mm(pU[0:64, h * 64:(h + 1) * 64], MT[dd][:, hq(h), :], X0[dd][0][:, h, :], inc=(h == 3))
                    s.ts(U0[dd][b], pU[0:64, 0:256], -1.0, None, ALU.mult)
            yield
            if delta:
                for dd in DD:
                    pU = nb()
                    for hp in range(2):
                        s.mm(pU[0:64, hp * 128:(hp + 1) * 128], KtT[dd][b][:, hp, :], H2[dd][hp], inc=(hp == 1))
                    s.tt(U[dd][0], U0[dd][b], pU[0:64, 0:256], ALU.subtract)
            yield
            for dd in DD:
                if out_chunks is not None and cks[dd] not in out_chunks:
                    continue
                pO = nb()
                for hp in range(2):
                    s.mm(pO[0:64, hp * 128:(hp + 1) * 128], fm['rTin'][dd][b][:, hp, :], H2[dd][hp], start=(hp == 0), stop=False)
                for h in range(4):
                    last = (h == 3) and not delta
                    s.mm(pO[0:64, h * 64:(h + 1) * 64], ArkT[dd][b][:, hq(h), :], Vb[dd][b][:, h * 64:(h + 1) * 64],
                         start=False, stop=last, inc=last)
                if delta:
                    for h in range(4):
                        s.mm(pO[0:64, h * 64:(h + 1) * 64], ArbT[dd][b][:, hq(h), :], U[dd][0][:, h * 64:(h + 1) * 64],
                             start=False, stop=(h == 3), inc=(h == 3))
                s.copy(Osb[dd][b], pO[0:64, 0:256], eng='act')
                r0 = cks[dd] * 64
                s.dma(OUTD[dd][r0:r0 + 64, :], Osb[dd][b], writes=[('par', OUTD[dd])], q='pool')
            yield
            for dd in DD:
                pH = nb()
                for hp in range(2):
                    cs_ = slice(hp * 128, (hp + 1) * 128)
                    s.mm(pH[:, cs_], kout[dd][b][:, cs_], T['V'][dd][b][:, cs_], start=True, stop=not delta, inc=(hp == 1 and not delta))
                    if delta:
                        s.mm(pH[:, cs_], bout[dd][b][:, cs_], U[dd][0][:, cs_], start=False, stop=True, inc=(hp == 1))
                for hp in range(2):
                    s.tt(tmpH[dd][0], pH[:, hp * 128:(hp + 1) * 128], BD, ALU.mult)
                    s.stt(H2[dd][hp], H2[dd][hp], eS[dd][b][:, hp, 1:2], tmpH[dd][0], ALU.mult, ALU.add)
            yield


def stage_scans(C, l):
    with Stage(C, f"scans{l}") as t:
        banks_r = [C.ps[0], C.ps[1], C.ps[2], C.ps[3]]
        banks_h = [C.ps[4], C.ps[5], C.psb[0].bitcast(F32), C.psb[1].bitcast(F32)]
        kw = {} if l == 0 else dict(nfwd=36, out_chunks=set(range(4, 36)))
        gens = [scan_gen(C, t, 'r', banks_r, C.SCR, RWC, C.ORW, True, **kw),
                scan_gen(C, t, 'h', banks_h, C.SCH, HGC, C.OHG, False, **kw)]
        if l == 0:
            gens.append(precast_gen(C, t))
        live = list(gens)
        rnd = 0
        while live:
            rnd += 1
            for gi, g in enumerate(list(live)):
                if g is gens[-1] and len(gens) == 3 and len(live) > 1 and rnd % 6 != 0:
                    continue
                try:
                    next(g)
                except StopIteration:
                    live.remove(g)


def stage_hg_read(C, l):
    s, d = C.s, C.d
    with Stage(C, f"hgr{l}") as t:
        NG = bcast_load(C, t, d['hg_norm_g'][l], 256, "ng")
        ofs = [t.sb([128, 256], F32, f"of{i}") for i in range(2)]
        obs = [t.sb([128, 256], F32, f"ob{i}") for i in range(2)]
        gs = [t.sb([128, 256], F32, f"g{i}") for i in range(2)]
        sq = t.sb([128, 256], F32, "sq")
        r4 = t.sb([128, 4], F32, "r4")
        for i in (range(NT) if l == 0 else range(2, 18)):
            of, ob, g = ofs[i % 2], obs[i % 2], gs[i % 2]
            r0 = prow(i * 128)
            s.dma(of, C.OHG[0][i * 128:(i + 1) * 128, :])
            s.dma(ob, C.OHG[1][i * 128:(i + 1) * 128, :])
            s.dma(g, C.P[r0:r0 + 128, 1024:1280])
            s.tt(of, of, ob, ALU.add)
            s.tt(sq, of, of, ALU.mult, eng='pool')
            s.reduce(r4, sq.rearrange("p (h n) -> p h n", h=4), ALU.add)
            s.act(r4, r4, AF.Sqrt, bias=EPS, scale=1.0 / 64)
            s.op('dve', lambda e: e.reciprocal(r4, r4), reads=[r4], writes=[r4])
            ov = of.rearrange("p (h n) -> p h n", h=4)
            s.tt(ov, ov, r4.unsqueeze(2).to_broadcast([128, 4, 64]), ALU.mult)
            s.act(g, g, AF.Silu)
            s.tt(of, of, NG, ALU.mult, eng='pool')
            s.tt(of, of, g, ALU.mult)
            s.dma(C.CAT[i * 128:(i + 1) * 128, 0:256], of, writes=[('par', C.CAT)], q='pool')


def stage_rw_read(C, l):
    s, d = C.s, C.d
    with Stage(C, f"rwr{l}") as t:
        LG = bcast_load(C, t, d['rw_ln_g'][l], 256, "lg")
        LB_ = bcast_load(C, t, d['rw_ln_b'][l], 256, "lb")
        ofs = [t.sb([128, 256], F32, f"of{i}") for i in range(2)]
        obs = [t.sb([128, 256], F32, f"ob{i}") for i in range(2)]
        gbs = [t.sb([128, 512], F32, f"gb{i}") for i in range(2)]
        sq = t.sb([128, 256], F32, "sq")
        r4 = t.sb([128, 4], F32, "r4")
        for i in (range(NT) if l == 0 else range(2, 18)):
            of, ob, gb = ofs[i % 2], obs[i % 2], gbs[i % 2]
            s.dma(of, C.ORW[0][i * 128:(i + 1) * 128, :])
            s.dma(ob, C.ORW[1][i * 128:(i + 1) * 128, :])
            s.dma(gb, C.SCR[i * 128:(i + 1) * 128, RWC['g']:RWC['g'] + 512])
            s.tt(of, of, ob, ALU.add)
            ov = of.rearrange("p (h n) -> p h n", h=4)
            s.reduce(r4, ov, ALU.add)
            s.ts(r4, r4, 1.0 / 64, None, ALU.mult)
            s.tt(ov, ov, r4.unsqueeze(2).to_broadcast([128, 4, 64]), ALU.subtract)
            s.tt(sq, of, of, ALU.mult, eng='pool')
            s.reduce(r4, sq.rearrange("p (h n) -> p h n", h=4), ALU.add)
            s.act(r4, r4, AF.Sqrt, bias=64e-5, scale=1.0 / 64)
            s.op('dve', lambda e: e.reciprocal(r4, r4), reads=[r4], writes=[r4])
            s.tt(ov, ov, r4.unsqueeze(2).to_broadcast([128, 4, 64]), ALU.mult)
            s.tt(of, of, LG, ALU.mult, eng='pool')
            s.tt(of, of, LB_, ALU.add)
            s.tt(of, of, gb[:, 256:512], ALU.add, eng='pool')
            s.tt(of, of, gb[:, 0:256], ALU.mult)
            s.dma(C.CAT[i * 128:(i + 1) * 128, 256:512], of, writes=[('par', C.CAT)], q='pool')


_NC_CACHE = {}


def kernel(**inputs):
    if 'nc' not in _NC_CACHE:
        _NC_CACHE['nc'] = build()
    nc = _NC_CACHE['nc']
    in_maps = []
    for b in range(4):
        in_maps.append(host_inputs(inputs, b, rev=False))
        in_maps.append(host_inputs(inputs, b, rev=True))
    res = run_bass_kernel_spmd(nc, in_maps, core_ids=list(range(8)))
    out = np.empty((4, 4096, D), np.float32)
    for b in range(4):
        out[b, 0:2048] = np.asarray(res.results[2 * b]['out'], dtype=np.float32)
        out[b, 2048:4096] = np.asarray(res.results[2 * b + 1]['out'], dtype=np.float32)[::-1]
    return out
```

```python
import numpy as np
import concourse.bass as bass
import concourse.mybir as mybir
from concourse.bass_utils import run_bass_kernel_spmd

F32 = mybir.dt.float32
BF16 = mybir.dt.bfloat16
ALU = mybir.AluOpType
AF = mybir.ActivationFunctionType
AX = mybir.AxisListType


class Sched:
    NDMA = 40
    ROT = 20000

    def __init__(self, nc):
        self.nc = nc
        self.e = dict(pe=nc.tensor, act=nc.scalar, dve=nc.vector, pool=nc.gpsimd, sp=nc.sync)
        self.sem = {}
        self.cnt = {}
        self.nsem = {}
        for k in ('pe', 'act', 'dve', 'pool'):
            self.nsem[k] = 0
            self.sem[k] = nc.alloc_semaphore(f"s_{k}_0")
            self.cnt[k] = 0
        self.seen = {k: {} for k in self.e}
        self.dsem = [nc.alloc_semaphore(f"sd{i}") for i in range(self.NDMA)]
        self.dcnt = [0] * self.NDMA
        self.drr = 0
        self.res = {}
        self.ninst = 0
        self.out_evs = {}

    @staticmethod
    def _merge(dst, src):
        for n, (h, v) in src.items():
            if n not in dst or dst[n][1] < v:
                dst[n] = (h, v)

    def _st(self, key):
        s = self.res.get(key)
        if s is None:
            s = dict(w={}, r={}, old={}, phase='w')
            self.res[key] = s
        return s

    def _key(self, x):
        if isinstance(x, str):
            return x, False
        if isinstance(x, tuple):
            return self._key(x[1])[0], True
        return x.name, False

    def _need(self, reads, writes):
        need = {}
        for r in reads:
            k, _ = self._key(r)
            self._merge(need, self._st(k)['w'])
            if k.startswith('ps'):
                self._merge(need, self._st(k)['r'])
        for w in writes:
            k, par = self._key(w)
            s = self._st(k)
            if par:
                if s['phase'] == 'r':
                    old = {}
                    self._merge(old, s['w'])
                    self._merge(old, s['r'])
                    s['old'] = old
                    s['w'] = {}
                    s['r'] = {}
                    s['phase'] = 'w'
                self._merge(need, s['old'])
            else:
                self._merge(need, s['w'])
                self._merge(need, s['r'])
                self._merge(need, s['old'])
        return need

    def _commit(self, reads, writes, ev):
        for r in reads:
            k, _ = self._key(r)
            s = self._st(k)
            self._merge(s['r'], ev)
            s['phase'] = 'r'
        for w in writes:
            k, par = self._key(w)
            s = self._st(k)
            if par:
                self._merge(s['w'], ev)
            else:
                s['w'] = dict(ev)
                s['r'] = {}
                s['old'] = {}
                s['phase'] = 'w'

    def _wait(self, eng, need):
        seen = self.seen[eng]
        for n, (h, v) in need.items():
            if eng == 'pe' and n.startswith('s_pe_'):
                continue
            if seen.get(n, 0) >= v:
                continue
            for k in ('pe', 'act', 'dve', 'pool'):
                if n == f"s_{k}_{self.nsem[k]}":
                    assert v <= self.cnt[k], (eng, n, v, self.cnt[k])
            self.e[eng].wait_ge(h, v)
            self.ninst += 1
            seen[n] = v

    def op(self, eng, fn, reads=(), writes=(), inc=True):
        need = self._need(reads, writes)
        self._wait(eng, need)
        ins = fn(self.e[eng])
        self.ninst += 1
        if inc:
            if self.cnt[eng] >= self.ROT:
                self.nsem[eng] += 1
                self.sem[eng] = self.nc.alloc_semaphore(f"s_{eng}_{self.nsem[eng]}")
                self.cnt[eng] = 0
            self.cnt[eng] += 1
            ins.then_inc(self.sem[eng], 1)
            ev = {self.sem[eng].name: (self.sem[eng], self.cnt[eng])}
        else:
            assert self.cnt[eng] < self.ROT - 64
            ev = {self.sem[eng].name: (self.sem[eng], self.cnt[eng] + 1)}
        self._commit(reads, writes, ev)
        return ev

    def dma(self, out, in_, reads=None, writes=None, q='sp', **kw):
        reads = [in_] if reads is None else reads
        writes = [out] if writes is None else writes
        i = self.drr
        self.drr = (self.drr + 1) % self.NDMA
        h = self.dsem[i]
        need = self._need(reads, writes)
        if self.dcnt[i] > 0:
            self._merge(need, {h.name: (h, 16 * self.dcnt[i])})
        self._wait(q, need)
        self.e[q].dma_start(out=out, in_=in_, **kw).then_inc(h, 16)
        self.ninst += 1
        self.dcnt[i] += 1
        ev = {h.name: (h, 16 * self.dcnt[i])}
        self._commit(reads, writes, ev)
        return ev

    def finish(self, keys, eng='sp'):
        need = {}
        for k in keys:
            kk, _ = self._key(k)
            self._merge(need, self._st(kk)['w'])
        self._wait(eng, need)

    def mm(self, out, lhsT, rhs, start=True, stop=True, inc=None, extra_r=()):
        inc = stop if inc is None else inc
        return self.op('pe', lambda e: e.matmul(out, lhsT, rhs, start=start, stop=stop),
                       reads=[lhsT, rhs, *extra_r], writes=[out], inc=inc)

    def tr(self, out, in_, ident, inc=True):
        return self.op('pe', lambda e: e.transpose(out, in_, ident), reads=[in_, ident], writes=[out], inc=inc)

    def act(self, out, in_, func, bias=None, scale=None, accum_out=None, eng='act'):
        kw = {}
        reads = [in_]
        writes = [out]
        if bias is not None:
            kw['bias'] = bias
            if not isinstance(bias, (int, float)):
                reads.append(bias)
        if scale is not None:
            kw['scale'] = scale
            if not isinstance(scale, (int, float)):
                reads.append(scale)
        if accum_out is not None:
            kw['accum_out'] = accum_out
            writes.append(accum_out)
        return self.op('act', lambda e: e.activation(out, in_, func, **kw), reads=reads, writes=writes)

    def tt(self, out, in0, in1, op, eng='dve'):
        return self.op(eng, lambda e: e.tensor_tensor(out, in0, in1, op), reads=[in0, in1], writes=[out])

    def ts(self, out, in0, s1, s2, op0, op1=None, eng='dve', accum_out=None):
        reads = [in0] + [s for s in (s1, s2) if s is not None and not isinstance(s, (int, float))]
        writes = [out] + ([accum_out] if accum_out is not None else [])
        kw = {}
        if accum_out is not None:
            kw['accum_out'] = accum_out
        if op1 is None:
            return self.op(eng, lambda e: e.tensor_scalar(out, in0, s1, None, op0, **kw), reads=reads, writes=writes)
        return self.op(eng, lambda e: e.tensor_scalar(out, in0, s1, s2, op0, op1, **kw), reads=reads, writes=writes)

    def stt(self, out, in0, scalar, in1, op0, op1, eng='dve'):
        reads = [in0, in1] + ([scalar] if not isinstance(scalar, (int, float)) else [])
        return self.op(eng, lambda e: e.scalar_tensor_tensor(out, in0, scalar, in1, op0, op1), reads=reads, writes=[out])

    def copy(self, out, in_, eng='dve'):
        if eng == 'act':
            return self.op('act', lambda e: e.copy(out, in_), reads=[in_], writes=[out])
        return self.op(eng, lambda e: e.tensor_copy(out, in_), reads=[in_], writes=[out])

    def memset(self, ap, val, eng='dve'):
        return self.op(eng, lambda e: e.memset(ap, val), reads=[], writes=[ap])

    def reduce(self, out, in_, op, eng='dve'):
        return self.op(eng, lambda e: e.tensor_reduce(out, in_, AX.X, op), reads=[in_], writes=[out])


from contextlib import ExitStack

D = 1024
NT = 34
TOK = 4352
INC = 3232
EPS = 1e-6
PROWS = 4356


def prow(t):
    return 1 + t if t < 256 else 3 + t


class Ctx:
    pass


def barrier(C):
    s = C.s
    need = {}
    for k in ('pe', 'act', 'dve', 'pool'):
        if s.cnt[k] > 0:
            need[s.sem[k].name] = (s.sem[k], s.cnt[k])
    for i, h in enumerate(s.dsem):
        if s.dcnt[i] > 0:
            need[h.name] = (h, 16 * s.dcnt[i])
    for eng in ('pe', 'act', 'dve', 'pool', 'sp'):
        seen = s.seen[eng]
        for n, (h, v) in need.items():
            if seen.get(n, 0) >= v:
                continue
            s.e[eng].wait_ge(h, v)
            s.ninst += 1
            seen[n] = v
    s.res = {}


class Stage:
    def __init__(self, C, name):
        self.C = C
        self.name = name
        self.es = ExitStack()
        self.n = 0

    def __enter__(self):
        self.es.__enter__()
        return self

    def sb(self, shape, dtype=F32, name=None):
        self.n += 1
        nm = f"{self.name}_{name or 't'}{self.n}"
        t = self.es.enter_context(self.C.nc.sbuf_tensor(nm, list(shape), dtype))
        return t.ap()

    def __exit__(self, *a):
        barrier(self.C)
        return self.es.__exit__(*a)


def bcast_load(C, st, vec_ap, n, name, q='sp'):
    t = st.sb([128, n], F32, name)
    C.s.dma(t, vec_ap.partition_broadcast(128), q=q)
    return t


def stage_consts(C):
    nc, s = C.nc, C.s
    C.ident = nc.alloc_sbuf_tensor("ident_sb", [128, 128], F32).ap()
    C.identb = nc.alloc_sbuf_tensor("identb", [128, 128], BF16).ap()
    C.zero = nc.alloc_sbuf_tensor("zero_sb", [128, 808], F32).ap()
    C.ones = nc.alloc_sbuf_tensor("ones_sb", [128, 128], F32).ap()
    s.dma(C.ident, C.d['ident'])
    s.copy(C.identb, C.ident)
    s.memset(C.zero, 0.0)
    s.memset(C.ones, 1.0)
    for r in (0, 257, 258, 4355):
        for q4 in range(4):
            s.dma(C.P[r:r + 1, q4 * 808:(q4 + 1) * 808], C.zero[0:1, :], writes=[('par', C.P)], q='pool')
    C.ps = [nc.alloc_psum_tensor(f"ps{i}", [128, 512], F32).ap() for i in range(6)]
    C.psb = [nc.alloc_psum_tensor(f"psb{i}", [128, 1024], BF16).ap() for i in range(2)]
    barrier(C)


def stage_mod(C, l):
    nc, s, d = C.nc, C.s, C.d
    with Stage(C, f"mod{l}") as t:
        MOD = t.sb([128, 2, 6144], F32, "MOD")
        cT = t.sb([128, 16], F32)
        cs = t.sb([128, 16], F32)
        cb = t.sb([128, 16, 128], F32)
        brow = t.sb([1, 6144], F32)
        s.dma(cT, d['cT'])
        s.dma(brow, d['b_ada'][l:l + 1, :])
        s.act(cs, cT, AF.Silu)
        for j in range(16):
            s.copy(cb[:, j, :], cs[:, j:j + 1].to_broadcast([128, 128]), eng='pool' if j % 2 else 'dve')
        wts = [t.sb([128, 8, 512], F32, f"wa{i}") for i in range(2)]
        wv = d['w_ada'][l].rearrange("(c p) n -> p c n", p=128)
        for n in range(12):
            w = wts[n % 2]
            s.dma(w, wv[:, :, n * 512:(n + 1) * 512])
            for r in range(2):
                ps = C.ps[(2 * n + r) % 4]
                for k in range(8):
                    s.mm(ps, cb[:, r * 8 + k, :], w[:, k, :], start=(k == 0), stop=False)
                s.mm(ps, C.ones[0:1, :], brow[0:1, n * 512:(n + 1) * 512], start=False, stop=True)
                s.copy(MOD[:, r, n * 512:(n + 1) * 512], ps, eng='act' if r else 'dve')
        g1 = bcast_load(C, t, d['norm1_g'][l], 1024, "g1")
        g2 = bcast_load(C, t, d['norm2_g'][l], 1024, "g2")
        for r in range(2):
            for (seg, g) in ((1, g1), (4, g2)):
                sl = MOD[:, r, seg * 1024:(seg + 1) * 1024]
                s.stt(sl, sl, 1.0, g, ALU.add, ALU.mult)
        for r in range(2):
            s.dma(C.MODD[l][r:r + 1, :], MOD[0:1, r, :], writes=[('par', C.MODD[l])], q='pool')


def rmsnorm_mod_T(C, st, xt, G, sh, hT, tmp):
    s = C.s
    s.act(tmp['junk'], xt, AF.Square, accum_out=tmp['ss'])
    s.act(tmp['rs'], tmp['ss'], AF.Sqrt, bias=EPS, scale=1.0 / D)
    s.op('dve', lambda e: e.reciprocal(tmp['rs'], tmp['rs']), reads=[tmp['rs']], writes=[tmp['rs']])
    s.stt(tmp['t1'], xt, tmp['rs'], G, ALU.mult, ALU.mult)
    s.tt(tmp['hb'], tmp['t1'], sh, ALU.add, eng='pool')
    pb = C.psb[tmp['i'] % 2]
    tmp['i'] += 1
    for k in range(8):
        s.tr(pb[:, k * 128:(k + 1) * 128], tmp['hb'][:, k * 128:(k + 1) * 128], C.identb, inc=(k == 7))
    s.copy(hT, pb.rearrange("p (c n) -> p c n", c=8), eng='act')


def norm_tmp(st):
    return dict(junk=st.sb([128, 1024], BF16), ss=st.sb([128, 1], F32), rs=st.sb([128, 1], F32),
                t1=st.sb([128, 1024], F32), hb=st.sb([128, 1024], BF16), i=0)


def load_cast_w(C, st, dst_bf, src_rows, stg, q='sp', eng='pool'):
    C.s.dma(stg, src_rows, q=q)
    C.s.copy(dst_bf, stg, eng=eng)


def stage_win(C, l, MODD, xsrc):
    nc, s, d = C.nc, C.s, C.d
    with Stage(C, f"win{l}") as t:
        wb = t.sb([128, 8, INC], BF16, "wb")
        stg = [t.sb([128, INC], F32, f"stg{i}") for i in range(2)]
        for k in range(8):
            load_cast_w(C, t, wb[:, k, :], d['w_in'][l, k * 128:(k + 1) * 128, :], stg[k % 2], eng='pool')
        tmp = norm_tmp(t)
        seg = {(r, sg): bcast_load(C, t, MODD[r, sg * 1024:(sg + 1) * 1024], 1024, f"seg{r}{sg}") for r in (0, 1) for sg in (0, 1)}
        xts = [t.sb([128, 1024], F32, f"x{i}") for i in range(2)]
        hTs = [t.sb([128, 8, 128], BF16, f"hT{i}") for i in range(2)]
        pts = [t.sb([128, INC], F32, f"p{i}") for i in range(2)]
        for i in range(NT):
            r = 0 if i < 2 else 1
            xt, hT, pt = xts[i % 2], hTs[i % 2], pts[i % 2]
            s.dma(xt, xsrc[i * 128:(i + 1) * 128, :])
            rmsnorm_mod_T(C, t, xt, seg[(r, 1)], seg[(r, 0)], hT, tmp)
            for n in range(7):
                c0, c1 = n * 512, min(INC, (n + 1) * 512)
                ps = C.ps[n % 4]
                for k in range(8):
                    s.mm(ps[:, 0:c1 - c0], hT[:, k, :], wb[:, k, c0:c1], start=(k == 0), stop=(k == 7))
                s.copy(pt[:, c0:c1], ps[:, 0:c1 - c0], eng='act' if n % 2 else 'dve')
            r0 = prow(i * 128)
            s.dma(C.P[r0:r0 + 128, :], pt, writes=[('par', C.P)], q='pool')


WSHAPES = dict(
    w_ada=(2, 1024, 6144), b_ada=(2, 6144), norm1_g=(2, 1024), norm2_g=(2, 1024), w_in=(2, 1024, 3232),
    hg_lb=(2, 2, 256), hg_norm_g=(2, 256), rw_mu=(2, 2, 1088), rw_w0=(2, 2, 256), rw_w2=(2, 2, 64, 256),
    rw_a0=(2, 256), rw_a2=(2, 64, 256), rw_g2=(2, 128, 256), rw_k_k=(2, 256), rw_k_a=(2, 256),
    rw_r_k=(2, 4, 64), rw_ln_g=(2, 256), rw_ln_b=(2, 256), swa_sink=(2, 4), mla_qnorm_g=(2, 192),
    mla_wuq=(2, 192, 384), mla_kvnorm_g=(2, 128), mla_wukv=(2, 128, 512), w_out=(2, 1024, 1024),
    ffn_w1=(1, 1024, 2816), ffn_w3=(1, 1024, 2816), ffn_w2=(1, 2816, 1024), moe_router=(1, 1024, 8),
    moe_w1=(1, 8, 1024, 1408), moe_w3=(1, 8, 1024, 1408), moe_w2=(1, 8, 1408, 1024), final_norm_g=(1024,),
)
CSHAPES = dict(ident=(128, 128), cT=(128, 16), cosH=(4096, 32), sinH=(4096, 32), cosM=(4096, 16), sinM=(4096, 16), maskP=(128, 128), maskN=(128, 128), scn=(2, 64, 450), bdm=(128, 128))


def build(upto=None, dbg=(), din=()):
    nc = bass.Bass("TRN2", target_bir_lowering=False)
    C = Ctx()
    C.nc = nc
    C.s = Sched(nc)
    C.d = {}
    C.d['xin'] = nc.dram_tensor("xin", [TOK, D], F32, kind="ExternalInput").ap()
    for k, shp in {**WSHAPES, **CSHAPES}.items():
        C.d[k] = nc.dram_tensor(k, list(shp), F32, kind="ExternalInput").ap()
    C.out = nc.dram_tensor("out", [2048, D], F32, kind="ExternalOutput").ap()

    def scratch(name, shape):
        kind = "ExternalOutput" if name in dbg else "Internal"
        return nc.dram_tensor(name, list(shape), F32, kind=kind).ap()
    C.P = scratch("P", [PROWS, INC])
    C.scratch = scratch
    C.MODD = [scratch(f"MODD{l}", [2, 6144]) for l in range(2)]
    C.CAT = scratch("CAT", [TOK, D]) if "CAT" not in din else nc.dram_tensor("CAT", [TOK, D], F32, kind="ExternalInput").ap()
    C.XMID = scratch("XMID", [TOK, D])
    C.XS = scratch("XS", [TOK, D])

    def bscratch(name, shape):
        return nc.dram_tensor(name, list(shape), BF16).ap()
    C.WB = dict(f1=bscratch("WBf1", [1024, 2816]), f3=bscratch("WBf3", [1024, 2816]), f2=bscratch("WBf2", [2816, 1024]),
                m1=[bscratch(f"WBm1_{e}", [1024, 1408]) for e in range(8)],
                m3=[bscratch(f"WBm3_{e}", [1024, 1408]) for e in range(8)],
                m2=[bscratch(f"WBm2_{e}", [1408, 1024]) for e in range(8)])
    C.SCH = scratch("SCH", [TOK, HGC['n']])
    C.SCR = scratch("SCR", [TOK, RWC['n']])
    C.OHG = [scratch(f"OHG{i}", [TOK, 256]) for i in range(2)]
    C.ORW = [scratch(f"ORW{i}", [TOK, 256]) for i in range(2)]
    stage_consts(C)
    program(C, upto)
    C.s.finish([C.out] + [n for n in dbg], 'sp')
    print("ninst", C.s.ninst, "sems", {k: v for k, v in C.s.nsem.items()})
    return nc


def program(C, upto):
    if upto in ('ffn', 'moe'):
        with Stage(C, "precast") as t_:
            for _ in precast_gen(C, t_):
                pass
    for l in range(2):
        xsrc = C.d['xin'] if l == 0 else C.XS
        ctx_out = l < 1
        stage_mod(C, l)
        stage_win(C, l, C.MODD[l], xsrc)
        if upto == 'win':
            return
        if upto in (None, 'scans'):
            stage_hg_prep(C, l)
            stage_rw_prep(C, l)
            stage_scans(C, l)
            stage_hg_read(C, l)
            stage_rw_read(C, l)
            if upto == 'scans':
                return
        if upto in (None, 'attn'):
            stage_swa(C, l, ctx_out)
            stage_mla(C, l, ctx_out)
            if upto == 'attn':
                return
        stage_out_ffn(C, l, C.MODD[l], xsrc, moe=(l % 2 == 1))
        if upto == 'ffn':
            return
        if upto == 'L0':
            return
    stage_final(C)


def host_inputs(inputs, b, rev=False):
    m = {}
    cx, xx = np.asarray(inputs['ctx'][b], np.float32), np.asarray(inputs['x'][b], np.float32)
    if rev:
        cx, xx = cx[::-1], xx[::-1]
    m['xin'] = np.ascontiguousarray(np.concatenate([cx, xx], axis=0), dtype=np.float32)
    cc = np.stack([np.asarray(inputs['c_ctx'], np.float32), np.asarray(inputs['c'][b], np.float32)], 0)
    m['cT'] = np.ascontiguousarray(cc.reshape(2, 8, 128).transpose(2, 0, 1).reshape(128, 16))
    m['ident'] = np.eye(128, dtype=np.float32)
    m.update(rope_consts())
    m.update(scan_consts())
    for k in WSHAPES:
        m[k] = np.ascontiguousarray(inputs[k], dtype=np.float32)
    if rev:
        for k in ('cosH', 'sinH', 'cosM', 'sinM'):
            m[k] = np.ascontiguousarray(m[k][::-1])
        perm = np.arange(INC)
        perm[256:512], perm[512:768] = np.arange(512, 768), np.arange(256, 512)
        o = 1280 + 768
        perm[o:o + 64], perm[o + 64:o + 128] = np.arange(o + 64, o + 128), np.arange(o, o + 64)
        m['w_in'] = np.ascontiguousarray(m['w_in'][:, :, perm])
        mu = m['rw_mu'][:, ::-1, :]
        m['rw_mu'] = np.ascontiguousarray(mu[:, :, perm[1280:2368] - 1280])
        m['hg_lb'] = np.ascontiguousarray(m['hg_lb'][:, ::-1, :])
        m['rw_w0'] = np.ascontiguousarray(m['rw_w0'][:, ::-1, :])
        m['rw_w2'] = np.ascontiguousarray(m['rw_w2'][:, ::-1, :, :])
    return m


def precast_gen(C, t):
    nc, s, d = C.nc, C.s, C.d
    jobs = []
    for nm, src in (('f1', d['ffn_w1'][0]), ('f3', d['ffn_w3'][0]), ('f2', d['ffn_w2'][0])):
        jobs.append((C.WB[nm], src))
    for e in range(8):
        jobs.append((C.WB['m1'][e], d['moe_w1'][0, e]))
        jobs.append((C.WB['m3'][e], d['moe_w3'][0, e]))
        jobs.append((C.WB['m2'][e], d['moe_w2'][0, e]))
    NB = 2
    stg = [t.sb([128, 2816], F32, f"pcs{i}") for i in range(NB)]
    wbf = [t.sb([128, 2816], BF16, f"pcw{i}") for i in range(NB)]
    it = 0
    for dst, src in jobs:
        rows, cols = src.shape
        per = max(1, 2816 // cols)
        nchunk = rows // 128
        c0 = 0
        while c0 < nchunk:
            n = min(per, nchunk - c0)
            sg = stg[it % NB]
            wb = wbf[it % NB]
            sv = sg[:, 0:n * cols].rearrange("p (a n) -> p a n", a=n)
            wv = wb[:, 0:n * cols].rearrange("p (a n) -> p a n", a=n)
            s.dma(sv, src[c0 * 128:(c0 + n) * 128, :].rearrange("(a p) n -> p a n", p=128), reads=[src], writes=[sg])
            s.copy(wv, sv, eng='act')
            s.dma(dst[c0 * 128:(c0 + n) * 128, :].rearrange("(a p) n -> p a n", p=128), wv, reads=[wb], writes=[('par', dst)], q='pool')
            c0 += n
            it += 1
            yield


def stage_out_ffn(C, l, MODD, xsrc, moe):
    nc, s, d = C.nc, C.s, C.d
    G = 8
    nlat = 32 if l == 0 else 16
    groups = ([[0, 1]] if l == 0 else []) + [list(range(2 + g * G, 2 + (g + 1) * G)) for g in range(nlat // G)]
    WB = C.WB
    if moe:
        experts = [(WB['m1'][e], WB['m3'][e], WB['m2'][e]) for e in range(8)]
    else:
        experts = [(WB['f1'][:, h * 1408:(h + 1) * 1408], WB['f3'][:, h * 1408:(h + 1) * 1408],
                    WB['f2'][h * 1408:(h + 1) * 1408, :]) for h in range(2)]
    with Stage(C, f"ffn{l}") as t:
        wob = t.sb([128, 8, 1024], BF16, "wob")
        stg = [t.sb([128, 1024], F32, f"stg{i}") for i in range(2)]
        for k in range(8):
            load_cast_w(C, t, wob[:, k, :], d['w_out'][l, k * 128:(k + 1) * 128, :], stg[k % 2])
        segt = {sg: t.sb([128, 1024], F32, f"seg{sg}") for sg in (2, 3, 4, 5)}
        seg = {(r, sg): segt[sg] for r in (0, 1) for sg in (2, 3, 4, 5)}
        cur_r = [None]
        if moe:
            rt = t.sb([128, 8, 8], F32, "router")
            s.dma(rt, d['moe_router'][0].rearrange("(c p) e -> p c e", p=128))
        tmp = norm_tmp(t)
        cts = [t.sb([128, 1024], F32, f"cat{i}") for i in range(1)]
        xts = [t.sb([128, 1024], F32, f"x{i}") for i in range(1)]
        catb = t.sb([128, 1024], BF16, "catb")
        catT = t.sb([128, 8, 128], BF16, "catT")
        xm = t.sb([128, 1024], F32, "xm")
        h2T = t.sb([128, 8, G * 128], BF16, "h2T")
        Y = t.sb([128, G, 1024], F32, "Y")
        actT = t.sb([128, 11, 512], BF16, "actT")
        sa = [t.sb([128, 512], BF16, f"sa{i}") for i in range(2)]
        w1b = t.sb([128, 8, 1408], BF16, "w1b")
        w3b = t.sb([128, 8, 1408], BF16, "w3b")
        w2b = t.sb([128, 11, 1024], BF16, "w2b")
        comb = t.sb([128, G, 8], F32, "comb")
        if moe:
            h2f = t.sb([128, 1024], F32, "h2f")
            h2Tf = t.sb([128, 8, 128], F32, "h2Tf")
            lg = t.sb([128, 8], F32, "lg")
            mx = t.sb([128, 8], F32, "mx")
            nv1 = t.sb([128, 1], F32, "nv1")
            msk = t.sb([128, 8], F32, "msk")
            ex = t.sb([128, 8], F32, "ex")
            sm = t.sb([128, 1], F32, "sm")
        ti = 0
        for grp in groups:
            r = 0 if grp[0] < 2 else 1
            ng = len(grp)
            if cur_r[0] != r:
                cur_r[0] = r
                for sg in (2, 3, 4, 5):
                    s.dma(segt[sg], MODD[r, sg * 1024:(sg + 1) * 1024].partition_broadcast(128))
            for j, i in enumerate(grp):
                ct, xt = cts[0], xts[0]
                ti += 1
                s.dma(ct, C.CAT[i * 128:(i + 1) * 128, :])
                s.dma(xt, xsrc[i * 128:(i + 1) * 128, :])
                s.copy(catb, ct, eng='pool')
                pb = C.psb[tmp['i'] % 2]
                tmp['i'] += 1
                for k in range(8):
                    s.tr(pb[:, k * 128:(k + 1) * 128], catb[:, k * 128:(k + 1) * 128], C.identb, inc=(k == 7))
                s.copy(catT, pb.rearrange("p (c n) -> p c n", c=8), eng='act')
                for hf in range(2):
                    ps = C.ps[hf]
                    for k in range(8):
                        s.mm(ps, catT[:, k, :], wob[:, k, hf * 512:(hf + 1) * 512], start=(k == 0), stop=(k == 7))
                    s.tt(xm[:, hf * 512:(hf + 1) * 512], ps, seg[(r, 2)][:, hf * 512:(hf + 1) * 512], ALU.mult)
                s.tt(xm, xm, xt, ALU.add, eng='pool')
                s.dma(C.XMID[i * 128:(i + 1) * 128, :], xm, writes=[('par', C.XMID)], q='pool')
                rmsnorm_mod_T(C, t, xm, seg[(r, 4)], seg[(r, 3)], h2T[:, :, j * 128:(j + 1) * 128], tmp)
                if moe:
                    s.tt(h2f, tmp['t1'], seg[(r, 3)], ALU.add)
                    for hf in range(2):
                        ps = C.ps[2 + hf]
                        for k in range(4):
                            kk = hf * 4 + k
                            s.tr(ps[:, k * 128:(k + 1) * 128], h2f[:, kk * 128:(kk + 1) * 128], C.ident, inc=(k == 3))
                        s.copy(h2Tf[:, hf * 4:(hf + 1) * 4, :], ps.rearrange("p (c n) -> p c n", c=4), eng='act')
                    ps = C.ps[4]
                    for k in range(8):
                        s.mm(ps[:, 0:8], h2Tf[:, k, :], rt[:, k, :], start=(k == 0), stop=(k == 7))
                    s.copy(lg, ps[:, 0:8])
                    s.op('dve', lambda e: e.max(out=mx, in_=lg), reads=[lg], writes=[mx])
                    s.ts(nv1, mx[:, 0:1], -1.0, None, ALU.mult)
                    s.ts(msk, lg, mx[:, 1:2], None, ALU.is_ge)
                    s.act(ex, lg, AF.Exp, bias=nv1)
                    s.tt(ex, ex, msk, ALU.mult)
                    s.reduce(sm, ex, ALU.add)
                    s.op('dve', lambda e: e.reciprocal(sm, sm), reads=[sm], writes=[sm])
                    s.ts(comb[:, j, :], ex, sm, None, ALU.mult)
            nh = (ng * 128 + 511) // 512
            for ei, (w1, w3, w2) in enumerate(experts):
                s.dma(w1b, w1.rearrange("(k p) n -> p k n", p=128))
                s.dma(w3b, w3.rearrange("(k p) n -> p k n", p=128))
                s.dma(w2b, w2.rearrange("(k p) n -> p k n", p=128))
                for hf in range(nh):
                    t0, t1 = hf * 512, min(ng * 128, (hf + 1) * 512)
                    for c in range(11):
                        pa, pbk = C.ps[(2 * c) % 4], C.ps[(2 * c + 1) % 4]
                        for k in range(8):
                            s.mm(pa[:, 0:t1 - t0], w1b[:, k, c * 128:(c + 1) * 128], h2T[:, k, t0:t1], start=(k == 0), stop=(k == 7))
                        for k in range(8):
                            s.mm(pbk[:, 0:t1 - t0], w3b[:, k, c * 128:(c + 1) * 128], h2T[:, k, t0:t1], start=(k == 0), stop=(k == 7))
                        sx = sa[c % 2]
                        s.act(sx[:, 0:t1 - t0], pa[:, 0:t1 - t0], AF.Silu)
                        s.tt(actT[:, c, 0:t1 - t0], sx[:, 0:t1 - t0], pbk[:, 0:t1 - t0], ALU.mult)
                    for j in range(hf * 4, min(ng, hf * 4 + 4)):
                        jl = j - hf * 4
                        for h2_ in range(2):
                            ps = C.ps[4 + (2 * j + h2_) % 2]
                            for c in range(11):
                                s.mm(ps, actT[:, c, jl * 128:(jl + 1) * 128], w2b[:, c, h2_ * 512:(h2_ + 1) * 512], start=(c == 0), stop=(c == 10))
                            ys = Y[:, j, h2_ * 512:(h2_ + 1) * 512]
                            if moe:
                                if ei == 0:
                                    s.ts(ys, ps, comb[:, j, ei:ei + 1], None, ALU.mult)
                                else:
                                    s.stt(ys, ps, comb[:, j, ei:ei + 1], ys, ALU.mult, ALU.add)
                            else:
                                if ei == 0:
                                    s.copy(ys, ps, eng='act')
                                else:
                                    s.tt(ys, ys, ps, ALU.add)
            for j, i in enumerate(grp):
                xt = xts[0]
                ti += 1
                s.dma(xt, C.XMID[i * 128:(i + 1) * 128, :])
                s.tt(Y[:, j, :], Y[:, j, :], seg[(r, 5)], ALU.mult, eng='pool')
                s.tt(Y[:, j, :], Y[:, j, :], xt, ALU.add)
                s.dma(C.XS[i * 128:(i + 1) * 128, :], Y[:, j, :], writes=[('par', C.XS)], q='pool')


def stage_final(C):
    s, d = C.s, C.d
    with Stage(C, "final") as t:
        g = bcast_load(C, t, d['final_norm_g'], 1024, "fg")
        xts = [t.sb([128, 1024], F32, f"x{i}") for i in range(2)]
        junk = t.sb([128, 1024], BF16)
        ss = t.sb([128, 1], F32)
        rs = t.sb([128, 1], F32)
        for i in range(16):
            xt = xts[i % 2]
            s.dma(xt, C.XS[(i + 2) * 128:(i + 3) * 128, :])
            s.act(junk, xt, AF.Square, accum_out=ss)
            s.act(rs, ss, AF.Sqrt, bias=EPS, scale=1.0 / D)
            s.op('dve', lambda e: e.reciprocal(rs, rs), reads=[rs], writes=[rs])
            s.stt(xt, xt, rs, g, ALU.mult, ALU.mult)
            s.dma(C.out[i * 128:(i + 1) * 128, :], xt, writes=[('par', C.out)], q='pool')


def rope_tm(C, out, x, cos, sin, H, n, ta, tb):
    s = C.s
    xv = x.rearrange("p (h a f n) -> p h a f n", h=H, a=2, f=2)
    ov = out.rearrange("p (h a f n) -> p h a f n", h=H, a=2, f=2)
    x1, x2 = xv[:, :, :, 0, :], xv[:, :, :, 1, :]
    cb = cos.rearrange("p (a n) -> p a n", a=2).unsqueeze(1).to_broadcast([128, H, 2, n])
    sb_ = sin.rearrange("p (a n) -> p a n", a=2).unsqueeze(1).to_broadcast([128, H, 2, n])
    tav = ta[:, 0:H * 2 * n].rearrange("p (h a n) -> p h a n", h=H, a=2)
    tbv = tb[:, 0:H * 2 * n].rearrange("p (h a n) -> p h a n", h=H, a=2)
    s.tt(tav, x1, cb, ALU.mult)
    s.tt(tbv, x2, sb_, ALU.mult, eng='pool')
    s.tt(ov[:, :, :, 0, :], tav, tbv, ALU.subtract)
    s.tt(tav, x1, sb_, ALU.mult)
    s.tt(tbv, x2, cb, ALU.mult, eng='pool')
    s.tt(ov[:, :, :, 1, :], tav, tbv, ALU.add)


def kmax_bcast(C, t, KS, nkm):
    s = C.s
    m1 = t.sb([128, 1], F32)
    row = t.sb([1, 128], F32)
    v = t.sb([1, 1], F32)
    s.reduce(m1, KS, ALU.max)
    ps = C.ps[5]
    s.tr(ps[0:1, 0:128], m1, C.ident)
    s.copy(row, ps[0:1, 0:128])
    s.reduce(v, row, ALU.max)
    s.act(v, v, AF.Sqrt)
    s.ts(v, v, -1.0, None, ALU.mult)
    s.mm(ps[:, 0:1], C.ones[0:1, :], v[0:1, 0:1])
    s.copy(nkm, ps[:, 0:1])


def attend(C, OT, ncols, q_rhs, blocks, scale, PTs, cnt, tail=None):
    s = C.s
    nb = len(blocks)
    LOOK = 2
    pss = {}

    def emit_scores(bi):
        ps = C.ps[(cnt[0] + bi) % 3]
        s.mm(ps[:, 0:ncols], blocks[bi][0], q_rhs)
        pss[bi] = ps
    for bi in range(min(LOOK, nb)):
        emit_scores(bi)
    for bi, (kT, vA, mask) in enumerate(blocks):
        if bi + LOOK < nb:
            emit_scores(bi + LOOK)
        ps = pss.pop(bi)
        PT = PTs[(cnt[0] + bi) % len(PTs)]
        s.act(PT[:, 0:ncols], ps[:, 0:ncols], AF.Exp, scale=scale)
        if mask is not None:
            nrep = ncols // 128
            pv = PT[:, 0:ncols].rearrange("p (r n) -> p r n", r=nrep)
            s.tt(pv, pv, mask.unsqueeze(1).to_broadcast([128, nrep, 128]), ALU.mult, eng='pool')
        s.mm(OT[0:65, 0:ncols], vA, PT[:, 0:ncols], start=(bi == 0), stop=(bi == nb - 1 and tail is None))
    cnt[0] += nb
    if tail is not None:
        tail()


def stage_swa(C, l, ctx_out):
    nc, s, d = C.nc, C.s, C.d
    scale = 64 ** -0.5
    with Stage(C, f"swa{l}") as t:
        KT = t.sb([65, 2, TOK], BF16, "KT")
        VA = t.sb([128, NT, 2, 65], BF16, "VA")
        QT = t.sb([65, 4, TOK], BF16, "QT")
        QR = t.sb([128, NT, 4, 65], BF16, "QR")
        SSQ = t.sb([128, NT, 4], F32, "SSQ")
        KS = t.sb([128, NT], F32, "KS")
        nkm = t.sb([128, 1], F32, "nkm")
        mP = t.sb([128, 128], BF16, "mP")
        mN = t.sb([128, 128], BF16, "mN")
        mf = t.sb([128, 128], F32, "mf")
        s.dma(mf, d['maskP'])
        s.copy(mP, mf)
        s.dma(mf, d['maskN'])
        s.copy(mN, mf)
        SINK = t.sb([65, 4], F32, "SINK")
        s.dma(SINK[64:65, :], d['swa_sink'][l:l + 1, :])
        E64 = t.sb([65, 65], BF16, "E64")
        s.memset(E64, 0.0)
        s.memset(E64[64:65, 64:65], 1.0)
        es = t.sb([65, 256], BF16, "es")
        s.memset(VA[:, :, :, 64:65], 1.0)
        kaug = t.sb([128, 2, 65], BF16, "kaug")
        s.memset(kaug[:, :, 64:65], 1.0)
        pqs = [t.sb([128, 512], F32, f"pq{i}") for i in range(2)]
        cs = [t.sb([128, 32], F32, f"cos{i}") for i in range(2)]
        sn = [t.sb([128, 32], F32, f"sin{i}") for i in range(2)]
        rot = t.sb([128, 384], F32, "rot")
        ta = t.sb([128, 384], F32, "ta")
        tb = t.sb([128, 384], F32, "tb")
        ssk = t.sb([128, 6], F32, "ssk")
        s.memset(KS, 0.0)
        for i in (range(NT) if l == 0 else range(19)):
            pq = pqs[i % 2]
            r0 = prow(i * 128)
            s.dma(pq, C.P[r0:r0 + 128, 2368:2880])
            if i >= 2:
                s.dma(cs[i % 2], d['cosH'][(i - 2) * 128:(i - 1) * 128, :])
                s.dma(sn[i % 2], d['sinH'][(i - 2) * 128:(i - 1) * 128, :])
                rope_tm(C, rot, pq[:, 0:384], cs[i % 2], sn[i % 2], 6, 16, ta, tb)
                src = rot
            else:
                src = pq[:, 0:384]
            s.tt(ta, pq[:, 0:384], pq[:, 0:384], ALU.mult)
            s.reduce(ssk, ta.rearrange("p (h n) -> p h n", h=6), ALU.add)
            s.copy(SSQ[:, i, :], ssk[:, 0:4], eng='pool')
            s.reduce(KS[:, i:i + 1], ssk[:, 4:6], ALU.max)
            s.copy(QR[:, i, :, 0:64], src[:, 0:256].rearrange("p (h n) -> p h n", h=4), eng='pool')
            s.copy(kaug[:, :, 0:64], src[:, 256:384].rearrange("p (h n) -> p h n", h=2))
            s.copy(VA[:, i, :, 0:64], pq[:, 384:512].rearrange("p (h n) -> p h n", h=2), eng='pool')
            pb = C.psb[i % 2]
            for j in range(2):
                s.tr(pb[0:65, j * 128:(j + 1) * 128], kaug[:, j, :], C.identb, inc=(j == 1))
            s.copy(KT[:, :, i * 128:(i + 1) * 128], pb[0:65, 0:256].rearrange("p (j n) -> p j n", j=2), eng='act')
        kmax_bcast(C, t, KS, nkm)
        nq = t.sb([128, 4], F32, "nq")
        for i in (range(NT) if l == 0 else range(2, 18)):
            if i < 2 and not ctx_out:
                continue
            s.act(nq, SSQ[:, i, :], AF.Sqrt)
            s.ts(QR[:, i, :, 64], nq, nkm, None, ALU.mult)
            pb = C.psb[i % 2]
            for h in range(4):
                s.tr(pb[0:65, h * 128:(h + 1) * 128], QR[:, i, h, :], C.identb, inc=(h == 3))
            s.copy(QT[:, :, i * 128:(i + 1) * 128], pb[0:65, 0:512].rearrange("p (j n) -> p j n", j=4), eng='act')
        PTs = [t.sb([128, 256], BF16, f"PT{i}") for i in range(3)]
        osb = t.sb([65, 256], F32, "osb")
        rden = t.sb([128, 2], F32, "rden")
        otile = [t.sb([128, 256], F32, f"ot{i}") for i in range(2)]
        cnt = [0]
        qblocks = ([0, 1] if ctx_out else []) + list(range(2, NT if l == 0 else 18))
        for qi, n in enumerate(qblocks):
            ot = otile[qi % 2]
            for j in range(2):
                if n < 2:
                    kbs = [(0, None), (1, None)]
                else:
                    kbs = [(0, None), (1, None)]
                    if n - 1 >= 2:
                        kbs.append((n - 1, mP))
                    kbs.append((n, None))
                    if n + 1 < NT:
                        kbs.append((n + 1, mN))
                blocks = [(KT[0:65, j, kb * 128:(kb + 1) * 128], VA[:, kb, j, :], m) for kb, m in kbs]
                OT = C.ps[3 + (qi * 2 + j) % 2]
                q_rhs = QT[0:65, 2 * j:2 * j + 2, n * 128:(n + 1) * 128]

                def tail(OT=OT, j=j, n=n):
                    for hh in range(2):
                        s.act(es[64:65, hh * 128:(hh + 1) * 128], QT[64:65, 2 * j + hh, n * 128:(n + 1) * 128], AF.Exp,
                              bias=SINK[64:65, 2 * j + hh:2 * j + hh + 1], scale=scale)
                    s.mm(OT[0:65, 0:256], E64[64:65, :], es[64:65, :], start=False, stop=True)
                attend(C, OT, 256, q_rhs, blocks, scale, PTs, cnt, tail)
                s.copy(osb, OT[0:65, 0:256])
                ps = C.ps[5]
                for hh in range(2):
                    s.tr(ps[:, hh * 65:(hh + 1) * 65], osb[:, hh * 128:(hh + 1) * 128], C.ident[0:65, 0:65], inc=(hh == 1))
                pv = ps[:, 0:130].rearrange("p (h n) -> p h n", h=2)
                s.op('dve', lambda e, pv=pv: e.reciprocal(rden, pv[:, :, 64]), reads=[ps], writes=[rden])
                s.tt(ot[:, j * 128:(j + 1) * 128].rearrange("p (h n) -> p h n", h=2), pv[:, :, 0:64],
                     rden.unsqueeze(2).to_broadcast([128, 2, 64]), ALU.mult)
            s.dma(C.CAT[n * 128:(n + 1) * 128, 512:768], ot, writes=[('par', C.CAT)], q='pool')


def stage_mla(C, l, ctx_out):
    nc, s, d = C.nc, C.s, C.d
    scale = 96 ** -0.5
    with Stage(C, f"mla{l}") as t:
        KT = t.sb([97, 4, TOK], BF16, "KT")
        VA = t.sb([128, NT, 4, 65], BF16, "VA")
        QT = t.sb([97, 4, TOK], BF16, "QT")
        QR = t.sb([128, NT, 4, 97], BF16, "QR")
        SSQ = t.sb([128, NT, 4], F32, "SSQ")
        KS = t.sb([128, NT], F32, "KS")
        nkm = t.sb([128, 1], F32, "nkm")
        s.memset(VA[:, :, :, 64:65], 1.0)
        kaug = t.sb([128, 4, 97], BF16, "kaug")
        s.memset(kaug[:, :, 96:97], 1.0)
        stg = t.sb([128, 512], F32, "stg")
        wq0 = t.sb([128, 384], BF16, "wq0")
        wq1 = t.sb([64, 384], BF16, "wq1")
        wkv = t.sb([128, 512], BF16, "wkv")
        s.dma(stg[:, 0:384], d['mla_wuq'][l, 0:128, :])
        s.copy(wq0, stg[:, 0:384])
        s.dma(stg[0:64, 0:384], d['mla_wuq'][l, 128:192, :])
        s.copy(wq1, stg[0:64, 0:384])
        s.dma(stg, d['mla_wukv'][l])
        s.copy(wkv, stg)
        gq = bcast_load(C, t, d['mla_qnorm_g'][l], 192, "gq")
        gkv = bcast_load(C, t, d['mla_kvnorm_g'][l], 128, "gkv")
        pms = [t.sb([128, 352], F32, f"pm{i}") for i in range(2)]
        cs = [t.sb([128, 16], F32, f"cos{i}") for i in range(2)]
        sn = [t.sb([128, 16], F32, f"sin{i}") for i in range(2)]
        junk = t.sb([128, 192], F32, "junk")
        ss2 = t.sb([128, 2], F32, "ss2")
        cn = t.sb([128, 320], BF16, "cn")
        cT = t.sb([128, 3, 128], BF16, "cT")
        qf = t.sb([128, 384], F32, "qf")
        kvf = t.sb([128, 512], F32, "kvf")
        qr_in = t.sb([128, 128], F32, "qr_in")
        qr_out = t.sb([128, 128], F32, "qr_out")
        kr_out = t.sb([128, 32], F32, "kr_out")
        ta = t.sb([128, 64], F32, "ta")
        tb = t.sb([128, 64], F32, "tb")
        sq = t.sb([128, 512], F32, "sq")
        s4 = t.sb([128, 4], F32, "s4")
        s4b = t.sb([128, 4], F32, "s4b")
        s1 = t.sb([128, 1], F32, "s1")
        for i in range(NT):
            pm = pms[i % 2]
            r0 = prow(i * 128)
            s.dma(pm, C.P[r0:r0 + 128, 2880:3232])
            s.act(junk[:, 0:192], pm[:, 0:192], AF.Square, accum_out=ss2[:, 0:1])
            s.act(junk[:, 0:128], pm[:, 192:320], AF.Square, accum_out=ss2[:, 1:2])
            s.act(ss2[:, 0:1], ss2[:, 0:1], AF.Sqrt, bias=EPS, scale=1.0 / 192)
            s.act(ss2[:, 1:2], ss2[:, 1:2], AF.Sqrt, bias=EPS, scale=1.0 / 128)
            s.op('dve', lambda e: e.reciprocal(ss2, ss2), reads=[ss2], writes=[ss2])
            s.stt(cn[:, 0:192], pm[:, 0:192], ss2[:, 0:1], gq, ALU.mult, ALU.mult)
            s.stt(cn[:, 192:320], pm[:, 192:320], ss2[:, 1:2], gkv, ALU.mult, ALU.mult)
            pb = C.psb[i % 2]
            s.tr(pb[:, 0:128], cn[:, 0:128], C.identb, inc=False)
            s.tr(pb[0:64, 128:256], cn[:, 128:192], C.identb, inc=False)
            s.tr(pb[:, 256:384], cn[:, 192:320], C.identb)
            s.copy(cT, pb[:, 0:384].rearrange("p (c n) -> p c n", c=3), eng='act')
            pq, pk = C.ps[0], C.ps[1]
            s.mm(pq[:, 0:384], cT[:, 0, :], wq0, start=True, stop=False)
            s.mm(pq[:, 0:384], cT[0:64, 1, :], wq1, start=False, stop=True)
            s.mm(pk, cT[:, 2, :], wkv)
            s.copy(qf, pq[:, 0:384], eng='act')
            s.copy(kvf, pk)
            qv = qf.rearrange("p (h n) -> p h n", h=4)
            kvv = kvf.rearrange("p (h n) -> p h n", h=4)
            s.tt(sq[:, 0:384], qf, qf, ALU.mult, eng='pool')
            s.reduce(SSQ[:, i, :], sq[:, 0:384].rearrange("p (h n) -> p h n", h=4), ALU.add)
            s.tt(sq, kvf, kvf, ALU.mult, eng='pool')
            s.reduce(s4, sq.rearrange("p (h n) -> p h n", h=4)[:, :, 0:64], ALU.add)
            s.tt(ta[:, 0:32], pm[:, 320:352], pm[:, 320:352], ALU.mult)
            s.reduce(s1, ta[:, 0:32], ALU.add)
            s.ts(s4b, s4, s1, None, ALU.add)
            s.reduce(KS[:, i:i + 1], s4b, ALU.max)
            if i >= 2:
                s.dma(cs[i % 2], d['cosM'][(i - 2) * 128:(i - 1) * 128, :])
                s.dma(sn[i % 2], d['sinM'][(i - 2) * 128:(i - 1) * 128, :])
                s.copy(qr_in.rearrange("p (h n) -> p h n", h=4), qv[:, :, 64:96], eng='pool')
                rope_tm(C, qr_out, qr_in, cs[i % 2], sn[i % 2], 4, 8, ta, tb)
                rope_tm(C, kr_out, pm[:, 320:352], cs[i % 2], sn[i % 2], 1, 8, ta, tb)
                qr_src = qr_out.rearrange("p (h n) -> p h n", h=4)
                kr_src = kr_out
            else:
                qr_src = qv[:, :, 64:96]
                kr_src = pm[:, 320:352]
            s.copy(QR[:, i, :, 0:64], qv[:, :, 0:64], eng='pool')
            s.copy(QR[:, i, :, 64:96], qr_src, eng='pool')
            s.copy(kaug[:, :, 0:64], kvv[:, :, 0:64])
            s.copy(kaug[:, :, 64:96], kr_src.unsqueeze(1).to_broadcast([128, 4, 32]))
            s.copy(VA[:, i, :, 0:64], kvv[:, :, 64:128], eng='pool')
            pb = C.psb[(i + 1) % 2]
            for h in range(4):
                s.tr(pb[0:97, h * 128:(h + 1) * 128], kaug[:, h, :], C.identb, inc=(h == 3))
            s.copy(KT[:, :, i * 128:(i + 1) * 128], pb[0:97, 0:512].rearrange("p (j n) -> p j n", j=4), eng='act')
        kmax_bcast(C, t, KS, nkm)
        nq = t.sb([128, 4], F32, "nq")
        for i in (range(NT) if l == 0 else range(2, 18)):
            if i < 2 and not ctx_out:
                continue
            s.act(nq, SSQ[:, i, :], AF.Sqrt)
            s.ts(QR[:, i, :, 96], nq, nkm, None, ALU.mult)
            pb = C.psb[i % 2]
            for h in range(4):
                s.tr(pb[0:97, h * 128:(h + 1) * 128], QR[:, i, h, :], C.identb, inc=(h == 3))
            s.copy(QT[:, :, i * 128:(i + 1) * 128], pb[0:97, 0:512].rearrange("p (j n) -> p j n", j=4), eng='act')
        PTs = [t.sb([128, 512], BF16, f"PT{i}") for i in range(3)]
        osb = t.sb([65, 512], F32, "osb")
        rden = t.sb([128, 4], F32, "rden")
        otile = [t.sb([128, 4, 256], F32, f"ot{i}") for i in range(2)]
        cnt = [0]
        qtiles = ([(0, 256)] if ctx_out else []) + [(256 + q * 512, 512) for q in range(8 if l == 0 else 4)]
        for qi, (q0, qn) in enumerate(qtiles):
            ot = otile[qi % 2]
            kbs = [0, 1] if q0 < 256 else list(range(NT))
            for h in range(4):
                blocks = [(KT[0:97, h, kb * 128:(kb + 1) * 128], VA[:, kb, h, :], None) for kb in kbs]
                OT = C.ps[3 + (qi * 4 + h) % 2]
                attend(C, OT, qn, QT[0:97, h, q0:q0 + qn], blocks, scale, PTs, cnt)
                s.copy(osb[:, 0:qn], OT[0:65, 0:qn])
                ps = C.ps[5]
                nsub = qn // 128
                for sb_ in range(nsub):
                    s.tr(ps[:, sb_ * 65:(sb_ + 1) * 65], osb[:, sb_ * 128:(sb_ + 1) * 128], C.ident[0:65, 0:65], inc=(sb_ == nsub - 1))
                pv = ps[:, 0:nsub * 65].rearrange("p (h n) -> p h n", h=nsub)
                s.op('dve', lambda e, pv=pv, nsub=nsub: e.reciprocal(rden[:, 0:nsub], pv[:, :, 64]), reads=[ps], writes=[rden])
                s.tt(ot[:, 0:nsub, h * 64:(h + 1) * 64], pv[:, :, 0:64],
                     rden[:, 0:nsub].unsqueeze(2).to_broadcast([128, nsub, 64]), ALU.mult)
            for sb_ in range(qn // 128):
                s.dma(C.CAT[q0 + sb_ * 128:q0 + (sb_ + 1) * 128, 768:1024], ot[:, sb_, :], writes=[('par', C.CAT)], q='pool')


def rope_consts():
    out = {}
    row = np.repeat(np.arange(64, dtype=np.float32), 64)
    col = np.tile(np.arange(64, dtype=np.float32), 64)
    for nm, rot in (('H', 64), ('M', 32)):
        nf = rot // 4
        inv = (np.float32(10000.0) ** (-np.arange(nf, dtype=np.float32) / np.float32(nf))).astype(np.float32)
        ang = np.stack([row[:, None] * inv, col[:, None] * inv], axis=1).astype(np.float32)
        out['cos' + nm] = np.cos(ang).reshape(4096, 2 * nf).astype(np.float32)
        out['sin' + nm] = np.sin(ang).reshape(4096, 2 * nf).astype(np.float32)
    j = np.arange(128)[:, None]
    i = np.arange(128)[None, :]
    out['maskP'] = (j >= i).astype(np.float32)
    out['maskN'] = (j <= i).astype(np.float32)
    return out


HGC = dict(r=0, kf=256, kb=512, v=768, lwf=1024, lwb=1280, n=1536)
RWC = dict(r=0, kf=256, kb=256, v=512, lwf=768, lwb=1024, kk=1280, b=1536, g=1792, bonus=2048, n=2304)
NEG_EXP_HALF = -0.6065306597126334


def stage_hg_prep(C, l):
    s, d = C.s, C.d
    with Stage(C, f"hgp{l}") as t:
        LB = t.sb([128, 512], F32, "LB")
        OML = t.sb([128, 512], F32, "OML")
        if l == 0:
            s.memset(LB, 0.0)
        else:
            a0 = bcast_load(C, t, d['hg_lb'][0].rearrange("a n -> (a n)"), 512, "a0")
            a1 = bcast_load(C, t, d['hg_lb'][1].rearrange("a n -> (a n)"), 512, "a1")
            s.tt(a1, a1, a0, ALU.subtract)
            s.act(LB, a1, AF.Sigmoid)
        s.ts(OML, LB, -1.0, 1.0, ALU.mult, ALU.add)
        pzs = [t.sb([128, 1024], F32, f"pz{i}") for i in range(2)]
        scs = [t.sb([128, HGC['n']], F32, f"sc{i}") for i in range(2)]
        sg = t.sb([128, 512], F32, "sg")
        for i in range(NT):
            pz, sc = pzs[i % 2], scs[i % 2]
            r0 = prow(i * 128)
            s.dma(pz, C.P[r0:r0 + 128, 0:1024])
            s.act(sc[:, 0:256], pz[:, 0:256], AF.Silu)
            s.copy(sc[:, 768:1024], pz[:, 768:1024], eng='pool')
            s.act(sg, pz[:, 256:768], AF.Sigmoid)
            s.tt(sg, sg, OML, ALU.mult)
            s.tt(sg, sg, LB, ALU.add, eng='pool')
            s.ts(sg, sg, 1e-30, None, ALU.max)
            s.ts(sc[:, 256:768], sg, -1.0, 1.0, ALU.mult, ALU.add, eng='pool')
            s.act(sc[:, 1024:1536], sg, AF.Ln)
            s.dma(C.SCH[i * 128:(i + 1) * 128, :], sc, writes=[('par', C.SCH)], q='pool')


def stage_rw_prep(C, l):
    s, d = C.s, C.d
    with Stage(C, f"rwp{l}") as t:
        MU0 = bcast_load(C, t, d['rw_mu'][l, 0], 1088, "mu0")
        MU1 = bcast_load(C, t, d['rw_mu'][l, 1], 1088, "mu1")
        W0 = bcast_load(C, t, d['rw_w0'][l].rearrange("a n -> (a n)"), 512, "w0")
        A0 = bcast_load(C, t, d['rw_a0'][l], 256, "a0")
        KKb = bcast_load(C, t, d['rw_k_k'][l], 256, "kkb")
        KA = bcast_load(C, t, d['rw_k_a'][l], 256, "ka")
        RK = bcast_load(C, t, d['rw_r_k'][l].rearrange("a n -> (a n)"), 256, "rk")
        OMKA = t.sb([128, 256], F32, "omka")
        s.ts(OMKA, KA, -1.0, 1.0, ALU.mult, ALU.add)
        W2 = t.sb([128, 256], F32, "W2")
        s.dma(W2, d['rw_w2'][l].rearrange("a k n -> (a k) n"))
        A2 = t.sb([64, 256], F32, "A2")
        s.dma(A2, d['rw_a2'][l])
        G2 = t.sb([128, 256], F32, "G2")
        s.dma(G2, d['rw_g2'][l])
        p0s = [t.sb([128, 1088], F32, f"p0{i}") for i in range(2)]
        pms = [t.sb([128, 1088], F32, f"pm{i}") for i in range(2)]
        pns = [t.sb([128, 1088], F32, f"pn{i}") for i in range(2)]
        scs = [t.sb([128, RWC['n']], F32, f"sc{i}") for i in range(2)]
        xs = t.sb([128, 1088], F32, "xs")
        thT = t.sb([128, 128], F32, "thT")
        yaT = t.sb([64, 128], F32, "yaT")
        sgT = t.sb([128, 128], F32, "sgT")
        a = t.sb([128, 256], F32, "a")
        tq = t.sb([128, 256], F32, "tq")
        tq2 = t.sb([128, 256], F32, "tq2")
        r4 = t.sb([128, 4], F32, "r4")
        for i in range(NT):
            p0, pm, pn, sc = p0s[i % 2], pms[i % 2], pns[i % 2], scs[i % 2]
            r0 = prow(i * 128)
            s.dma(p0, C.P[r0:r0 + 128, 1280:2368])
            s.dma(pm, C.P[r0 - 1:r0 + 127, 1280:2368])
            s.dma(pn, C.P[r0 + 1:r0 + 129, 1280:2368])
            s.tt(pm, pm, p0, ALU.subtract)
            s.tt(pn, pn, p0, ALU.subtract, eng='pool')
            s.tt(pm, pm, MU0, ALU.mult)
            s.tt(pn, pn, MU1, ALU.mult, eng='pool')
            s.tt(xs, p0, pm, ALU.add)
            s.tt(xs, xs, pn, ALU.add)
            ps = C.ps[0]
            s.tr(ps[:, 0:128], xs[:, 768:896], C.ident)
            s.act(thT, ps[:, 0:128], AF.Tanh)
            ps = C.ps[1]
            s.tr(ps[0:64, 0:128], xs[:, 896:960], C.ident)
            s.copy(yaT, ps[0:64, 0:128])
            ps = C.ps[2]
            s.tr(ps[:, 0:128], xs[:, 960:1088], C.ident)
            s.act(sgT, ps[:, 0:128], AF.Sigmoid)
            pw = C.ps[3]
            pw2 = C.ps[5]
            s.mm(pw[:, 0:256], thT[0:64, :], W2[0:64, :])
            s.mm(pw2[:, 0:256], thT[64:128, :], W2[64:128, :])
            pa = C.ps[4]
            s.mm(pa[:, 0:256], yaT, A2)
            s.mm(pa[:, 256:512], sgT, G2)
            lw = sc[:, RWC['lwf']:RWC['lwf'] + 512]
            s.tt(lw[:, 0:256], pw[:, 0:256], W0[:, 0:256], ALU.add)
            s.tt(lw[:, 256:512], pw2[:, 0:256], W0[:, 256:512], ALU.add)
            s.act(lw, lw, AF.Sigmoid)
            s.ts(lw, lw, NEG_EXP_HALF, None, ALU.mult, eng='pool')
            s.tt(a, pa[:, 0:256], A0, ALU.add)
            s.act(a, a, AF.Sigmoid)
            s.copy(sc[:, RWC['g']:RWC['g'] + 256], pa[:, 256:512], eng='act')
            s.copy(sc[:, 0:256], xs[:, 0:256], eng='pool')
            s.copy(sc[:, 512:768], xs[:, 512:768], eng='pool')
            kk = sc[:, RWC['kk']:RWC['kk'] + 256]
            s.tt(kk, xs[:, 256:512], KKb, ALU.mult)
            s.tt(tq, kk, kk, ALU.mult)
            s.reduce(r4, tq.rearrange("p (h n) -> p h n", h=4), ALU.add)
            s.act(r4, r4, AF.Sqrt)
            s.ts(r4, r4, 1e-12, None, ALU.max)
            s.op('dve', lambda e: e.reciprocal(r4, r4), reads=[r4], writes=[r4])
            kkv = kk.rearrange("p (h n) -> p h n", h=4)
            s.tt(kkv, kkv, r4.unsqueeze(2).to_broadcast([128, 4, 64]), ALU.mult)
            s.tt(tq, a, KA, ALU.mult, eng='pool')
            s.tt(tq, tq, OMKA, ALU.add, eng='pool')
            s.tt(sc[:, 256:512], xs[:, 256:512], tq, ALU.mult)
            s.tt(sc[:, RWC['b']:RWC['b'] + 256], kk, a, ALU.mult, eng='pool')
            s.tt(tq2, xs[:, 0:256], sc[:, 256:512], ALU.mult)
            s.tt(tq2, tq2, RK, ALU.mult)
            s.reduce(r4, tq2.rearrange("p (h n) -> p h n", h=4), ALU.add)
            s.tt(sc[:, RWC['bonus']:RWC['bonus'] + 256].rearrange("p (h n) -> p h n", h=4),
                 xs[:, 512:768].rearrange("p (h n) -> p h n", h=4), r4.unsqueeze(2).to_broadcast([128, 4, 64]), ALU.mult)
            s.dma(C.SCR[i * 128:(i + 1) * 128, :], sc, writes=[('par', C.SCR)], q='pool')


def scan_consts():
    out = np.zeros((2, 64, 450), np.float32)
    sidx = np.arange(64)[:, None]
    tidx = np.arange(64)[None, :]
    for dd in range(2):
        if dd == 0:
            tri = (sidx <= tidx)
            tris = (sidx < tidx)
            mid = 31
        else:
            tri = (sidx >= tidx)
            tris = (sidx > tidx)
            mid = 32
        tri = tri.astype(np.float32)
        tris = tris.astype(np.float32)
        cm = tri[:, mid:mid + 1]
        out[dd, :, 0:64] = tri - cm
        out[dd, :, 64:128] = tris - cm
        out[dd, :, 128] = cm[:, 0]
        out[dd, :, 129] = 1.0
        out[dd, :, 130:194] = tris.T
        out[dd, :, 194:258] = tris
        out[dd, :, 258:322] = tri
        out[dd, :, 322:386] = tris
        out[dd, :, 386:450] = tris.T
    bd = np.zeros((128, 128), np.float32)
    bd[:64, :64] = 1.0
    bd[64:, 64:] = 1.0
    return dict(scn=out, bdm=bd)


def hq(h):
    return 2 * (h % 2) + h // 2


def scan_gen(C, t, tag, banks, SC, cols, OUTD, delta, nfwd=None, out_chunks=None):
    s, d = C.s, C.d
    nsteps = TOK // 64
    if True:
        K_ = t.sb([64, 2, 450], F32, "scn" + tag)
        s.dma(K_, d['scn'].rearrange("a s n -> s a n"))
        BD = t.sb([128, 128], F32, "BD" + tag)
        s.dma(BD, d['bdm'])
        I4 = t.sb([64, 4, 64], BF16, "I4" + tag)
        for h in range(4):
            s.copy(I4[:, h, :], C.ident[0:64, 0:64])
        H2 = [[t.sb([128, 128], F32, f"H{tag}{dd}{hp}") for hp in range(2)] for dd in range(2)]
        for dd in range(2):
            for hp in range(2):
                s.memset(H2[dd][hp], 0.0)
        bank = [0]

        def nb():
            bank[0] = (bank[0] + 1) % len(banks)
            return banks[bank[0]]

        def mk(name, shape, n=2, dt=F32):
            return [[t.sb(shape, dt, f"{name}{tag}{dd}{k}") for k in range(n)] for dd in range(2)]
        names_tok = ['LW', 'R', 'K', 'V'] + (['KK', 'B'] if delta else [])
        T = {nm: mk(nm, [64, 256]) for nm in names_tok}
        eP = mk('eP', [128, 2, 128])
        eM = mk('eM', [128, 2, 64])
        eS = mk('eS', [128, 2, 2])
        eT = mk('eT', [64, 512])
        fm = {nm: mk(nm, [128, 2, 64], dt=(F32 if nm == 'rTin' else BF16))
              for nm in (['rTm', 'kTm', 'rTin'] + (['kkTm', 'bTm'] if delta else []))}
        ePin = mk('ePin', [128, 2, 64])
        Vb = mk('Vb', [64, 256], dt=BF16)
        kout = mk('kout', [64, 256])
        ArkT = mk('ArkT', [64, 4, 64], dt=BF16)
        for dd in range(2):
            for k_ in range(2):
                s.memset(ArkT[dd][k_], 0.0)
        Osb = mk('Osb', [64, 256])
        tmpH = mk('tmpH', [128, 128], 1)
        if delta:
            bout = mk('bout', [64, 256], dt=BF16)
            kkin = mk('kkin', [64, 256], dt=BF16)
            ArbT = mk('ArbT', [64, 4, 64], dt=BF16)
            AkkT = mk('AkkT', [64, 4, 64], dt=BF16)
            Xq = mk('Xq', [64, 4, 64], 2, dt=BF16)
            Xt = mk('Xt', [64, 4, 64], 2, dt=BF16)
            Pm = mk('Pm', [64, 4, 64], 2, dt=BF16)
            X0 = mk('X0', [64, 4, 64], 1, dt=BF16)
            U0 = mk('U0', [64, 256])
            KtT = mk('KtT', [128, 2, 64])
            U = mk('U', [64, 256], 1, dt=BF16)

        def chunk_of(dd, i):
            if dd == 0:
                return i
            return 3 - i if i < 4 else 71 - i

        for i in range(nsteps):
            b = i % 2
            cks = [chunk_of(0, i), chunk_of(1, i)]
            DD = (0, 1) if (nfwd is None or i < nfwd) else (1,)
            for dd in DD:
                r0 = cks[dd] * 64
                lwc = cols['lwf'] if dd == 0 else cols['lwb']
                kc = cols['kf'] if dd == 0 else cols['kb']
                s.dma(T['LW'][dd][b], SC[r0:r0 + 64, lwc:lwc + 256])
                s.dma(T['R'][dd][b], SC[r0:r0 + 64, cols['r']:cols['r'] + 256])
                s.dma(T['K'][dd][b], SC[r0:r0 + 64, kc:kc + 256])
                s.dma(T['V'][dd][b], SC[r0:r0 + 64, cols['v']:cols['v'] + 256])
                if delta:
                    s.dma(T['KK'][dd][b], SC[r0:r0 + 64, cols['kk']:cols['kk'] + 256])
                    s.dma(T['B'][dd][b], SC[r0:r0 + 64, cols['b']:cols['b'] + 256])
            yield
            for dd in DD:
                LW = T['LW'][dd][b]
                pE = nb()
                for hp in range(2):
                    s.mm(pE[:, hp * 130:(hp + 1) * 130], LW[:, hp * 128:(hp + 1) * 128], K_[:, dd, 0:130], inc=(hp == 1))
                pEv = pE[:, 0:260].rearrange("p (h n) -> p h n", h=2)
                s.act(eP[dd][b], pEv[:, :, 0:128], AF.Exp)
                s.act(eM[dd][b], pEv[:, :, 0:64], AF.Exp, scale=-1.0)
                s.act(eS[dd][b], pEv[:, :, 128:130], AF.Exp)
                pT = nb()
                s.mm(pT[0:64, 0:256], K_[:, dd, 130:194], LW, inc=False)
                s.mm(pT[0:64, 256:512], K_[:, dd, 194:258], LW)
                s.act(eT[dd][b], pT[0:64, :], AF.Exp)
            yield
            for dd in DD:
                pR = nb()
                srcs = ['R', 'K'] + (['KK', 'B'] if delta else [])
                for si, nm in enumerate(srcs):
                    for hp in range(2):
                        last = (si == len(srcs) - 1 and hp == 1)
                        s.tr(pR[:, (si * 2 + hp) * 64:(si * 2 + hp + 1) * 64], T[nm][dd][b][:, hp * 128:(hp + 1) * 128],
                             C.ident[0:64, 0:64], inc=last)
                pv = pR.rearrange("p (a h n) -> p a h n", a=4, h=2)
                s.tt(fm['rTm'][dd][b], pv[:, 0], eP[dd][b][:, :, 0:64], ALU.mult)
                s.tt(fm['kTm'][dd][b], pv[:, 1], eM[dd][b], ALU.mult)
                if delta:
                    s.tt(fm['kkTm'][dd][b], pv[:, 2], eP[dd][b][:, :, 64:128], ALU.mult)
                    s.tt(fm['bTm'][dd][b], pv[:, 3], eM[dd][b], ALU.mult)
                s.tt(ePin[dd][b], eP[dd][b][:, :, 0:64], eS[dd][b][:, :, 0:1].to_broadcast([128, 2, 64]), ALU.mult)
                s.tt(fm['rTin'][dd][b], pv[:, 0], ePin[dd][b], ALU.mult)
                s.copy(Vb[dd][b], T['V'][dd][b], eng='pool')
                s.tt(kout[dd][b], T['K'][dd][b], eT[dd][b][:, 0:256], ALU.mult, eng='pool')
                if delta:
                    s.tt(bout[dd][b], T['B'][dd][b], eT[dd][b][:, 0:256], ALU.mult, eng='pool')
                    s.tt(kkin[dd][b], T['KK'][dd][b], eT[dd][b][:, 256:512], ALU.mult, eng='pool')
            yield
            for dd in DD:
                mInc = K_[:, dd, 258:322].unsqueeze(1).to_broadcast([64, 4, 64])
                mStr = K_[:, dd, 322:386].unsqueeze(1).to_broadcast([64, 4, 64])
                mStrT = K_[:, dd, 386:450].unsqueeze(1).to_broadcast([64, 4, 64])

                def amat(dst, lname, rname, mask, eng='dve', quad=False):
                    pX, pY = nb(), nb()
                    for hp in range(2):
                        for par, pA in ((0, pX), (1, pY)):
                            pr = 64 * par
                            L = fm[lname][dd][b][pr:pr + 64, hp, :]
                            R_ = fm[rname][dd][b][pr:pr + 64, hp, :]
                            o_ = pA[0:64, hp * 64:(hp + 1) * 64]
                            if not quad:
                                s.mm(o_, L, R_, inc=(hp == 1))
                            elif dd == 0:
                                s.mm(o_[0:32, :], L[:, 0:32], R_, inc=False)
                                s.mm(o_[32:64, 32:64], L[:, 32:64], R_[:, 32:64], inc=(hp == 1))
                            else:
                                s.mm(o_[32:64, :], L[:, 32:64], R_, inc=False)
                                s.mm(o_[0:32, 0:32], L[:, 0:32], R_[:, 0:32], inc=(hp == 1))
                    for par, pA in ((0, pX), (1, pY)):
                        dv = dst[:, 2 * par:2 * par + 2, :]
                        pv_ = pA[0:64, 0:128].rearrange("p (h n) -> p h n", h=2)
                        mk_ = mask[:, 0:2, :]
                        if not quad:
                            s.tt(dv, pv_, mk_, ALU.mult, eng=eng)
                        elif dd == 0:
                            s.tt(dv[0:32], pv_[0:32], mk_[0:32], ALU.mult, eng=eng)
                            s.tt(dv[32:64, :, 32:64], pv_[32:64, :, 32:64], mk_[32:64, :, 32:64], ALU.mult, eng=eng)
                        else:
                            s.tt(dv[32:64], pv_[32:64], mk_[32:64], ALU.mult, eng=eng)
                            s.tt(dv[0:32, :, 0:32], pv_[0:32, :, 0:32], mk_[0:32, :, 0:32], ALU.mult, eng=eng)
                amat(ArkT[dd][b], 'kTm', 'rTm', mInc, quad=not delta)
                if delta:
                    amat(ArbT[dd][b], 'bTm', 'rTm', mInc)
                    amat(AkkT[dd][b], 'kTm', 'kkTm', mStr)
                    amat(Xq[dd][0], 'bTm', 'kkTm', mStr)
                    amat(Xt[dd][0], 'kkTm', 'bTm', mStrT)
            if delta:
                for dd in DD:
                    s.tt(Pm[dd][0], I4, Xq[dd][0], ALU.subtract, eng='pool')
                for j in range(5):
                    yield
                    a_, b_ = j % 2, (j + 1) % 2
                    for dd in DD:
                        pQ, pQ2 = nb(), nb()
                        for h in range(4):
                            s.mm(pQ[0:64, h * 64:(h + 1) * 64], Xt[dd][a_][:, h, :], Xq[dd][a_][:, h, :], inc=(h == 3))
                        for h in range(4):
                            s.mm(pQ2[0:64, h * 64:(h + 1) * 64], Xq[dd][a_][:, h, :], Xt[dd][a_][:, h, :], inc=(h == 3))
                        s.copy(Xq[dd][b_], pQ[0:64, 0:256].rearrange("p (h n) -> p h n", h=4), eng='act')
                        s.copy(Xt[dd][b_], pQ2[0:64, 0:256].rearrange("p (h n) -> p h n", h=4))
                    for dd in DD:
                        pP = nb()
                        for h in range(4):
                            s.mm(pP[0:64, h * 64:(h + 1) * 64], Xt[dd][b_][:, h, :], Pm[dd][a_][:, h, :], inc=(h == 3))
                        s.tt(Pm[dd][b_], Pm[dd][a_], pP[0:64, 0:256].rearrange("p (h n) -> p h n", h=4), ALU.add)
                MT = {dd: Pm[dd][1] for dd in DD}
                yield
                for dd in DD:
                    pX = nb()
                    for h in range(4):
                        s.mm(pX[0:64, h * 64:(h + 1) * 64], AkkT[dd][b][:, hq(h), :], Vb[dd][b][:, h * 64:(h + 1) * 64], inc=(h == 3))
                    s.copy(X0[dd][0], pX[0:64, 0:256].rearrange("p (h n) -> p h n", h=4), eng='act')
                    pK0, pK1 = nb(), nb()
                    for hp in range(2):
                        for par, pK in ((0, pK0), (1, pK1)):
                            h = 2 * hp + par
                            pr = 64 * par
                            s.mm(pK[pr:pr + 64, hp * 64:(hp + 1) * 64], kkin[dd][b][:, h * 64:(h + 1) * 64], MT[dd][:, hq(h), :], inc=(hp == 1))
                    s.copy(KtT[dd][b][0:64], pK0[0:64, 0:128].rearrange("p (h n) -> p h n", h=2))
                    s.copy(KtT[dd][b][64:128], pK1[64:128, 0:128].rearrange("p (h n) -> p h n", h=2), eng='act')
                for dd in DD:
                    pU = nb()
                    for h in range(4):
                        s.mm(pU[0:64, h * 64:(h + 1) * 64], MT[dd][:, hq(h), :], X0[dd][0][:, h, :], inc=(h == 3))
                    s.ts(U0[dd][b], pU[0:64, 0:256], -1.0, None, ALU.mult)
            yield
            if delta:
                for dd in DD:
                    pU = nb()
                    for hp in range(2):
                        s.mm(pU[0:64, hp * 128:(hp + 1) * 128], KtT[dd][b][:, hp, :], H2[dd][hp], inc=(hp == 1))
                    s.tt(U[dd][0], U0[dd][b], pU[0:64, 0:256], ALU.subtract)
            yield
            for dd in DD:
                if out_chunks is not None and cks[dd] not in out_chunks:
                    continue
                pO = nb()
                for hp in range(2):
                    s.mm(pO[0:64, hp * 128:(hp + 1) * 128], fm['rTin'][dd][b][:, hp, :], H2[dd][hp], start=(hp == 0), stop=False)
                for h in range(4):
                    last = (h == 3) and not delta
                    s.mm(pO[0:64, h * 64:(h + 1) * 64], ArkT[dd][b][:, hq(h), :], Vb[dd][b][:, h * 64:(h + 1) * 64],
                         start=False, stop=last, inc=last)
                if delta:
                    for h in range(4):
                        s.mm(pO[0:64, h * 64:(h + 1) * 64], ArbT[dd][b][:, hq(h), :], U[dd][0][:, h * 64:(h + 1) * 64],
                             start=False, stop=(h == 3), inc=(h == 3))
                s.copy(Osb[dd][b], pO[0:64, 0:256], eng='act')
                r0 = cks[dd] * 64
                s.dma(OUTD[dd][r0:r0 + 64, :], Osb[dd][b], writes=[('par', OUTD[dd])], q='pool')
            yield
            for dd in DD:
                pH = nb()
                for hp in range(2):
                    cs_ = slice(hp * 128, (hp + 1) * 128)
                    s.mm(pH[:, cs_], kout[dd][b][:, cs_], T['V'][dd][b][:, cs_], start=True, stop=not delta, inc=(hp == 1 and not delta))
                    if delta:
                        s.mm(pH[:, cs_], bout[dd][b][:, cs_], U[dd][0][:, cs_], start=False, stop=True, inc=(hp == 1))
                for hp in range(2):
                    s.tt(tmpH[dd][0], pH[:, hp * 128:(hp + 1) * 128], BD, ALU.mult)
                    s.stt(H2[dd][hp], H2[dd][hp], eS[dd][b][:, hp, 1:2], tmpH[dd][0], ALU.mult, ALU.add)
            yield


def stage_scans(C, l):
    with Stage(C, f"scans{l}") as t:
        banks_r = [C.ps[0], C.ps[1], C.ps[2], C.ps[3]]
        banks_h = [C.ps[4], C.ps[5], C.psb[0].bitcast(F32), C.psb[1].bitcast(F32)]
        kw = {} if l == 0 else dict(nfwd=36, out_chunks=set(range(4, 36)))
        gens = [scan_gen(C, t, 'r', banks_r, C.SCR, RWC, C.ORW, True, **kw),
                scan_gen(C, t, 'h', banks_h, C.SCH, HGC, C.OHG, False, **kw)]
        if l == 0:
            gens.append(precast_gen(C, t))
        live = list(gens)
        rnd = 0
        while live:
            rnd += 1
            for gi, g in enumerate(list(live)):
                if g is gens[-1] and len(gens) == 3 and len(live) > 1 and rnd % 6 != 0:
                    continue
                try:
                    next(g)
                except StopIteration:
                    live.remove(g)


def stage_hg_read(C, l):
    s, d = C.s, C.d
    with Stage(C, f"hgr{l}") as t:
        NG = bcast_load(C, t, d['hg_norm_g'][l], 256, "ng")
        ofs = [t.sb([128, 256], F32, f"of{i}") for i in range(2)]
        obs = [t.sb([128, 256], F32, f"ob{i}") for i in range(2)]
        gs = [t.sb([128, 256], F32, f"g{i}") for i in range(2)]
        sq = t.sb([128, 256], F32, "sq")
        r4 = t.sb([128, 4], F32, "r4")
        for i in (range(NT) if l == 0 else range(2, 18)):
            of, ob, g = ofs[i % 2], obs[i % 2], gs[i % 2]
            r0 = prow(i * 128)
            s.dma(of, C.OHG[0][i * 128:(i + 1) * 128, :])
            s.dma(ob, C.OHG[1][i * 128:(i + 1) * 128, :])
            s.dma(g, C.P[r0:r0 + 128, 1024:1280])
            s.tt(of, of, ob, ALU.add)
            s.tt(sq, of, of, ALU.mult, eng='pool')
            s.reduce(r4, sq.rearrange("p (h n) -> p h n", h=4), ALU.add)
            s.act(r4, r4, AF.Sqrt, bias=EPS, scale=1.0 / 64)
            s.op('dve', lambda e: e.reciprocal(r4, r4), reads=[r4], writes=[r4])
            ov = of.rearrange("p (h n) -> p h n", h=4)
            s.tt(ov, ov, r4.unsqueeze(2).to_broadcast([128, 4, 64]), ALU.mult)
            s.act(g, g, AF.Silu)
            s.tt(of, of, NG, ALU.mult, eng='pool')
            s.tt(of, of, g, ALU.mult)
            s.dma(C.CAT[i * 128:(i + 1) * 128, 0:256], of, writes=[('par', C.CAT)], q='pool')


def stage_rw_read(C, l):
    s, d = C.s, C.d
    with Stage(C, f"rwr{l}") as t:
        LG = bcast_load(C, t, d['rw_ln_g'][l], 256, "lg")
        LB_ = bcast_load(C, t, d['rw_ln_b'][l], 256, "lb")
        ofs = [t.sb([128, 256], F32, f"of{i}") for i in range(2)]
        obs = [t.sb([128, 256], F32, f"ob{i}") for i in range(2)]
        gbs = [t.sb([128, 512], F32, f"gb{i}") for i in range(2)]
        sq = t.sb([128, 256], F32, "sq")
        r4 = t.sb([128, 4], F32, "r4")
        for i in (range(NT) if l == 0 else range(2, 18)):
            of, ob, gb = ofs[i % 2], obs[i % 2], gbs[i % 2]
            s.dma(of, C.ORW[0][i * 128:(i + 1) * 128, :])
            s.dma(ob, C.ORW[1][i * 128:(i + 1) * 128, :])
            s.dma(gb, C.SCR[i * 128:(i + 1) * 128, RWC['g']:RWC['g'] + 512])
            s.tt(of, of, ob, ALU.add)
            ov = of.rearrange("p (h n) -> p h n", h=4)
            s.reduce(r4, ov, ALU.add)
            s.ts(r4, r4, 1.0 / 64, None, ALU.mult)
            s.tt(ov, ov, r4.unsqueeze(2).to_broadcast([128, 4, 64]), ALU.subtract)
            s.tt(sq, of, of, ALU.mult, eng='pool')
            s.reduce(r4, sq.rearrange("p (h n) -> p h n", h=4), ALU.add)
            s.act(r4, r4, AF.Sqrt, bias=64e-5, scale=1.0 / 64)
            s.op('dve', lambda e: e.reciprocal(r4, r4), reads=[r4], writes=[r4])
            s.tt(ov, ov, r4.unsqueeze(2).to_broadcast([128, 4, 64]), ALU.mult)
            s.tt(of, of, LG, ALU.mult, eng='pool')
            s.tt(of, of, LB_, ALU.add)
            s.tt(of, of, gb[:, 256:512], ALU.add, eng='pool')
            s.tt(of, of, gb[:, 0:256], ALU.mult)
            s.dma(C.CAT[i * 128:(i + 1) * 128, 256:512], of, writes=[('par', C.CAT)], q='pool')


_NC_CACHE = {}


def kernel(**inputs):
    if 'nc' not in _NC_CACHE:
        _NC_CACHE['nc'] = build()
    nc = _NC_CACHE['nc']
    in_maps = []
    for b in range(4):
        in_maps.append(host_inputs(inputs, b, rev=False))
        in_maps.append(host_inputs(inputs, b, rev=True))
    res = run_bass_kernel_spmd(nc, in_maps, core_ids=list(range(8)))
    out = np.empty((4, 4096, D), np.float32)
    for b in range(4):
        out[b, 0:2048] = np.asarray(res.results[2 * b]['out'], dtype=np.float32)
        out[b, 2048:4096] = np.asarray(res.results[2 * b + 1]['out'], dtype=np.float32)[::-1]
    return out
```
